# Optimizing a Trainium2 kernel written in Bass

```python
import jax, jax.numpy as jnp
from jax import lax
import numpy as np

D_MODEL = 1024
BATCH = 2
SEQ = 8192
DEPTH = 4

HEAD_DIM = 64
RET_HEADS = (3 * D_MODEL // 8) // HEAD_DIM
MOBA_HEADS = (3 * D_MODEL // 8) // HEAD_DIM
CONV_GROUPS = (D_MODEL // 4) // HEAD_DIM
RET_WIDTH = RET_HEADS * HEAD_DIM
MOBA_WIDTH = MOBA_HEADS * HEAD_DIM
CONV_WIDTH = CONV_GROUPS * HEAD_DIM
MIX_WIDTH = RET_WIDTH + MOBA_WIDTH + CONV_WIDTH
IN_WIDTH = 4 * RET_WIDTH + 3 * MOBA_WIDTH + 2 * CONV_WIDTH
D_FF = ((8 * D_MODEL // 3 + 255) // 256) * 256
RET_CHUNK = 256
RET_ROT_BASE = 10000.0
MOBA_BLOCK = 256
MOBA_TOPK = 3
MOBA_QBLOCK = 128
ROPE_THETA = 500000.0
ROPE_DIMS = HEAD_DIM // 4
CONV_KERNEL = 31
FFN_RES_WEIGHT = 0.5
EPS = 1e-6
PAD_MULT = 256

kernel_name = "hybrid_retention_moba_conformer_macaron"


def rms_norm(x, g):
    xf = x.astype(jnp.float32)
    y = xf * lax.rsqrt(jnp.mean(xf * xf, -1, keepdims=True) + EPS)
    return (y * g.astype(jnp.float32)).astype(x.dtype)


def swiglu(h, wg, wu, wd):
    return (jax.nn.silu(h @ wg) * (h @ wu)) @ wd


def to_heads(t, n_heads):
    b, s, _ = t.shape
    return t.reshape(b, s, n_heads, HEAD_DIM).transpose(0, 2, 1, 3)


def from_heads(t):
    b, h, s, d = t.shape
    return t.transpose(0, 2, 1, 3).reshape(b, s, h * d)


def rotary(x, pos, inv_freq):
    half = inv_freq.shape[0]
    n_rot = 2 * half
    ang = pos.astype(jnp.float32)[:, None, :, None] * inv_freq
    cos, sin = jnp.cos(ang), jnp.sin(ang)
    xr = x[..., :n_rot].astype(jnp.float32)
    x1, x2 = xr[..., :half], xr[..., half:]
    rot = jnp.concatenate([x1 * cos - x2 * sin, x2 * cos + x1 * sin], -1).astype(x.dtype)
    return jnp.concatenate([rot, x[..., n_rot:]], -1)


def retention(q, k, v):
    b, h, s, d = q.shape
    c = RET_CHUNK
    n = s // c
    log_gamma = jnp.log1p(-jnp.exp2(-5.0 - jnp.arange(h, dtype=jnp.float32)))
    lg = log_gamma[:, None]
    qc = q.reshape(b, h, n, c, d)
    kc = (k * (d ** -0.5)).reshape(b, h, n, c, d)
    vc = v.reshape(b, h, n, c, d)
    idx = jnp.arange(c, dtype=jnp.float32)
    rel = idx[:, None] - idx[None, :]
    decay_in = jnp.where(rel >= 0, jnp.exp(lg[:, :, None] * jnp.maximum(rel, 0.0)), 0.0)
    scores = jnp.einsum('bhncd,bhnmd->bhncm', qc, kc) * decay_in[None, :, None]
    inner = jnp.einsum('bhncm,bhnme->bhnce', scores, vc)
    w_state = jnp.exp(lg * (c - 1 - idx))
    kv = jnp.einsum('bhncd,hc,bhnce->bhnde', kc, w_state, vc)
    chunk_decay = jnp.exp(log_gamma * c).astype(kv.dtype)[None, :, None, None]

    def step(state, kv_i):
        return state * chunk_decay + kv_i, state

    _, prev = lax.scan(step, jnp.zeros((b, h, d, d), kv.dtype), jnp.moveaxis(kv, 2, 0))
    prev = jnp.moveaxis(prev, 0, 2)
    w_cross = jnp.exp(lg * (idx + 1.0))
    cross = jnp.einsum('bhncd,bhnde->bhnce', qc, prev) * w_cross[None, :, None, :, None]
    return (inner + cross).reshape(b, h, s, d)


def moba_attention(q, k, v):
    b, h, s, d = q.shape
    bs = MOBA_BLOCK
    nb = s // bs
    qb = MOBA_QBLOCK
    nq = s // qb
    topk = min(MOBA_TOPK, nb)
    scale = d ** -0.5
    kb = k.reshape(b, h, nb, bs, d)
    vb = v.reshape(b, h, nb, bs, d)
    kmean = jnp.mean(kb.astype(jnp.float32), axis=3)
    b_i = jnp.arange(b)[:, None, None]
    h_i = jnp.arange(h)[None, :, None]
    klocal = jnp.arange(bs)

    def one_qblock(args):
        qblk, qi = args
        qpos = qi * qb + jnp.arange(qb)
        own = (qi * qb) // bs
        gate = jnp.einsum('bhqd,bhnd->bhqn', qblk.astype(jnp.float32), kmean)
        gate = jnp.where(jnp.arange(nb) < own, gate, -jnp.inf)
        _, sel = lax.top_k(gate, topk)
        valid = sel < own
        k_own = lax.dynamic_index_in_dim(kb, own, axis=2, keepdims=False)
        v_own = lax.dynamic_index_in_dim(vb, own, axis=2, keepdims=False)
        s_own = jnp.einsum('bhqd,bhkd->bhqk', qblk, k_own).astype(jnp.float32) * scale
        causal = (own * bs + klocal)[None, :] <= qpos[:, None]
        s_list = [jnp.where(causal, s_own, -jnp.inf)]
        for j in range(topk):
            k_j = kb[b_i, h_i, sel[..., j]]
            s_j = jnp.einsum('bhqd,bhqkd->bhqk', qblk, k_j).astype(jnp.float32) * scale
            s_list.append(jnp.where(valid[..., j, None], s_j, -jnp.inf))
        p = jax.nn.softmax(jnp.concatenate(s_list, -1), axis=-1).astype(v.dtype)
        out = jnp.einsum('bhqk,bhkd->bhqd', p[..., :bs], v_own)
        for j in range(topk):
            v_j = vb[b_i, h_i, sel[..., j]]
            out = out + jnp.einsum('bhqk,bhqkd->bhqd', p[..., (j + 1) * bs:(j + 2) * bs], v_j)
        return out

    qs = jnp.moveaxis(q.reshape(b, h, nq, qb, d), 2, 0)
    outs = lax.map(one_qblock, (qs, jnp.arange(nq)))
    return jnp.moveaxis(outs, 0, 2).reshape(b, h, s, d)


def conv_module(a, gate, w, bias, ln_g, ln_b):
    bsz, s, c = a.shape
    u = a * jax.nn.sigmoid(gate)
    y = lax.conv_general_dilated(u, w[:, None, :], window_strides=(1,), padding=[(CONV_KERNEL - 1, 0)],
                                 dimension_numbers=('NWC', 'WIO', 'NWC'), feature_group_count=c) + bias
    yg = y.reshape(bsz, s, CONV_GROUPS, c // CONV_GROUPS).astype(jnp.float32)
    mu = jnp.mean(yg, -1, keepdims=True)
    var = jnp.mean(jnp.square(yg - mu), -1, keepdims=True)
    yn = ((yg - mu) * lax.rsqrt(var + EPS)).reshape(bsz, s, c) * ln_g.astype(jnp.float32) + ln_b.astype(jnp.float32)
    return jax.nn.silu(yn).astype(a.dtype)


def setup_inputs(seed: int = 0) -> dict:
    key = jax.random.key(seed)
    ks = jax.random.split(key, 20)
    f32 = jnp.float32

    def w(k, shape, fan_in):
        return jax.random.normal(k, shape, f32) * (fan_in ** -0.5)

    def gain(k, shape):
        return 1.0 + 0.02 * jax.random.normal(k, shape, f32)

    return {
        "x": jax.random.normal(ks[0], (BATCH, SEQ, D_MODEL), f32),
        "positions": jnp.broadcast_to(jnp.arange(SEQ, dtype=jnp.int32), (BATCH, SEQ)),
        "ffn1_norm": gain(ks[1], (DEPTH, D_MODEL)),
        "ffn1_wg": w(ks[2], (DEPTH, D_MODEL, D_FF), D_MODEL),
        "ffn1_wu": w(ks[3], (DEPTH, D_MODEL, D_FF), D_MODEL),
        "ffn1_wd": w(ks[4], (DEPTH, D_FF, D_MODEL), D_FF),
        "mix_norm": gain(ks[5], (DEPTH, D_MODEL)),
        "w_in": w(ks[6], (DEPTH, D_MODEL, IN_WIDTH), D_MODEL),
        "conv_w": w(ks[7], (DEPTH, CONV_KERNEL, CONV_WIDTH), CONV_KERNEL),
        "conv_b": 0.02 * jax.random.normal(ks[8], (DEPTH, CONV_WIDTH), f32),
        "conv_ln_g": gain(ks[9], (DEPTH, CONV_WIDTH)),
        "conv_ln_b": 0.02 * jax.random.normal(ks[10], (DEPTH, CONV_WIDTH), f32),
        "w_out": w(ks[11], (DEPTH, MIX_WIDTH, D_MODEL), MIX_WIDTH),
        "ffn2_norm": gain(ks[12], (DEPTH, D_MODEL)),
        "ffn2_wg": w(ks[13], (DEPTH, D_MODEL, D_FF), D_MODEL),
        "ffn2_wu": w(ks[14], (DEPTH, D_MODEL, D_FF), D_MODEL),
        "ffn2_wd": w(ks[15], (DEPTH, D_FF, D_MODEL), D_FF),
        "final_norm": gain(ks[16], (D_MODEL,)),
    }


def reference(x, positions, ffn1_norm, ffn1_wg, ffn1_wu, ffn1_wd, mix_norm, w_in, conv_w, conv_b,
              conv_ln_g, conv_ln_b, w_out, ffn2_norm, ffn2_wg, ffn2_wu, ffn2_wd, final_norm):
    s = x.shape[1]
    s_pad = ((s + PAD_MULT - 1) // PAD_MULT) * PAD_MULT
    pad = s_pad - s
    pos = jnp.pad(positions, ((0, 0), (0, pad)))
    half_ret = HEAD_DIM // 2
    ret_inv = RET_ROT_BASE ** (-jnp.linspace(0.0, 1.0, half_ret, dtype=jnp.float32))
    half_rope = ROPE_DIMS // 2
    rope_inv = ROPE_THETA ** (-jnp.arange(half_rope, dtype=jnp.float32) / half_rope)
    sizes = [RET_WIDTH] * 4 + [MOBA_WIDTH] * 3 + [CONV_WIDTH] * 2
    cuts = [int(c) for c in np.cumsum(sizes)[:-1]]

    for l in range(DEPTH):
        h = rms_norm(x, ffn1_norm[l])
        x = x + FFN_RES_WEIGHT * swiglu(h, ffn1_wg[l], ffn1_wu[l], ffn1_wd[l])

        h = rms_norm(x, mix_norm[l])
        z = jnp.pad(h @ w_in[l], ((0, 0), (0, pad), (0, 0)))
        rq, rk, rv, rg, mq, mk, mv, ca, cg = jnp.split(z, cuts, axis=-1)

        rq = rotary(to_heads(rq, RET_HEADS), pos, ret_inv)
        rk = rotary(to_heads(rk, RET_HEADS), pos, ret_inv)
        y_ret = retention(rq, rk, to_heads(rv, RET_HEADS)).astype(jnp.float32)
        y_ret = y_ret * lax.rsqrt(jnp.mean(y_ret * y_ret, -1, keepdims=True) + EPS)
        y_ret = jax.nn.silu(rg) * from_heads(y_ret).astype(rg.dtype)

        mq = rotary(to_heads(mq, MOBA_HEADS), pos, rope_inv)
        mk = rotary(to_heads(mk, MOBA_HEADS), pos, rope_inv)
        y_moba = from_heads(moba_attention(mq, mk, to_heads(mv, MOBA_HEADS)))

        y_conv = conv_module(ca, cg, conv_w[l], conv_b[l], conv_ln_g[l], conv_ln_b[l])

        mix = jnp.concatenate([y_ret, y_moba, y_conv], axis=-1)[:, :s]
        x = x + mix @ w_out[l]

        h = rms_norm(x, ffn2_norm[l])
        x = x + FFN_RES_WEIGHT * swiglu(h, ffn2_wg[l], ffn2_wu[l], ffn2_wd[l])

    return rms_norm(x, final_norm)
```

```python
import math
from contextlib import ExitStack

import numpy as np
import concourse.bass as bass
import concourse.mybir as mybir
from concourse.bass_utils import run_bass_kernel_spmd

F32 = mybir.dt.float32
BF16 = mybir.dt.bfloat16
I32 = mybir.dt.int32
ALU = mybir.AluOpType
AF = mybir.ActivationFunctionType

D_MODEL = 1024
SEQ = 8192
DEPTH = 4
D_FF = 2816
IN_W = 3200
NCORES = 8
TOK = 2048
NT = 16
NEG = -30000.0
EPS = 1e-6

ENGS = ("pe", "act", "dve", "pool", "sp")
NDSEM = 8


class Sched:
    def __init__(self, nc):
        self.nc = nc
        self.q = {e: [] for e in ENGS}
        self.sig = {e: 0 for e in ENGS}
        self.dcnt = {e: 0 for e in ENGS}
        self.ccnt = 0
        self.seen = {e: {} for e in ENGS}
        self.issued = {}
        self.lastw = {}
        self.readers = {}

    def _deps(self, eng, reads, writes):
        toks = []
        for b in reads:
            t = self.lastw.get(b)
            if t is not None:
                toks.append(t)
        for b in writes:
            t = self.lastw.get(b)
            if t is not None:
                toks.append(t)
            toks.extend(self.readers.get(b, ()))
        need = {}
        for (sk, v, e) in toks:
            if e == eng and sk[0] == "c":
                if eng == "pe" or v > self.sig[eng]:
                    continue
            if self.seen[eng].get(sk, 0) >= v:
                continue
            if need.get(sk, 0) < v:
                need[sk] = v
        for sk, v in need.items():
            self.seen[eng][sk] = v
        return list(need.items())

    def _commit(self, tok, reads, writes):
        for b in reads:
            self.readers.setdefault(b, []).append(tok)
        for b in writes:
            self.lastw[b] = tok
            self.readers[b] = []

    def op(self, eng, fn, reads=(), writes=(), signal=True):
        waits = self._deps(eng, reads, writes)
        if signal:
            self.sig[eng] += 1
            val = self.sig[eng]
            self.issued[("c", eng)] = val
        else:
            val = self.sig[eng] + 1
        tok = (("c", eng), val, eng)
        self.q[eng].append(("op", waits, fn, signal, None))
        self._commit(tok, reads, writes)
        return tok

    def dma(self, eng, fn, reads=(), writes=()):
        waits = self._deps(eng, reads, writes)
        i = self.dcnt[eng]
        self.dcnt[eng] += 1
        sk = ("d", eng, i % NDSEM)
        val = 16 * (i // NDSEM + 1)
        if val > 16 and self.seen[eng].get(sk, 0) < val - 16:
            waits.append((sk, val - 16))
            self.seen[eng][sk] = val - 16
        tok = (sk, val, eng)
        self.issued[sk] = val
        self.q[eng].append(("dma", waits, fn, True, sk))
        self._commit(tok, reads, writes)
        return tok

    def cc(self, fn, reads=(), writes=()):
        eng = "pool"
        waits = self._deps(eng, reads, writes)
        self.ccnt += 1
        sk = ("x", "cc")
        if self.ccnt > 1 and self.seen[eng].get(sk, 0) < self.ccnt - 1:
            waits.append((sk, self.ccnt - 1))
            self.seen[eng][sk] = self.ccnt - 1
        tok = (sk, self.ccnt, eng)
        self.issued[sk] = self.ccnt
        self.q[eng].append(("cc", waits, fn, True, sk))
        self._commit(tok, reads, writes)
        return tok

    def barrier(self, engines=ENGS):
        for e in engines:
            waits = []
            for sk, v in self.issued.items():
                if sk == ("c", e):
                    continue
                if self.seen[e].get(sk, 0) < v:
                    waits.append((sk, v))
                    self.seen[e][sk] = v
            self.q[e].append(("wait", waits, None, False, None))

    def wait_all(self, eng, bufs):
        waits = self._deps(eng, bufs, ())
        self.q[eng].append(("wait", waits, None, False, None))

    def emit(self):
        nc = self.nc
        used = []
        seen = set()
        for e in ENGS:
            for (kind, waits, fn, signal, sk) in self.q[e]:
                keys = [wk for (wk, v) in waits]
                if kind in ("dma", "cc"):
                    keys.append(sk)
                elif kind == "op" and signal:
                    keys.append(("c", e))
                for k in keys:
                    if k not in seen:
                        seen.add(k)
                        used.append(k)
        with ExitStack() as st:
            sems = {}
            for s in used:
                sems[s] = st.enter_context(nc.semaphore("s_" + "_".join(str(x) for x in s)))
            block = st.enter_context(nc.Block())

            def runner(e):
                def run(engobj):
                    for (kind, waits, fn, signal, sk) in self.q[e]:
                        for (wk, v) in waits:
                            engobj.wait_ge(sems[wk], v)
                        if kind == "op":
                            ins = fn(engobj)
                            if signal:
                                ins.then_inc(sems[("c", e)], 1)
                        elif kind == "dma":
                            fn(engobj).then_inc(sems[sk], 16)
                        elif kind == "cc":
                            fn(engobj).then_inc(sems[sk], 1)
                return run

            block.tensor(runner("pe"))
            block.scalar(runner("act"))
            block.vector(runner("dve"))
            block.gpsimd(runner("pool"))
            block.sync(runner("sp"))


CF = {}
_off = 0
for _name, _w in [("identf", 128), ("bd", 128), ("onesf", 128), ("invr", 32), ("invm", 8),
                  ("qs", 12), ("ks", 12), ("dec", 3), ("coef", 12), ("cap", 256),
                  ("pastneg", 256), ("futneg", 256), ("sel", 4), ("c256", 1), ("zero", 1),
                  ("mask01", 384), ("identR", 512)]:
    CF[_name] = (_off, _w)
    _off += _w
NCF = _off
NCS = CF["mask01"][0]


def _host_consts(rank):
    cf = np.zeros((128, NCF), np.float32)

    def put(name, arr):
        o, w = CF[name]
        cf[:, o:o + w] = np.asarray(arr, np.float32).reshape(128, w)

    p = np.arange(128)
    put("identf", np.eye(128))
    bd = np.zeros((128, 128))
    bd[:64, :64] = 1.0 / 64
    bd[64:, 64:] = 1.0 / 64
    put("bd", bd)
    put("onesf", np.ones((128, 128)))
    ret_inv = (10000.0 ** (-np.linspace(0.0, 1.0, 32, dtype=np.float32))).astype(np.float32)
    rope_inv = (500000.0 ** (-np.arange(8, dtype=np.float32) / 8)).astype(np.float32)
    put("invr", np.broadcast_to(ret_inv, (128, 32)))
    put("invm", np.broadcast_to(rope_inv, (128, 8)))
    hh = np.arange(6, dtype=np.float64)
    lg = np.log1p(-np.exp2(-5.0 - hh))
    qs = np.zeros((128, 2, 6))
    ks = np.zeros((128, 2, 6))
    for par in range(2):
        c = par * 128 + p
        qs[:, par] = np.exp(lg[None, :] * (c[:, None] + 1.0))
        ks[:, par] = np.exp(-lg[None, :] * (c[:, None] + 1.0)) * 0.125
    put("qs", qs)
    put("ks", ks)
    hd = np.zeros((128, 3), np.int64)
    for pr in range(3):
        hd[:64, pr] = 2 * pr
        hd[64:, pr] = 2 * pr + 1
    dec = np.exp(lg[hd] * 256.0)
    put("dec", dec)
    coef = np.zeros((128, 4, 3))
    for i in range(4):
        if i < rank:
            coef[:, i] = np.exp(lg[hd] * 2048.0 * (rank - 1 - i))
    put("coef", coef)
    cap = np.zeros((128, 8, 32))
    pastneg = np.zeros((128, 8, 32))
    futneg = np.zeros((128, 8, 32))
    n = np.arange(32)
    for b in range(8):
        own = 8 * rank + b
        cap[:, b] = np.where(n < own, 3.0e38, -1.0e9)[None, :]
        pastneg[:, b] = np.where(n < own, NEG, 0.0)[None, :]
        futneg[:, b] = np.where(n > own, NEG, 0.0)[None, :]
    put("cap", cap)
    put("pastneg", pastneg)
    put("futneg", futneg)
    sel = np.zeros((128, 4))
    if rank > 0:
        sel[:, rank - 1] = 1.0
    put("sel", sel)
    tri = (p[:, None] <= p[None, :]).astype(np.float32)
    put("mask01", np.concatenate([tri, np.ones((128, 128)), tri], 1))
    idr = np.zeros((128, 4, 128))
    idr[:, rank] = np.eye(128)
    put("identR", idr)
    put("c256", np.full((128, 1), 1.0 / 256))
    cm = np.zeros((128, 4, 512), np.float32)
    q = np.arange(512)
    for t in range(4):
        cm[:, t] = np.where((t * 128 + p)[:, None] > q[None, :], NEG, 0.0)
    er = np.zeros((4, 32, 2048), np.float32)
    key = np.arange(2048)
    for i in range(4):
        er[i, 8 * i + key // 256, key] = 1.0
    return cf, cm.reshape(128, 2048), er


ARENA = 176 * 1024
A_X = 0
A_HT = 65536
A_ACT = 98304
A_WD = 131072
A_WGU = 147456
A_GAIN = 159744
A_TMP = 163840
A_QRT = 0
A_KRT = 12288
A_VTOK = 24576
A_RG = 36864
A_MQT = 49152
A_MKT = 98304
A_KVS = 110592
A_UT = 116736
A_WIN = 125056
A_CW = 137344
A_WOUT = 145536
A_QAUG = 0
A_KT = 24576
A_CONVTMP = 0
A_VR = 125056


class Prog:
    def __init__(self, depth, do_final, taps=(), stop=None):
        self.stop = stop
        self.D = depth
        self.do_final = do_final
        self.taps = taps
        self.nc = bass.Bass("TRN2", target_bir_lowering=False)
        self.S = Sched(self.nc)
        self.tapouts = {}

    def carve(self, off, shape, dt):
        n = int(np.prod(shape[1:]))
        sz = 2 if dt == BF16 else 4
        assert off % 4 == 0 and off + n * sz <= ARENA, (off, shape)
        v = self.arena[:, off // 2: off // 2 + n * sz // 2]
        if dt != BF16:
            v = v.bitcast(dt)
        if len(shape) == 3:
            v = v.rearrange("p (a b) -> p a b", a=shape[1])
        elif len(shape) == 4:
            v = v.rearrange("p (a b c) -> p a b c", a=shape[1], b=shape[2])
        if shape[0] != 128:
            v = v[0:shape[0]]
        return v

    def cfv(self, name):
        o, w = CF[name]
        return self.cf[:, o:o + w]

    def build(self):
        nc, S, D = self.nc, self.S, self.D
        dram = lambda name, shape, dt, kind: nc.dram_tensor(name, shape, dt, kind=kind).ap()
        self.x_in = dram("x", [TOK, D_MODEL], F32, "ExternalInput")
        self.pos_in = dram("pos", [128, NT], I32, "ExternalInput")
        self.w = {}
        for f in (1, 2):
            self.w[f"wg{f}"] = dram(f"wg{f}", [D, D_MODEL, D_FF], F32, "ExternalInput")
            self.w[f"wu{f}"] = dram(f"wu{f}", [D, D_MODEL, D_FF], F32, "ExternalInput")
            self.w[f"wd{f}"] = dram(f"wd{f}", [D, D_FF, D_MODEL], F32, "ExternalInput")
        self.w["w_in"] = dram("w_in", [D, D_MODEL, IN_W], F32, "ExternalInput")
        self.w["w_out"] = dram("w_out", [D, D_MODEL, D_MODEL], F32, "ExternalInput")
        self.norms = dram("norms", [D, 3, D_MODEL], F32, "ExternalInput")
        self.fnorm = dram("fnorm", [1, D_MODEL], F32, "ExternalInput")
        self.convp = dram("convp", [D, 128, 2, 34], F32, "ExternalInput")
        self.cf_in = dram("cf", [128, NCF], F32, "ExternalInput")
        self.cm_in = dram("cmask", [128, 2048], F32, "ExternalInput")
        self.er_in = dram("erows", [4, 32, 2048], F32, "ExternalInput")
        self.y_out = dram("y", [TOK, D_MODEL], F32, "ExternalOutput")
        self.xsp = nc.dram_tensor("xsp", [128, NT * D_MODEL], F32).ap()
        self.ccK_in = [nc.dram_tensor(f"ccK_in{j}", [128, 2048], BF16).ap() for j in range(3)]
        self.ccK_out = [nc.dram_tensor(f"ccK_out{j}", [512, 2048], BF16).ap() for j in range(3)]
        self.ccV_in = [nc.dram_tensor(f"ccV_in{j}", [128, 2048], BF16).ap() for j in range(3)]
        self.ccV_out = [nc.dram_tensor(f"ccV_out{j}", [512, 2048], BF16).ap() for j in range(3)]
        self.ccB_in = nc.dram_tensor("ccB_in", [128, 276], F32).ap()
        self.ccB_out = nc.dram_tensor("ccB_out", [512, 276], F32).ap()
        for t in self.taps:
            self.tapouts[t] = dram("tap_" + t, [128, NT * D_MODEL], F32, "ExternalOutput")

        with ExitStack() as st:
            sb = lambda name, shape, dt: st.enter_context(nc.sbuf_tensor(name, shape, dt))
            self.arena = sb("arena", [128, ARENA // 2], BF16)
            self.cf = sb("cf_sb", [128, NCS], F32)
            self.identb = sb("identb", [128, 128], BF16)
            self.mask01 = sb("mask01", [128, 384], BF16)
            self.identR = sb("identR", [128, 4, 128], BF16)
            self.cmask = sb("cmask_sb", [128, 4, 512], BF16)
            self.c256b = sb("c256b", [128, 1], BF16)
            self.cosr = sb("cosr", [128, NT, 32], F32)
            self.sinr = sb("sinr", [128, NT, 32], F32)
            self.cosm = sb("cosm", [128, NT, 8], F32)
            self.sinm = sb("sinm", [128, NT, 8], F32)
            self.posi = sb("posi", [128, NT], I32)
            self.posf = sb("posf", [128, NT], F32)
            self.ms = sb("ms", [128, NT], F32)
            self.rstd = sb("rstd", [128, NT], F32)
            self.cpar = sb("cpar", [128, 2, 34], F32)
            self.state = sb("state", [128, 3, 64], F32)
            self.stbf = sb("stbf", [128, 3, 64], BF16)
            self.kmsb = sb("kmsb", [128, 24], F32)
            self.kmall = sb("kmall", [64, 6, 32], F32)
            self.kmhi = sb("kmhi", [64, 6, 32], BF16)
            self.kmlo = sb("kmlo", [64, 6, 32], BF16)
            self.kmt = sb("kmt", [64, 6, 32], F32)
            self.send = sb("send", [128, 4, 192], F32)
            self.tails = sb("tails", [128, 4, 60], F32)
            self.tail_f = sb("tail_f", [128, 2, 30], F32)
            self.halo = sb("halo", [128, 60], F32)
            self.mtpad = [sb(f"mtpad{i}", [128, 96], BF16) for i in range(2)]
            self.g1 = [sb(f"g1_{i}", [128, 32], F32) for i in range(2)]
            self.m8 = [sb(f"m8_{i}", [128, 8], F32) for i in range(2)]
            self.t1 = [sb(f"t1_{i}", [128, 32], F32) for i in range(2)]
            self.ssq = sb("ssq", [128, 6], F32)
            self.psf = [st.enter_context(nc.psum_tensor(f"psf{i}", [128, 512], F32)) for i in range(6)]
            self.psb = [st.enter_context(nc.psum_tensor(f"psb{i}", [128, 1024], BF16)) for i in range(2)]
            self.tcount = 0
            self.body()
            S.emit()
        return nc

    def cp_eng(self):
        self.tcount += 1
        return "act" if self.tcount % 2 else "dve"

    def copy(self, eng, out, in_, reads, writes):
        if eng == "act":
            self.S.op("act", lambda e: e.activation(out=out, in_=in_, func=AF.Copy), reads=reads, writes=writes)
        else:
            self.S.op(eng, lambda e: e.tensor_copy(out=out, in_=in_), reads=reads, writes=writes)

    def body(self):
        S, nc = self.S, self.nc
        self.setup()
        self.load_x()
        for l in range(self.D):
            self.ffn(l, 1)
            if "x_ffn1" in self.taps and l == 0:
                self.tap_x("x_ffn1")
            if self.stop == "ffn1":
                break
            self.mixer(l)
            if self.stop is not None:
                S.barrier()
                S.dma("sp", lambda e: e.dma_start(out=self.carve(A_X, [128, NT, D_MODEL], F32), in_=self.xsp.rearrange("p (t f) -> p t f", t=NT)),
                      reads=["xsp"], writes=[f"x{t}" for t in range(NT)])
                break
            if "x_mix" in self.taps and l == 0:
                self.tap_x("x_mix")
            self.ffn(l, 2)
        if self.do_final:
            self.final()
        else:
            self.store_x()
        S.barrier(("sp",))

    def tap_x(self, name):
        S = self.S
        X = self.carve(A_X, [128, NT, D_MODEL], F32)
        S.dma("sp", lambda e: e.dma_start(out=self.tapouts[name].rearrange("p (t f) -> p t f", t=NT), in_=X),
              reads=[f"x{t}" for t in range(NT)], writes=["tap_" + name])

    def tap_buf(self, name, view, reads, width):
        S = self.S
        S.dma("sp", lambda e: e.dma_start(out=self.tapouts[name][:, 0:width], in_=view), reads=reads, writes=["tap_" + name])

    def setup(self):
        S = self.S
        S.dma("sp", lambda e: e.dma_start(out=self.cf[:], in_=self.cf_in[:, 0:NCS]), writes=["cf"])
        S.dma("sp", lambda e: e.dma_start(out=self.posi[:], in_=self.pos_in), writes=["posi"])
        o, w = CF["identf"]
        S.dma("pool", lambda e: e.dma_start(out=self.identb[:], in_=self.cf_in[:, o:o + w]), writes=["identb"])
        o2, w2 = CF["mask01"]
        S.dma("pool", lambda e: e.dma_start(out=self.mask01[:], in_=self.cf_in[:, o2:o2 + w2]), writes=["mask01"])
        o3, w3 = CF["identR"]
        S.dma("pool", lambda e: e.dma_start(out=self.identR[:], in_=self.cf_in[:, o3:o3 + w3].rearrange("p (a b) -> p a b", a=4)),
              writes=["identR"])
        S.dma("pool", lambda e: e.dma_start(out=self.cmask[:], in_=self.cm_in.rearrange("p (a b) -> p a b", a=4)), writes=["cmask"])
        S.op("dve", lambda e: e.memset(self.c256b[:], 1.0 / 256), writes=["c256b"])
        self.KT = [self.carve(A_KT + i * 4096, [128, 2048], BF16) for i in range(4)]
        self.VR = [self.carve(A_VR + i * 2112, [128, 16, 66], BF16) for i in range(4)]
        for i in range(2):
            S.op("pool", lambda e, i=i: e.memset(self.mtpad[i][:], 0.0), writes=[f"mtpad{i}"])
        S.op("dve", lambda e: e.tensor_copy(out=self.posf[:], in_=self.posi[:]), reads=["posi"], writes=["posf"])
        tmp = self.carve(A_TMP, [128, NT, 32], F32)
        tmp2 = self.carve(A_TMP + 2048, [128, NT, 32], F32)
        tmpi = self.carve(A_TMP + 4096, [128, NT, 32], I32)
        tmp3 = self.carve(A_TMP + 6144, [128, NT, 32], F32)
        C1 = 6.28125
        C2 = 2 * math.pi - C1
        for (nf, inv, cosT, sinT) in ((32, "invr", self.cosr, self.sinr), (8, "invm", self.cosm, self.sinm)):
            ang, kf, ki, r2 = tmp[:, :, 0:nf], tmp2[:, :, 0:nf], tmpi[:, :, 0:nf], tmp3[:, :, 0:nf]
            invv = self.cfv(inv)
            for t in range(NT):
                S.op("dve", lambda e, t=t, ang=ang, invv=invv: e.tensor_scalar(
                    out=ang[:, t, :], in0=invv, scalar1=self.posf[:, t:t + 1], scalar2=None, op0=ALU.mult),
                    reads=["posf", "cf"], writes=["rt_ang"], signal=(t == NT - 1))
            S.op("dve", lambda e, ang=ang, kf=kf: e.tensor_scalar(out=kf, in0=ang, scalar1=1.0 / (2 * math.pi), scalar2=None, op0=ALU.mult),
                 reads=["rt_ang"], writes=["rt_kf"])
            S.op("dve", lambda e, ki=ki, kf=kf: e.tensor_copy(out=ki, in_=kf), reads=["rt_kf"], writes=["rt_ki"])
            S.op("dve", lambda e, ki=ki, kf=kf: e.tensor_copy(out=kf, in_=ki), reads=["rt_ki"], writes=["rt_kf"])
            S.op("dve", lambda e, ang=ang, kf=kf: e.scalar_tensor_tensor(out=ang, in0=kf, scalar=-C1, in1=ang, op0=ALU.mult, op1=ALU.add),
                 reads=["rt_kf", "rt_ang"], writes=["rt_ang"])
            S.op("dve", lambda e, ang=ang, kf=kf: e.scalar_tensor_tensor(out=ang, in0=kf, scalar=-C2, in1=ang, op0=ALU.mult, op1=ALU.add),
                 reads=["rt_kf", "rt_ang"], writes=["rt_ang"])
            S.op("dve", lambda e, ang=ang: e.tensor_scalar(out=ang, in0=ang, scalar1=math.pi, scalar2=-math.pi, op0=ALU.min, op1=ALU.max),
                 reads=["rt_ang"], writes=["rt_ang"])
            S.op("act", lambda e, ang=ang, sinT=sinT: e.activation(out=sinT[:], in_=ang, func=AF.Sin), reads=["rt_ang"], writes=["rt_sin"])
            S.op("dve", lambda e, ang=ang, kf=kf: e.tensor_scalar(out=kf, in0=ang, scalar1=math.pi / 2, scalar2=math.pi, op0=ALU.add, op1=ALU.is_gt),
                 reads=["rt_ang"], writes=["rt_kf"])
            S.op("dve", lambda e, ang=ang, kf=kf, r2=r2: e.scalar_tensor_tensor(out=r2, in0=kf, scalar=-2 * math.pi, in1=ang, op0=ALU.mult, op1=ALU.add),
                 reads=["rt_kf", "rt_ang"], writes=["rt_r2"])
            S.op("dve", lambda e, r2=r2: e.tensor_scalar(out=r2, in0=r2, scalar1=math.pi / 2, scalar2=math.pi, op0=ALU.add, op1=ALU.min),
                 reads=["rt_r2"], writes=["rt_r2"])
            S.op("act", lambda e, r2=r2, cosT=cosT: e.activation(out=cosT[:], in_=r2, func=AF.Sin), reads=["rt_r2"], writes=["rt_cos"])
        S.barrier()

    def load_x(self):
        S = self.S
        X = self.carve(A_X, [128, NT, D_MODEL], F32)
        xin = self.x_in.rearrange("(t p) f -> p t f", p=128)
        for q in range(4):
            S.dma("sp", lambda e, q=q: e.dma_start(out=X[:, 4 * q:4 * q + 4, :], in_=xin[:, 4 * q:4 * q + 4, :]),
                  writes=[f"x{t}" for t in range(4 * q, 4 * q + 4)])

    def store_x(self):
        S = self.S
        X = self.carve(A_X, [128, NT, D_MODEL], F32)
        yo = self.y_out.rearrange("(t p) f -> p t f", p=128)
        for q in range(4):
            S.dma("sp", lambda e, q=q: e.dma_start(out=yo[:, 4 * q:4 * q + 4, :], in_=X[:, 4 * q:4 * q + 4, :]),
                  reads=[f"x{t}" for t in range(4 * q, 4 * q + 4)], writes=[f"y{q}"])

    def rms_stats(self):
        S = self.S
        X = self.carve(A_X, [128, NT, D_MODEL], F32)
        junk = self.carve(A_TMP + 8192, [128, D_MODEL], BF16)
        for t in range(NT):
            S.op("act", lambda e, t=t: e.activation(out=junk, in_=X[:, t, :], func=AF.Square, accum_out=self.ms[:, t:t + 1]),
                 reads=[f"x{t}"], writes=["junk", "ms"])
        S.op("dve", lambda e: e.tensor_scalar(out=self.rstd[:], in0=self.ms[:], scalar1=1.0 / D_MODEL, scalar2=EPS, op0=ALU.mult, op1=ALU.add),
             reads=["ms"], writes=["rstd"])
        S.op("act", lambda e: e.activation(out=self.rstd[:], in_=self.rstd[:], func=AF.Sqrt), reads=["rstd"], writes=["rstd"])
        S.op("dve", lambda e: e.reciprocal(out=self.rstd[:], in_=self.rstd[:]), reads=["rstd"], writes=["rstd"])

    def norm_to_hT(self, gain_ap):
        S = self.S
        X = self.carve(A_X, [128, NT, D_MODEL], F32)
        HT = self.carve(A_HT, [128, 8, TOK], BF16)
        gain = self.carve(A_GAIN, [128, D_MODEL], F32)
        S.dma("sp", lambda e: e.dma_start(out=gain, in_=gain_ap.to_broadcast([128, D_MODEL])), writes=["gain"])
        self.rms_stats()
        hn = [self.carve(A_TMP + 2048 * i, [128, D_MODEL], BF16) for i in range(2)]
        for t in range(NT):
            h = hn[t % 2]
            S.op("dve", lambda e, t=t, h=h: e.scalar_tensor_tensor(out=h, in0=X[:, t, :], scalar=self.rstd[:, t:t + 1], in1=gain,
                                                                  op0=ALU.mult, op1=ALU.mult),
                 reads=[f"x{t}", "rstd", "gain"], writes=[f"hn{t % 2}"])
            pb = self.psb[t % 2]
            for kc in range(8):
                S.op("pe", lambda e, kc=kc, h=h, pb=pb: e.transpose(out=pb[:, kc * 128:(kc + 1) * 128], in_=h[:, kc * 128:(kc + 1) * 128],
                                                                   identity=self.identb[:]),
                     reads=[f"hn{t % 2}", "identb"], writes=[f"psb{t % 2}"], signal=(kc == 7))
            self.copy(self.cp_eng(), HT[:, :, t * 128:(t + 1) * 128], pb.rearrange("p (a b) -> p a b", a=8),
                      reads=[f"psb{t % 2}"], writes=[f"hT{t}"])

    def ffn(self, l, f):
        S = self.S
        wg, wu, wd = self.w[f"wg{f}"], self.w[f"wu{f}"], self.w[f"wd{f}"]
        S.barrier()
        self.norm_to_hT(self.norms[l, (0 if f == 1 else 2):(1 if f == 1 else 3), :])
        X = self.carve(A_X, [128, NT, D_MODEL], F32)
        HT = self.carve(A_HT, [128, 8, TOK], BF16)
        ACT = self.carve(A_ACT, [128, 8, TOK], BF16)
        WD = self.carve(A_WD, [128, 8, D_MODEL], BF16)
        WGU = [self.carve(A_WGU + 4096 * i, [128, 2, 8, 128], BF16) for i in range(3)]
        sg = [self.carve(A_TMP + 4096 + 2048 * i, [128, 512], F32) for i in range(2)]
        wgv = wg[l].rearrange("(k p) f -> p k f", p=128)
        wuv = wu[l].rearrange("(k p) f -> p k f", p=128)
        wdv = wd[l].rearrange("(c p) f -> p c f", p=128)
        cnt = 0
        for (c0, c1) in ((0, 8), (8, 16), (16, 22)):
            ncp = c1 - c0
            S.dma("pool", lambda e, c0=c0, c1=c1, ncp=ncp: e.dma_start(out=WD[:, 0:ncp, :], in_=wdv[:, c0:c1, :]), writes=["wd"])
            for c in range(c0, c1):
                slot = c % 3
                W = WGU[slot]
                S.dma("pool", lambda e, c=c, W=W: e.dma_start(out=W[:, 0, :, :], in_=wgv[:, :, c * 128:(c + 1) * 128]), writes=[f"wgu{slot}g"])
                S.dma("pool", lambda e, c=c, W=W: e.dma_start(out=W[:, 1, :, :], in_=wuv[:, :, c * 128:(c + 1) * 128]), writes=[f"wgu{slot}u"])
                for tg in range(4):
                    pg, pu = self.psf[cnt % 2], self.psf[2 + cnt % 2]
                    kg, ku = f"psf{cnt % 2}", f"psf{2 + cnt % 2}"
                    hk = [f"hT{t}" for t in range(4 * tg, 4 * tg + 4)]
                    for kc in range(8):
                        S.op("pe", lambda e, kc=kc, W=W, pg=pg, tg=tg: e.matmul(pg[:], lhsT=W[:, 0, kc, :], rhs=HT[:, kc, tg * 512:(tg + 1) * 512],
                                                                             start=(kc == 0), stop=(kc == 7)),
                             reads=hk + [f"wgu{slot}g"], writes=[kg], signal=(kc == 7))
                    for kc in range(8):
                        S.op("pe", lambda e, kc=kc, W=W, pu=pu, tg=tg: e.matmul(pu[:], lhsT=W[:, 1, kc, :], rhs=HT[:, kc, tg * 512:(tg + 1) * 512],
                                                                             start=(kc == 0), stop=(kc == 7)),
                             reads=hk + [f"wgu{slot}u"], writes=[ku], signal=(kc == 7))
                    s_ = sg[cnt % 2]
                    S.op("act", lambda e, s_=s_, pg=pg: e.activation(out=s_, in_=pg[:], func=AF.Silu), reads=[kg], writes=[f"sg{cnt % 2}"])
                    S.op("dve", lambda e, s_=s_, pu=pu, c=c, c0=c0, tg=tg: e.tensor_tensor(
                        out=ACT[:, c - c0, tg * 512:(tg + 1) * 512], in0=s_, in1=pu[:], op=ALU.mult),
                        reads=[f"sg{cnt % 2}", ku], writes=[f"act{c - c0}_{tg}"])
                    cnt += 1
            for t in range(NT):
                for hf in range(2):
                    pd = self.psf[4 + (2 * t + hf) % 2]
                    kd = f"psf{4 + (2 * t + hf) % 2}"
                    for cc in range(ncp):
                        S.op("pe", lambda e, cc=cc, t=t, hf=hf, pd=pd, ncp=ncp: e.matmul(pd[:], lhsT=ACT[:, cc, t * 128:(t + 1) * 128],
                                                                             rhs=WD[:, cc, hf * 512:(hf + 1) * 512],
                                                                             start=(cc == 0), stop=(cc == ncp - 1)),
                             reads=[f"act{cc}_{t // 4}", "wd"], writes=[kd], signal=(cc == ncp - 1))
                    S.op("dve", lambda e, t=t, hf=hf, pd=pd: e.scalar_tensor_tensor(
                        out=X[:, t, hf * 512:(hf + 1) * 512], in0=pd[:], scalar=0.5, in1=X[:, t, hf * 512:(hf + 1) * 512],
                        op0=ALU.mult, op1=ALU.add), reads=[kd, f"x{t}"], writes=[f"x{t}"])

    def rotary(self, ps, out_bf, t, half, cosT, sinT, tmp):
        S = self.S
        a, b = tmp[0][:, :, 0:half], tmp[1][:, :, 0:half]
        cb = cosT[:, t:t + 1, :].to_broadcast([128, 6, half])
        sbb = sinT[:, t:t + 1, :].to_broadcast([128, 6, half])
        x1, x2 = ps[:, :, 0:half], ps[:, :, half:2 * half]
        rk, wk = self._rot_keys
        S.op("dve", lambda e: e.tensor_tensor(out=a, in0=x1, in1=cb, op=ALU.mult), reads=rk, writes=["rot_a"])
        S.op("dve", lambda e: e.tensor_tensor(out=b, in0=x2, in1=sbb, op=ALU.mult), reads=rk, writes=["rot_b"])
        S.op("dve", lambda e: e.tensor_tensor(out=out_bf[:, :, 0:half], in0=a, in1=b, op=ALU.subtract), reads=["rot_a", "rot_b"], writes=wk)
        S.op("dve", lambda e: e.tensor_tensor(out=a, in0=x2, in1=cb, op=ALU.mult), reads=rk, writes=["rot_a"])
        S.op("dve", lambda e: e.tensor_tensor(out=b, in0=x1, in1=sbb, op=ALU.mult), reads=rk, writes=["rot_b"])
        S.op("dve", lambda e: e.tensor_tensor(out=out_bf[:, :, half:2 * half], in0=a, in1=b, op=ALU.add), reads=["rot_a", "rot_b"], writes=wk)

    def mixer(self, l):
        S = self.S
        S.barrier()
        self.norm_to_hT(self.norms[l, 1:2, :])
        X = self.carve(A_X, [128, NT, D_MODEL], F32)
        xkeys = [f"x{t}" for t in range(NT)]
        S.dma("sp", lambda e: e.dma_start(out=self.xsp.rearrange("p (t f) -> p t f", t=NT), in_=X), reads=xkeys, writes=["xsp"])
        S.dma("pool", lambda e: e.dma_start(out=self.cpar[:], in_=self.convp[l]), writes=["cpar"])
        S.barrier()
        if self.stop == "spill":
            return
        HT = self.carve(A_HT, [128, 8, TOK], BF16)
        QRT = self.carve(A_QRT, [128, 3, TOK], BF16)
        KRT = self.carve(A_KRT, [128, 3, TOK], BF16)
        VTOK = self.carve(A_VTOK, [128, NT, 384], BF16)
        RG = self.carve(A_RG, [128, NT, 384], BF16)
        MQT = self.carve(A_MQT, [128, 3, TOK], BF16)
        MKT = self.carve(A_MKT, [128, 3, TOK], BF16)
        KVS = self.carve(A_KVS, [128, 8, 3, 64], F32)
        UT = self.carve(A_UT, [128, 2, 2080], BF16)
        WIN = [self.carve(A_WIN + 6144 * i, [128, 8, 384], BF16) for i in range(2)]
        CW = self.carve(A_CW, [128, 4, 8, 128], BF16)
        VIMG = self.carve(A_WOUT, [128, 3, NT, 128], BF16)
        tokb = [self.carve(A_TMP + 768 * i, [128, 6, 64], BF16) for i in range(3)]
        rtmp = [self.carve(A_TMP + 2304 + 768 * i, [128, 6, 32], F32) for i in range(2)]
        winv = self.w["w_in"][l].rearrange("(k p) f -> p k f", p=128)
        hkeys = [f"hT{t}" for t in range(NT)]
        qs = self.cfv("qs").rearrange("p (a b) -> p a b", a=2)
        ks = self.cfv("ks").rearrange("p (a b) -> p a b", a=2)
        bc6 = lambda v: v.unsqueeze(2).to_broadcast([128, 6, 64])
        pcnt = 0
        tb = 0
        groups = [("rv", 768), ("rg", 1152), ("rk", 384), ("rq", 0), ("mv", 2304), ("mk", 1920), ("mq", 1536)]
        for gi, (gname, c0) in enumerate(groups):
            if self.stop is not None and self.stop.startswith("g:") and gi >= int(self.stop[2:]):
                return
            W = WIN[gi % 2]
            S.dma("pool", lambda e, W=W, c0=c0: e.dma_start(out=W, in_=winv[:, :, c0:c0 + 384]), writes=[f"win{gi % 2}"])
            for t in range(NT):
                ps = self.psf[pcnt % 2]
                pk = f"psf{pcnt % 2}"
                pcnt += 1
                for kc in range(8):
                    S.op("pe", lambda e, kc=kc, t=t, ps=ps, W=W: e.matmul(ps[:, 0:384], lhsT=HT[:, kc, t * 128:(t + 1) * 128], rhs=W[:, kc, :],
                                                                         start=(kc == 0), stop=(kc == 7)),
                         reads=[f"hT{t}", f"win{gi % 2}"], writes=[pk], signal=(kc == 7))
                psv = ps[:, 0:384].rearrange("p (h d) -> p h d", h=6)
                if gname == "rv":
                    S.op("act", lambda e, t=t, ps=ps: e.activation(out=VTOK[:, t, :], in_=ps[:, 0:384], func=AF.Copy), reads=[pk], writes=[f"vtok{t}"])
                elif gname == "rg":
                    S.op("act", lambda e, t=t, ps=ps: e.activation(out=RG[:, t, :], in_=ps[:, 0:384], func=AF.Silu), reads=[pk], writes=[f"rg{t}"])
                elif gname in ("rk", "rq"):
                    ob = tokb[tb % 3]
                    obk = f"tokb{tb % 3}"
                    tb += 1
                    self._rot_keys = ([pk, "rt_cos", "rt_sin"], [obk])
                    self.rotary(psv, ob, t, 32, self.cosr, self.sinr, rtmp)
                    sc = bc6((ks if gname == "rk" else qs)[:, t % 2, :])
                    obf = ob.rearrange("p h d -> p (h d)")
                    S.op("dve", lambda e, ob=ob, sc=sc: e.tensor_tensor(out=ob, in0=ob, in1=sc, op=ALU.mult), reads=[obk, "cf"], writes=[obk])
                    pb = self.psb[t % 2]
                    for pr in range(3):
                        S.op("pe", lambda e, pr=pr, pb=pb, obf=obf: e.transpose(out=pb[:, pr * 128:(pr + 1) * 128], in_=obf[:, pr * 128:(pr + 1) * 128],
                                                                               identity=self.identb[:]),
                             reads=[obk, "identb"], writes=[f"psb{t % 2}"], signal=(pr == 2))
                    dst = (KRT if gname == "rk" else QRT)
                    dk = ("krt" if gname == "rk" else "qrt") + str(t)
                    self.copy(self.cp_eng(), dst[:, :, t * 128:(t + 1) * 128], pb[:, 0:384].rearrange("p (a b) -> p a b", a=3),
                              reads=[f"psb{t % 2}"], writes=[dk])
                    if gname == "rk":
                        pkv = self.psf[2]
                        for pr in range(3):
                            S.op("pe", lambda e, pr=pr, t=t, obf=obf, pkv=pkv: e.matmul(
                                pkv[:, pr * 128:(pr + 1) * 128], lhsT=obf[:, pr * 128:(pr + 1) * 128], rhs=VTOK[:, t, pr * 128:(pr + 1) * 128],
                                start=(t % 2 == 0 and pr == 0), stop=(t % 2 == 1 and pr == 2)),
                                reads=[obk, f"vtok{t}"], writes=["psf2"], signal=(pr == 2))
                        if t % 2 == 1:
                            ch = t // 2
                            pv = pkv[:, 0:384].rearrange("p (a b) -> p a b", a=3)
                            S.op("act", lambda e, ch=ch, pv=pv: e.activation(out=KVS[0:64, ch, :, :], in_=pv[0:64, :, 0:64], func=AF.Copy),
                                 reads=["psf2"], writes=[f"kvs{ch}a"])
                            S.op("dve", lambda e, ch=ch, pv=pv: e.tensor_copy(out=KVS[64:128, ch, :, :], in_=pv[64:128, :, 64:128]),
                                 reads=["psf2"], writes=[f"kvs{ch}b"])
                elif gname == "mv":
                    S.op("act", lambda e, t=t, ps=ps: e.activation(out=VIMG[:, :, t, :], in_=ps[:, 0:384].rearrange("p (a b) -> p a b", a=3), func=AF.Copy),
                         reads=[pk], writes=[f"vimg{t}"])
                elif gname in ("mk", "mq"):
                    ob = tokb[tb % 3]
                    obk = f"tokb{tb % 3}"
                    tb += 1
                    obf = ob.rearrange("p h d -> p (h d)")
                    S.op("dve", lambda e, ob=ob, psv=psv: e.tensor_copy(out=ob[:, :, 16:64], in_=psv[:, :, 16:64]), reads=[pk], writes=[obk])
                    self._rot_keys = ([pk, "rt_cos", "rt_sin"], [obk])
                    self.rotary(psv, ob, t, 8, self.cosm, self.sinm, rtmp)
                    pb = self.psb[t % 2]
                    for pr in range(3):
                        S.op("pe", lambda e, pr=pr, pb=pb, obf=obf: e.transpose(out=pb[:, pr * 128:(pr + 1) * 128], in_=obf[:, pr * 128:(pr + 1) * 128],
                                                                               identity=self.identb[:]),
                             reads=[obk, "identb"], writes=[f"psb{t % 2}"], signal=(pr == 2))
                    dst = (MKT if gname == "mk" else MQT)
                    dk = ("mkt" if gname == "mk" else "mqt") + str(t)
                    self.copy(self.cp_eng(), dst[:, :, t * 128:(t + 1) * 128], pb[:, 0:384].rearrange("p (a b) -> p a b", a=3),
                              reads=[f"psb{t % 2}"], writes=[dk])
        if self.stop == "g:7":
            return
        S.op("dve", lambda e: e.tensor_reduce(out=self.kmsb[:], in_=MKT.rearrange("p a (b c) -> p (a b) c", c=256),
                                              axis=mybir.AxisListType.X, op=ALU.add),
             reads=[f"mkt{t}" for t in range(NT)], writes=["kmsb"])
        S.op("dve", lambda e: e.tensor_scalar(out=self.kmsb[:], in0=self.kmsb[:], scalar1=1.0 / 256, scalar2=None, op0=ALU.mult),
             reads=["kmsb"], writes=["kmsb"])
        S.dma("sp", lambda e: e.dma_start(out=self.ccB_in[:, 0:24], in_=self.kmsb[:]), reads=["kmsb"], writes=["ccB_in"])
        for pr in range(3):
            S.dma("sp", lambda e, pr=pr: e.dma_start(out=self.ccK_in[pr], in_=MKT[:, pr, :]),
                  reads=[f"mkt{t}" for t in range(NT)], writes=[f"ccK_in{pr}"])
            S.dma("sp", lambda e, pr=pr: e.dma_start(out=self.ccV_in[pr], in_=VIMG[:, pr, :, :].rearrange("p t c -> p (t c)")),
                  reads=[f"vimg{t}" for t in range(NT)], writes=[f"ccV_in{pr}"])
        dec = self.cfv("dec").unsqueeze(2).to_broadcast([128, 3, 64])
        S.op("dve", lambda e: e.tensor_copy(out=self.state[:], in_=KVS[:, 0, :, :]), reads=["kvs0a", "kvs0b"], writes=["state"])
        S.op("dve", lambda e: e.tensor_tensor(out=self.state[:], in0=self.state[:], in1=dec, op=ALU.mult), reads=["state", "cf"], writes=["state"])
        for ch in range(1, 8):
            S.op("dve", lambda e, ch=ch: e.tensor_tensor(out=self.state[:], in0=self.state[:], in1=KVS[:, ch, :, :], op=ALU.add),
                 reads=["state", f"kvs{ch}a", f"kvs{ch}b"], writes=["state"])
            S.op("dve", lambda e: e.tensor_tensor(out=self.state[:], in0=self.state[:], in1=dec, op=ALU.mult), reads=["state", "cf"], writes=["state"])
        S.dma("sp", lambda e: e.dma_start(out=self.ccB_in[:, 24:216], in_=self.state[:].rearrange("p a b -> p (a b)")),
              reads=["state"], writes=["ccB_in"])
        for j, c0 in enumerate((2688, 2816, 2944, 3072)):
            S.dma("pool", lambda e, j=j, c0=c0: e.dma_start(out=CW[:, j, :, :], in_=winv[:, :, c0:c0 + 128]), writes=[f"cw{j}"])
        sgm = [self.carve(A_TMP + 4096 + 2048 * i, [128, 512], F32) for i in range(2)]
        cc_ = 0
        for c in range(2):
            for tg in range(4):
                pa, pg = self.psf[cc_ % 2], self.psf[4 + cc_ % 2]
                ka, kg = f"psf{cc_ % 2}", f"psf{4 + cc_ % 2}"
                hk = [f"hT{t}" for t in range(4 * tg, 4 * tg + 4)]
                for kc in range(8):
                    S.op("pe", lambda e, kc=kc, c=c, tg=tg, pa=pa: e.matmul(pa[:], lhsT=CW[:, c, kc, :], rhs=HT[:, kc, tg * 512:(tg + 1) * 512],
                                                                         start=(kc == 0), stop=(kc == 7)),
                         reads=hk + [f"cw{c}"], writes=[ka], signal=(kc == 7))
                for kc in range(8):
                    S.op("pe", lambda e, kc=kc, c=c, tg=tg, pg=pg: e.matmul(pg[:], lhsT=CW[:, 2 + c, kc, :], rhs=HT[:, kc, tg * 512:(tg + 1) * 512],
                                                                         start=(kc == 0), stop=(kc == 7)),
                         reads=hk + [f"cw{2 + c}"], writes=[kg], signal=(kc == 7))
                s_ = sgm[cc_ % 2]
                S.op("act", lambda e, s_=s_, pg=pg: e.activation(out=s_, in_=pg[:], func=AF.Sigmoid), reads=[kg], writes=[f"sgm{cc_ % 2}"])
                S.op("dve", lambda e, s_=s_, pa=pa, c=c, tg=tg: e.tensor_tensor(out=UT[:, c, 32 + tg * 512:32 + (tg + 1) * 512], in0=pa[:], in1=s_, op=ALU.mult),
                     reads=[ka, f"sgm{cc_ % 2}"], writes=[f"ut{c}_{tg}"])
                cc_ += 1
        S.op("dve", lambda e: e.tensor_copy(out=self.tail_f[:], in_=UT[:, :, 2050:2080]), reads=["ut0_3", "ut1_3"], writes=["tail_f"])
        S.dma("sp", lambda e: e.dma_start(out=self.ccB_in[:, 216:276], in_=self.tail_f[:].rearrange("p a b -> p (a b)")),
              reads=["tail_f"], writes=["ccB_in"])
        if self.stop == "proj":
            return
        S.cc(lambda e: e.collective_compute("AllGather", ALU.bypass, replica_groups=[[0, 1, 2, 3], [4, 5, 6, 7]],
                                            ins=[self.ccB_in], outs=[self.ccB_out]), reads=["ccB_in"], writes=["ccB_out"])
        for pr in range(3):
            S.cc(lambda e, pr=pr: e.collective_compute("AllGather", ALU.bypass, replica_groups=[[0, 1, 2, 3], [4, 5, 6, 7]],
                                                       ins=[self.ccK_in[pr]], outs=[self.ccK_out[pr]]), reads=[f"ccK_in{pr}"], writes=[f"ccK_out{pr}"])
            S.cc(lambda e, pr=pr: e.collective_compute("AllGather", ALU.bypass, replica_groups=[[0, 1, 2, 3], [4, 5, 6, 7]],
                                                       ins=[self.ccV_in[pr]], outs=[self.ccV_out[pr]]), reads=[f"ccV_in{pr}"], writes=[f"ccV_out{pr}"])
        ccBv = self.ccB_out.rearrange("(r p) w -> p r w", p=128)
        S.dma("sp", lambda e: e.dma_start(out=self.send[:], in_=ccBv[:, :, 24:216]), reads=["ccB_out"], writes=["send"])
        S.dma("sp", lambda e: e.dma_start(out=self.tails[:], in_=ccBv[:, :, 216:276]), reads=["ccB_out"], writes=["tails"])
        for h in range(6):
            S.dma("sp", lambda e, h=h: e.dma_start(
                out=self.kmall[:, h, :].rearrange("p (r n) -> p r n", r=4),
                in_=ccBv[(h % 2) * 64:(h % 2) * 64 + 64, :, (h // 2) * 8:(h // 2) * 8 + 8]), reads=["ccB_out"], writes=["kmall"])
        if self.stop == "cc":
            return
        coefr = self.cfv("coef").rearrange("p (r a) -> p r a", r=4)
        coef = [coefr[:, r_, :].unsqueeze(2).to_broadcast([128, 3, 64]) for r_ in range(4)]
        sv = self.send[:].rearrange("p r (a b) -> p r a b", a=3)
        stt = self.carve(A_TMP + 3840, [128, 3, 64], F32)
        S.op("dve", lambda e: e.tensor_tensor(out=self.state[:], in0=sv[:, 0], in1=coef[0], op=ALU.mult), reads=["send", "cf", "state"], writes=["state"])
        for r_ in range(1, 4):
            S.op("dve", lambda e, r_=r_: e.tensor_tensor(out=stt, in0=sv[:, r_], in1=coef[r_], op=ALU.mult), reads=["send", "cf"], writes=["stt"])
            S.op("dve", lambda e: e.tensor_tensor(out=self.state[:], in0=self.state[:], in1=stt, op=ALU.add), reads=["state", "stt"], writes=["state"])
        MIXT = self.carve(A_HT, [128, 8, TOK], BF16)
        ptb = [self.carve(A_TMP + 4608 + 768 * i, [128, 384], BF16) for i in range(3)]
        ytok = [self.carve(A_TMP + 6912 + 768 * i, [128, 384], BF16) for i in range(2)]
        ysq = self.carve(A_TMP + 8448, [128, 6, 64], F32)
        rs6 = self.carve(A_TMP + 9984, [128, 6], F32)
        pti = 0
        for ch in range(8):
            S.op("act", lambda e: e.activation(out=self.stbf[:], in_=self.state[:], func=AF.Copy), reads=["state"], writes=["stbf"])
            t0, t1 = 2 * ch, 2 * ch + 1
            po = [self.psf[2], self.psf[3]]
            pok = ["psf2", "psf3"]
            first = [True, True]
            for h in range(6):
                pr, hh = h // 2, h % 2
                prt = slice(hh * 64, hh * 64 + 64)
                ps = self.psf[pti % 2]
                psk = f"psf{pti % 2}"
                S.op("pe", lambda e, ps=ps, pr=pr, prt=prt, t0=t0: e.matmul(ps[:, 0:256], lhsT=KRT[prt, pr, t0 * 128:(t0 + 1) * 128],
                                                                         rhs=QRT[prt, pr, t0 * 128:(t0 + 2) * 128], start=True, stop=False),
                     reads=[f"krt{t0}", f"qrt{t0}", f"qrt{t1}"], writes=[psk], signal=False)
                S.op("pe", lambda e, ps=ps, pr=pr, prt=prt, t1=t1: e.matmul(ps[:, 256:384], lhsT=KRT[prt, pr, t1 * 128:(t1 + 1) * 128],
                                                                         rhs=QRT[prt, pr, t1 * 128:(t1 + 1) * 128], start=False, stop=True),
                     reads=[f"krt{t1}", f"qrt{t1}"], writes=[psk])
                pt = ptb[pti % 3]
                ptk = f"ptb{pti % 3}"
                pti += 1
                S.op("dve", lambda e, pt=pt, ps=ps: e.tensor_tensor(out=pt, in0=ps[:, 0:384], in1=self.mask01[:], op=ALU.mult),
                     reads=[psk, "mask01"], writes=[ptk])
                hc = slice(h * 64, h * 64 + 64)
                S.op("pe", lambda e, pt=pt, hc=hc, t0=t0, f0=first[0]: e.matmul(po[0][:, hc], lhsT=pt[:, 0:128], rhs=VTOK[:, t0, hc], start=f0, stop=False),
                     reads=[ptk, f"vtok{t0}"], writes=[pok[0]], signal=False)
                first[0] = False
                S.op("pe", lambda e, hc=hc, pr=pr, prt=prt, t0=t0, h=h: e.matmul(po[0][:, hc], lhsT=QRT[prt, pr, t0 * 128:(t0 + 1) * 128],
                                                                              rhs=self.stbf[prt, pr, :], start=False, stop=(h == 5)),
                     reads=[f"qrt{t0}", "stbf"], writes=[pok[0]], signal=(h == 5))
                S.op("pe", lambda e, pt=pt, hc=hc, t0=t0, f1=first[1]: e.matmul(po[1][:, hc], lhsT=pt[:, 128:256], rhs=VTOK[:, t0, hc], start=f1, stop=False),
                     reads=[ptk, f"vtok{t0}"], writes=[pok[1]], signal=False)
                first[1] = False
                S.op("pe", lambda e, pt=pt, hc=hc, t1=t1: e.matmul(po[1][:, hc], lhsT=pt[:, 256:384], rhs=VTOK[:, t1, hc], start=False, stop=False),
                     reads=[ptk, f"vtok{t1}"], writes=[pok[1]], signal=False)
                S.op("pe", lambda e, hc=hc, pr=pr, prt=prt, t1=t1, h=h: e.matmul(po[1][:, hc], lhsT=QRT[prt, pr, t1 * 128:(t1 + 1) * 128],
                                                                              rhs=self.stbf[prt, pr, :], start=False, stop=(h == 5)),
                     reads=[f"qrt{t1}", "stbf"], writes=[pok[1]], signal=(h == 5))
            for ci, t in enumerate((t0, t1)):
                pv = po[ci][:, 0:384].rearrange("p (h d) -> p h d", h=6)
                S.op("act", lambda e, pv=pv: e.activation(out=ysq, in_=pv, func=AF.Square), reads=[pok[ci]], writes=["ysq"])
                S.op("dve", lambda e: e.tensor_reduce(out=self.ssq[:], in_=ysq, axis=mybir.AxisListType.X, op=ALU.add), reads=["ysq"], writes=["ssq"])
                S.op("dve", lambda e: e.tensor_scalar(out=rs6, in0=self.ssq[:], scalar1=1.0 / 64, scalar2=EPS, op0=ALU.mult, op1=ALU.add),
                     reads=["ssq"], writes=["rs6"])
                S.op("act", lambda e: e.activation(out=rs6, in_=rs6, func=AF.Sqrt), reads=["rs6"], writes=["rs6"])
                S.op("dve", lambda e: e.reciprocal(out=rs6, in_=rs6), reads=["rs6"], writes=["rs6"])
                yt = ytok[t % 2]
                ytk = f"ytok{t % 2}"
                rgv = RG[:, t, :].rearrange("p (h d) -> p h d", h=6)
                ytv = yt.rearrange("p (h d) -> p h d", h=6)
                for h in range(6):
                    S.op("dve", lambda e, h=h, pv=pv, ytv=ytv, rgv=rgv: e.scalar_tensor_tensor(
                        out=ytv[:, h, :], in0=pv[:, h, :], scalar=rs6[:, h:h + 1], in1=rgv[:, h, :], op0=ALU.mult, op1=ALU.mult),
                        reads=[pok[ci], "rs6", f"rg{t}"], writes=[ytk], signal=(h == 5))
                pb = self.psb[t % 2]
                for pr in range(3):
                    S.op("pe", lambda e, pr=pr, pb=pb, yt=yt: e.transpose(out=pb[:, pr * 128:(pr + 1) * 128], in_=yt[:, pr * 128:(pr + 1) * 128],
                                                                         identity=self.identb[:]),
                         reads=[ytk, "identb"], writes=[f"psb{t % 2}"], signal=(pr == 2))
                self.copy(self.cp_eng(), MIXT[:, 0:3, t * 128:(t + 1) * 128], pb[:, 0:384].rearrange("p (a b) -> p a b", a=3),
                          reads=[f"psb{t % 2}"], writes=[f"mixT{t}"])
            S.op("dve", lambda e, ch=ch: e.tensor_tensor(out=self.state[:], in0=self.state[:], in1=KVS[:, ch, :, :], op=ALU.add),
                 reads=["state", f"kvs{ch}a", f"kvs{ch}b"], writes=["state"])
            S.op("dve", lambda e: e.tensor_tensor(out=self.state[:], in0=self.state[:], in1=dec, op=ALU.mult), reads=["state", "cf"], writes=["state"])
        if "mixret" in self.taps and l == 0:
            S.barrier()
            self.tap_mixT("mixret")
        S.barrier()
        if self.stop == "ret":
            return
        self.conv(l)
        S.barrier()
        if self.stop == "conv":
            return
        self.moba(l)
        S.barrier()
        if self.stop == "moba":
            return
        if "mixT" in self.taps and l == 0:
            self.tap_mixT("mixT")
            S.barrier()
        WOUT = self.carve(A_WOUT, [128, 8, D_MODEL], BF16)
        S.dma("pool", lambda e: e.dma_start(out=WOUT, in_=self.w["w_out"][l].rearrange("(k p) f -> p k f", p=128)),
              writes=["wout", "gain"] + [f"vimg{t}" for t in range(NT)])
        S.dma("sp", lambda e: e.dma_start(out=X, in_=self.xsp.rearrange("p (t f) -> p t f", t=NT)), reads=["xsp"], writes=xkeys)
        for t in range(NT):
            for hf in range(2):
                pd = self.psf[4 + (2 * t + hf) % 2]
                kd = f"psf{4 + (2 * t + hf) % 2}"
                for kc in range(8):
                    S.op("pe", lambda e, kc=kc, t=t, hf=hf, pd=pd: e.matmul(pd[:], lhsT=MIXT[:, kc, t * 128:(t + 1) * 128],
                                                                         rhs=WOUT[:, kc, hf * 512:(hf + 1) * 512], start=(kc == 0), stop=(kc == 7)),
                         reads=["wout"], writes=[kd], signal=(kc == 7))
                S.op("dve", lambda e, t=t, hf=hf, pd=pd: e.tensor_tensor(out=X[:, t, hf * 512:(hf + 1) * 512], in0=pd[:],
                                                                       in1=X[:, t, hf * 512:(hf + 1) * 512], op=ALU.add),
                     reads=[kd, f"x{t}"], writes=[f"x{t}"])

    def tap_mixT(self, name):
        S = self.S
        MIXT = self.carve(A_HT, [128, 8, TOK], BF16)
        tmp = self.carve(A_CONVTMP, [128, TOK], F32)
        for kc in range(8):
            S.op("dve", lambda e, kc=kc: e.tensor_copy(out=tmp, in_=MIXT[:, kc, :]), writes=["taptmp"])
            S.dma("sp", lambda e, kc=kc: e.dma_start(out=self.tapouts[name][:, kc * TOK:(kc + 1) * TOK], in_=tmp), reads=["taptmp"],
                  writes=["tap_" + name + str(kc)])
        S.barrier()

    def conv(self, l):
        S = self.S
        UT = self.carve(A_UT, [128, 2, 2080], BF16)
        MIXT = self.carve(A_HT, [128, 8, TOK], BF16)
        sel = self.cfv("sel")
        identf = self.cfv("identf")
        bd = self.cfv("bd")
        tl = self.tails[:]
        S.op("dve", lambda e: e.tensor_scalar(out=self.halo[:], in0=tl[:, 0, :], scalar1=sel[:, 0:1], scalar2=None, op0=ALU.mult),
             reads=["tails", "cf"], writes=["halo"])
        for r_ in range(1, 4):
            S.op("dve", lambda e, r_=r_: e.scalar_tensor_tensor(out=self.halo[:], in0=tl[:, r_, :], scalar=sel[:, r_:r_ + 1], in1=self.halo[:],
                                                               op0=ALU.mult, op1=ALU.add), reads=["tails", "cf", "halo"], writes=["halo"])
        S.op("dve", lambda e: e.tensor_copy(out=UT[:, :, 2:32], in_=self.halo[:].rearrange("p (a b) -> p a b", a=2)), reads=["halo"], writes=["ut_halo"])
        diag = [self.carve(A_CONVTMP + 256 * i, [128, 128], BF16) for i in range(4)]
        ysb = self.carve(A_CONVTMP + 1024, [128, 512], F32)
        ysq = self.carve(A_CONVTMP + 3072, [128, 512], F32)
        msb = self.carve(A_CONVTMP + 5120, [128, 512], F32)
        var = self.carve(A_CONVTMP + 7168, [128, 512], F32)
        dj = 0
        for c in range(2):
            for j in range(31):
                dg = diag[dj % 4]
                dk = f"diag{dj % 4}"
                dj += 1
                S.op("dve", lambda e, dg=dg, c=c, j=j: e.tensor_scalar(out=dg, in0=identf, scalar1=self.cpar[:, c, j:j + 1], scalar2=None, op0=ALU.mult),
                     reads=["cf", "cpar"], writes=[dk])
                for tg in range(4):
                    S.op("pe", lambda e, dg=dg, c=c, j=j, tg=tg: e.matmul(self.psf[tg][:], lhsT=dg, rhs=UT[:, c, 2 + tg * 512 + j:2 + tg * 512 + j + 512],
                                                                         start=(j == 0), stop=(j == 30)),
                         reads=[dk, f"ut{c}_{tg}", "ut_halo"] + ([f"ut{c}_{tg - 1}"] if tg > 0 else []), writes=[f"psf{tg}"], signal=(tg == 3))
            for tg in range(4):
                pk = f"psf{tg}"
                S.op("act", lambda e, tg=tg, c=c: e.activation(out=ysb, in_=self.psf[tg][:], func=AF.Identity, bias=self.cpar[:, c, 31:32], scale=1.0),
                     reads=[pk, "cpar"], writes=["c_ysb"])
                S.op("act", lambda e: e.activation(out=ysq, in_=ysb, func=AF.Square), reads=["c_ysb"], writes=["c_ysq"])
                S.op("pe", lambda e: e.matmul(self.psf[4][:], lhsT=bd, rhs=ysb, start=True, stop=True), reads=["cf", "c_ysb"], writes=["psf4"])
                S.op("pe", lambda e: e.matmul(self.psf[5][:], lhsT=bd, rhs=ysq, start=True, stop=True), reads=["cf", "c_ysq"], writes=["psf5"])
                S.op("act", lambda e: e.activation(out=msb, in_=self.psf[4][:], func=AF.Copy), reads=["psf4"], writes=["c_msb"])
                S.op("dve", lambda e: e.tensor_tensor(out=var, in0=msb, in1=msb, op=ALU.mult), reads=["c_msb"], writes=["c_var"])
                S.op("dve", lambda e: e.tensor_tensor(out=var, in0=self.psf[5][:], in1=var, op=ALU.subtract), reads=["psf5", "c_var"], writes=["c_var"])
                S.op("dve", lambda e: e.tensor_scalar(out=var, in0=var, scalar1=EPS, scalar2=0.0, op0=ALU.add, op1=ALU.max), reads=["c_var"], writes=["c_var"])
                S.op("act", lambda e: e.activation(out=var, in_=var, func=AF.Sqrt), reads=["c_var"], writes=["c_var"])
                S.op("dve", lambda e: e.reciprocal(out=var, in_=var), reads=["c_var"], writes=["c_var"])
                S.op("dve", lambda e: e.tensor_tensor(out=ysb, in0=ysb, in1=msb, op=ALU.subtract), reads=["c_ysb", "c_msb"], writes=["c_ysb"])
                S.op("dve", lambda e: e.tensor_tensor(out=ysb, in0=ysb, in1=var, op=ALU.mult), reads=["c_ysb", "c_var"], writes=["c_ysb"])
                S.op("dve", lambda e, c=c: e.tensor_scalar(out=ysb, in0=ysb, scalar1=self.cpar[:, c, 32:33], scalar2=self.cpar[:, c, 33:34],
                                                          op0=ALU.mult, op1=ALU.add), reads=["c_ysb", "cpar"], writes=["c_ysb"])
                S.op("act", lambda e, c=c, tg=tg: e.activation(out=MIXT[:, 6 + c, tg * 512:(tg + 1) * 512], in_=ysb, func=AF.Silu),
                     reads=["c_ysb"], writes=[f"mixTc{c}_{tg}"])

    def moba(self, l):
        S = self.S
        MQT = self.carve(A_MQT, [128, 3, TOK], BF16)
        QAUG = self.carve(A_QAUG, [96, 6, TOK], BF16)
        MIXT = self.carve(A_HT, [128, 8, TOK], BF16)
        KT, VR = self.KT, self.VR
        onesf = self.cfv("onesf")
        cap = self.cfv("cap").rearrange("p (a b) -> p a b", a=8)
        pastneg = self.cfv("pastneg").rearrange("p (a b) -> p a b", a=8)
        futneg = self.cfv("futneg").rearrange("p (a b) -> p a b", a=8)
        for i in range(4):
            S.dma("pool", lambda e, i=i: e.dma_start(out=KT[i][64:96, :], in_=self.er_in[i]), writes=[f"kt{i}"])
            S.op("dve", lambda e, i=i: e.memset(VR[i][:, :, 64:66], 1.0), writes=[f"vr{i}"])
        for h in range(6):
            S.dma("sp", lambda e, h=h: e.dma_start(out=QAUG[0:64, h, :], in_=MQT[(h % 2) * 64:(h % 2) * 64 + 64, h // 2, :]),
                  reads=[f"mqt{t}" for t in range(NT)], writes=[f"qaug{h}"])
        S.op("dve", lambda e: e.tensor_copy(out=self.kmhi[:], in_=self.kmall[:]), reads=["kmall"], writes=["kmhi"])
        S.op("dve", lambda e: e.tensor_copy(out=self.kmt[:], in_=self.kmhi[:]), reads=["kmhi"], writes=["kmt"])
        S.op("dve", lambda e: e.tensor_tensor(out=self.kmt[:], in0=self.kmall[:], in1=self.kmt[:], op=ALU.subtract), reads=["kmall", "kmt"], writes=["kmt"])
        S.op("dve", lambda e: e.tensor_copy(out=self.kmlo[:], in_=self.kmt[:]), reads=["kmt"], writes=["kmlo"])
        gi = 0
        for h in range(6):
            for tg in range(4):
                pb = self.psb[(h * 4 + tg) % 2]
                pbk = f"psb{(h * 4 + tg) % 2}"
                for tt in range(4):
                    t = 4 * tg + tt
                    b = t // 2
                    pg = self.psf[gi % 2]
                    pgk = f"psf{gi % 2}"
                    g1, m8, t1, mt = self.g1[gi % 2], self.m8[gi % 2], self.t1[gi % 2], self.mtpad[gi % 2]
                    k_ = gi % 2
                    gi += 1
                    S.op("pe", lambda e, pg=pg, h=h, t=t: e.matmul(pg[:, 0:32], lhsT=QAUG[0:64, h, t * 128:(t + 1) * 128], rhs=self.kmhi[:, h, :],
                                                                 start=True, stop=False), reads=[f"qaug{h}", "kmhi"], writes=[pgk], signal=False)
                    S.op("pe", lambda e, pg=pg, h=h, t=t: e.matmul(pg[:, 0:32], lhsT=QAUG[0:64, h, t * 128:(t + 1) * 128], rhs=self.kmlo[:, h, :],
                                                                 start=False, stop=True), reads=[f"qaug{h}", "kmlo"], writes=[pgk])
                    S.op("dve", lambda e, pg=pg, g1=g1, b=b: e.tensor_tensor(out=g1[:], in0=pg[:, 0:32], in1=cap[:, b, :], op=ALU.min),
                         reads=[pgk, "cf"], writes=[f"g1_{k_}"])
                    S.op("dve", lambda e, g1=g1, m8=m8: e.max(out=m8[:], in_=g1[:]), reads=[f"g1_{k_}"], writes=[f"m8_{k_}"])
                    S.op("dve", lambda e, g1=g1, m8=m8, t1=t1, b=b: e.scalar_tensor_tensor(out=t1[:], in0=g1[:], scalar=m8[:, 2:3], in1=pastneg[:, b, :],
                                                                                       op0=ALU.is_lt, op1=ALU.mult),
                         reads=[f"g1_{k_}", f"m8_{k_}", "cf"], writes=[f"t1_{k_}"])
                    S.op("dve", lambda e, t1=t1, mt=mt, b=b: e.tensor_tensor(out=mt[:, 64:96], in0=t1[:], in1=futneg[:, b, :], op=ALU.add),
                         reads=[f"t1_{k_}", "cf"], writes=[f"mtpad{k_}"])
                    S.op("pe", lambda e, mt=mt, pb=pb, tt=tt: e.transpose(out=pb[0:96, tt * 128:(tt + 1) * 128], in_=mt[:], identity=self.identb[:]),
                         reads=[f"mtpad{k_}", "identb"], writes=[pbk], signal=(tt == 3))
                self.copy(self.cp_eng(), QAUG[64:96, h, tg * 512:(tg + 1) * 512], pb[64:96, 0:512], reads=[pbk], writes=[f"qaug{h}"])
        PT = [self.carve(A_TMP + 1024 * i, [128, 512], BF16) for i in range(3)]
        osb = self.carve(A_TMP + 3072, [128, 512], F32)
        rec = self.carve(A_TMP + 5120, [128, 512], F32)
        otmp = self.carve(A_TMP + 7168, [128, 512], BF16)
        for h in range(6):
            steps = []
            for i in range(4):
                S.dma("sp", lambda e, i=i, h=h: e.dma_start(
                    out=KT[i][0:64, :], in_=self.ccK_out[h // 2][i * 128 + (h % 2) * 64:i * 128 + (h % 2) * 64 + 64, :]),
                    reads=[f"ccK_out{h // 2}"], writes=[f"kt{i}"])
                S.dma("sp", lambda e, i=i, h=h: e.dma_start(
                    out=VR[i][:, :, 0:64],
                    in_=self.ccV_out[h // 2][i * 128:i * 128 + 128, :].rearrange("p (t c) -> p t c", c=128)[:, :, (h % 2) * 64:(h % 2) * 64 + 64]),
                    reads=[f"ccV_out{h // 2}"], writes=[f"vr{i}"])
                for t in range(NT):
                    for g in range(4):
                        steps.append((i, t, g))
            ns = len(steps)

            def emit_s(k):
                i, t, g = steps[k]
                ps = self.psf[4 + k % 2]
                need_c = (4 * g <= t < 4 * g + 4)
                S.op("pe", lambda e, ps=ps, i=i, t=t, g=g, h=h: e.matmul(ps[:], lhsT=KT[i][0:96, t * 128:(t + 1) * 128], rhs=QAUG[:, h, g * 512:(g + 1) * 512],
                                                                       start=True, stop=not need_c),
                     reads=[f"kt{i}", f"qaug{h}"], writes=[f"psf{4 + k % 2}"], signal=not need_c)
                if need_c:
                    S.op("pe", lambda e, ps=ps, i=i, t=t, g=g: e.matmul(ps[:], lhsT=self.identR[:, i, :], rhs=self.cmask[:, t - 4 * g, :], start=False, stop=True),
                         reads=["identR", "cmask"], writes=[f"psf{4 + k % 2}"])

            def emit_pv(k):
                i, t, g = steps[k]
                ps = self.psf[4 + k % 2]
                pt = PT[k % 3]
                S.op("act", lambda e, ps=ps, pt=pt: e.activation(out=pt, in_=ps[:], func=AF.Exp, scale=0.125), reads=[f"psf{4 + k % 2}"], writes=[f"pt{k % 3}"])
                S.op("pe", lambda e, pt=pt, i=i, t=t, g=g: e.matmul(self.psf[g][0:65, :], lhsT=VR[i][:, t, 0:65], rhs=pt,
                                                                  start=(i == 0 and t == 0), stop=(i == 3 and t == NT - 1)),
                     reads=[f"pt{k % 3}", f"vr{i}"], writes=[f"psf{g}"], signal=True)

            emit_s(0)
            for k in range(ns):
                if k + 1 < ns:
                    emit_s(k + 1)
                emit_pv(k)
            for g in range(4):
                po = self.psf[g]
                S.op("act", lambda e, po=po: e.activation(out=osb[0:65, :], in_=po[0:65, :], func=AF.Copy), reads=[f"psf{g}"], writes=["osb"])
                S.op("dve", lambda e: e.reciprocal(out=rec[64:65, :], in_=osb[64:65, :]), reads=["osb"], writes=["rec"])
                pr_ = self.psf[4 + g % 2]
                prk = f"psf{4 + g % 2}"
                S.op("pe", lambda e, pr_=pr_: e.matmul(pr_[0:64, :], lhsT=onesf[64:65, 0:64], rhs=rec[64:65, :], start=True, stop=True),
                     reads=["cf", "rec"], writes=[prk])
                ch = 3 + h // 2
                if h % 2 == 0:
                    S.op("dve", lambda e, pr_=pr_, g=g, ch=ch: e.tensor_tensor(out=MIXT[0:64, ch, g * 512:(g + 1) * 512], in0=osb[0:64, :], in1=pr_[0:64, :], op=ALU.mult),
                         reads=["osb", prk], writes=[f"mixTm{h}_{g}"])
                else:
                    S.op("dve", lambda e, pr_=pr_: e.tensor_tensor(out=otmp[0:64, :], in0=osb[0:64, :], in1=pr_[0:64, :], op=ALU.mult),
                         reads=["osb", prk], writes=["otmp"])
                    S.dma("sp", lambda e, g=g, ch=ch: e.dma_start(out=MIXT[64:128, ch, g * 512:(g + 1) * 512], in_=otmp[0:64, :]),
                          reads=["otmp"], writes=[f"mixTm{h}_{g}"])

    def final(self):
        S = self.S
        S.barrier()
        X = self.carve(A_X, [128, NT, D_MODEL], F32)
        gain = self.carve(A_GAIN, [128, D_MODEL], F32)
        S.dma("sp", lambda e: e.dma_start(out=gain, in_=self.fnorm[0:1, :].to_broadcast([128, D_MODEL])), writes=["gain"])
        self.rms_stats()
        yo = self.y_out.rearrange("(t p) f -> p t f", p=128)
        ob = [self.carve(A_HT + 4096 * i, [128, D_MODEL], F32) for i in range(2)]
        for t in range(NT):
            o = ob[t % 2]
            S.op("dve", lambda e, t=t, o=o: e.scalar_tensor_tensor(out=o, in0=X[:, t, :], scalar=self.rstd[:, t:t + 1], in1=gain, op0=ALU.mult, op1=ALU.mult),
                 reads=[f"x{t}", "rstd", "gain"], writes=[f"fo{t % 2}"])
            S.dma("sp", lambda e, t=t, o=o: e.dma_start(out=yo[:, t, :], in_=o), reads=[f"fo{t % 2}"], writes=[f"y{t}"])


_PROG_CACHE = {}


def _get_prog(depth, do_final, taps=(), stop=None):
    key = (depth, do_final, tuple(taps), stop)
    if key not in _PROG_CACHE:
        p = Prog(depth, do_final, taps, stop)
        p.build()
        _PROG_CACHE[key] = p
    return _PROG_CACHE[key]


def _layer_inputs(inp, l0, l1):
    f32 = np.float32
    sl = slice(l0, l1)
    d = {}
    for f in (1, 2):
        d[f"wg{f}"] = np.ascontiguousarray(inp[f"ffn{f}_wg"][sl], f32)
        d[f"wu{f}"] = np.ascontiguousarray(inp[f"ffn{f}_wu"][sl], f32)
        d[f"wd{f}"] = np.ascontiguousarray(inp[f"ffn{f}_wd"][sl], f32)
    d["w_in"] = np.ascontiguousarray(inp["w_in"][sl], f32)
    d["w_out"] = np.ascontiguousarray(inp["w_out"][sl], f32)
    d["norms"] = np.ascontiguousarray(np.stack([inp["ffn1_norm"][sl], inp["mix_norm"][sl], inp["ffn2_norm"][sl]], 1), f32)
    d["fnorm"] = np.ascontiguousarray(inp["final_norm"], f32).reshape(1, D_MODEL)
    nl = l1 - l0
    cp = np.zeros((nl, 128, 2, 34), f32)
    cw = np.asarray(inp["conv_w"][sl], f32)
    cp[:, :, :, 0:31] = cw.transpose(0, 2, 1).reshape(nl, 2, 128, 31).transpose(0, 2, 1, 3)
    for j, nm in ((31, "conv_b"), (32, "conv_ln_g"), (33, "conv_ln_b")):
        cp[:, :, :, j] = np.asarray(inp[nm][sl], f32).reshape(nl, 2, 128).transpose(0, 2, 1)
    d["convp"] = cp
    return d


def _run(inp, xs, l0, l1, do_final, taps=(), stop=None):
    prog = _get_prog(l1 - l0, do_final, taps, stop)
    shared = _layer_inputs(inp, l0, l1)
    pos = np.asarray(inp["positions"], np.int32)
    in_maps = []
    for c in range(NCORES):
        b, r = c // 4, c % 4
        cf, cm, er = _host_consts(r)
        m = dict(shared)
        m["x"] = np.ascontiguousarray(xs[c], np.float32)
        m["pos"] = np.ascontiguousarray(pos[b, r * TOK:(r + 1) * TOK].reshape(NT, 128).T)
        m["cf"] = cf
        m["cmask"] = cm
        m["erows"] = er
        in_maps.append(m)
    res = run_bass_kernel_spmd(prog.nc, in_maps, core_ids=list(range(NCORES)))
    return res.results


FUSED = False


def kernel(**inputs):
    inp = {k: np.asarray(v) for k, v in inputs.items()}
    x = np.asarray(inp["x"], np.float32)
    xs = [x[c // 4, (c % 4) * TOK:(c % 4 + 1) * TOK] for c in range(NCORES)]
    if FUSED:
        res = _run(inp, xs, 0, DEPTH, True)
        xs = [r["y"] for r in res]
    else:
        for l in range(DEPTH):
            res = _run(inp, xs, l, l + 1, l == DEPTH - 1)
            xs = [r["y"] for r in res]
    out = np.zeros((2, SEQ, D_MODEL), np.float32)
    for c in range(NCORES):
        out[c // 4, (c % 4) * TOK:(c % 4 + 1) * TOK] = xs[c]
    return out
```

```python
import math
from contextlib import ExitStack

import numpy as np
import concourse.bass as bass
import concourse.mybir as mybir
from concourse.bass_utils import run_bass_kernel_spmd

F32 = mybir.dt.float32
BF16 = mybir.dt.bfloat16
I32 = mybir.dt.int32
ALU = mybir.AluOpType
AF = mybir.ActivationFunctionType

D_MODEL = 1024
SEQ = 8192
DEPTH = 4
D_FF = 2816
IN_W = 3200
NCORES = 8
TOK = 2048
NT = 16
NEG = -30000.0
EPS = 1e-6

ENGS = ("pe", "act", "dve", "pool", "sp")
NDSEM = 8


class Sched:
    def __init__(self, nc):
        self.nc = nc
        self.q = {e: [] for e in ENGS}
        self.sig = {e: 0 for e in ENGS}
        self.dcnt = {e: 0 for e in ENGS}
        self.ccnt = 0
        self.seen = {e: {} for e in ENGS}
        self.issued = {}
        self.lastw = {}
        self.readers = {}

    def _deps(self, eng, reads, writes):
        toks = []
        for b in reads:
            t = self.lastw.get(b)
            if t is not None:
                toks.append(t)
        for b in writes:
            t = self.lastw.get(b)
            if t is not None:
                toks.append(t)
            toks.extend(self.readers.get(b, ()))
        need = {}
        for (sk, v, e) in toks:
            if e == eng and sk[0] == "c":
                if eng == "pe" or v > self.sig[eng]:
                    continue
            if self.seen[eng].get(sk, 0) >= v:
                continue
            if need.get(sk, 0) < v:
                need[sk] = v
        for sk, v in need.items():
            self.seen[eng][sk] = v
        return list(need.items())

    def _commit(self, tok, reads, writes):
        for b in reads:
            self.readers.setdefault(b, []).append(tok)
        for b in writes:
            self.lastw[b] = tok
            self.readers[b] = []

    def op(self, eng, fn, reads=(), writes=(), signal=True):
        waits = self._deps(eng, reads, writes)
        if signal:
            self.sig[eng] += 1
            val = self.sig[eng]
            self.issued[("c", eng)] = val
        else:
            val = self.sig[eng] + 1
        tok = (("c", eng), val, eng)
        self.q[eng].append(("op", waits, fn, signal, None))
        self._commit(tok, reads, writes)
        return tok

    def dma(self, eng, fn, reads=(), writes=()):
        waits = self._deps(eng, reads, writes)
        i = self.dcnt[eng]
        self.dcnt[eng] += 1
        sk = ("d", eng, i % NDSEM)
        val = 16 * (i // NDSEM + 1)
        if val > 16 and self.seen[eng].get(sk, 0) < val - 16:
            waits.append((sk, val - 16))
            self.seen[eng][sk] = val - 16
        tok = (sk, val, eng)
        self.issued[sk] = val
        self.q[eng].append(("dma", waits, fn, True, sk))
        self._commit(tok, reads, writes)
        return tok

    def cc(self, fn, reads=(), writes=()):
        eng = "pool"
        waits = self._deps(eng, reads, writes)
        self.ccnt += 1
        sk = ("x", "cc")
        if self.ccnt > 1 and self.seen[eng].get(sk, 0) < self.ccnt - 1:
            waits.append((sk, self.ccnt - 1))
            self.seen[eng][sk] = self.ccnt - 1
        tok = (sk, self.ccnt, eng)
        self.issued[sk] = self.ccnt
        self.q[eng].append(("cc", waits, fn, True, sk))
        self._commit(tok, reads, writes)
        return tok

    def barrier(self, engines=ENGS):
        for e in engines:
            waits = []
            for sk, v in self.issued.items():
                if sk == ("c", e):
                    continue
                if self.seen[e].get(sk, 0) < v:
                    waits.append((sk, v))
                    self.seen[e][sk] = v
            self.q[e].append(("wait", waits, None, False, None))

    def wait_all(self, eng, bufs):
        waits = self._deps(eng, bufs, ())
        self.q[eng].append(("wait", waits, None, False, None))

    def emit(self):
        nc = self.nc
        used = []
        seen = set()
        for e in ENGS:
            for (kind, waits, fn, signal, sk) in self.q[e]:
                keys = [wk for (wk, v) in waits]
                if kind in ("dma", "cc"):
                    keys.append(sk)
                elif kind == "op" and signal:
                    keys.append(("c", e))
                for k in keys:
                    if k not in seen:
                        seen.add(k)
                        used.append(k)
        with ExitStack() as st:
            sems = {}
            for s in used:
                sems[s] = st.enter_context(nc.semaphore("s_" + "_".join(str(x) for x in s)))
            block = st.enter_context(nc.Block())

            def runner(e):
                def run(engobj):
                    for (kind, waits, fn, signal, sk) in self.q[e]:
                        for (wk, v) in waits:
                            engobj.wait_ge(sems[wk], v)
                        if kind == "op":
                            ins = fn(engobj)
                            if signal:
                                ins.then_inc(sems[("c", e)], 1)
                        elif kind == "dma":
                            fn(engobj).then_inc(sems[sk], 16)
                        elif kind == "cc":
                            fn(engobj).then_inc(sems[sk], 1)
                return run

            block.tensor(runner("pe"))
            block.scalar(runner("act"))
            block.vector(runner("dve"))
            block.gpsimd(runner("pool"))
            block.sync(runner("sp"))


CF = {}
_off = 0
for _name, _w in [("identf", 128), ("bd", 128), ("onesf", 128), ("invr", 32), ("invm", 8),
                  ("qs", 12), ("ks", 12), ("dec", 3), ("coef", 12), ("cap", 256),
                  ("pastneg", 256), ("futneg", 256), ("sel", 4), ("c256", 1), ("zero", 1),
                  ("mask01", 384), ("identR", 512)]:
    CF[_name] = (_off, _w)
    _off += _w
NCF = _off
NCS = CF["mask01"][0]


def _host_consts(rank):
    cf = np.zeros((128, NCF), np.float32)

    def put(name, arr):
        o, w = CF[name]
        cf[:, o:o + w] = np.asarray(arr, np.float32).reshape(128, w)

    p = np.arange(128)
    put("identf", np.eye(128))
    bd = np.zeros((128, 128))
    bd[:64, :64] = 1.0 / 64
    bd[64:, 64:] = 1.0 / 64
    put("bd", bd)
    put("onesf", np.ones((128, 128)))
    ret_inv = (10000.0 ** (-np.linspace(0.0, 1.0, 32, dtype=np.float32))).astype(np.float32)
    rope_inv = (500000.0 ** (-np.arange(8, dtype=np.float32) / 8)).astype(np.float32)
    put("invr", np.broadcast_to(ret_inv, (128, 32)))
    put("invm", np.broadcast_to(rope_inv, (128, 8)))
    hh = np.arange(6, dtype=np.float64)
    lg = np.log1p(-np.exp2(-5.0 - hh))
    qs = np.zeros((128, 2, 6))
    ks = np.zeros((128, 2, 6))
    for par in range(2):
        c = par * 128 + p
        qs[:, par] = np.exp(lg[None, :] * (c[:, None] + 1.0))
        ks[:, par] = np.exp(-lg[None, :] * (c[:, None] + 1.0)) * 0.125
    put("qs", qs)
    put("ks", ks)
    hd = np.zeros((128, 3), np.int64)
    for pr in range(3):
        hd[:64, pr] = 2 * pr
        hd[64:, pr] = 2 * pr + 1
    dec = np.exp(lg[hd] * 256.0)
    put("dec", dec)
    coef = np.zeros((128, 4, 3))
    for i in range(4):
        if i < rank:
            coef[:, i] = np.exp(lg[hd] * 2048.0 * (rank - 1 - i))
    put("coef", coef)
    cap = np.zeros((128, 8, 32))
    pastneg = np.zeros((128, 8, 32))
    futneg = np.zeros((128, 8, 32))
    n = np.arange(32)
    for b in range(8):
        own = 8 * rank + b
        cap[:, b] = np.where(n < own, 3.0e38, -1.0e9)[None, :]
        pastneg[:, b] = np.where(n < own, NEG, 0.0)[None, :]
        futneg[:, b] = np.where(n > own, NEG, 0.0)[None, :]
    put("cap", cap)
    put("pastneg", pastneg)
    put("futneg", futneg)
    sel = np.zeros((128, 4))
    if rank > 0:
        sel[:, rank - 1] = 1.0
    put("sel", sel)
    tri = (p[:, None] <= p[None, :]).astype(np.float32)
    put("mask01", np.concatenate([tri, np.ones((128, 128)), tri], 1))
    idr = np.zeros((128, 4, 128))
    idr[:, rank] = np.eye(128)
    put("identR", idr)
    put("c256", np.full((128, 1), 1.0 / 256))
    cm = np.zeros((128, 4, 512), np.float32)
    q = np.arange(512)
    for t in range(4):
        cm[:, t] = np.where((t * 128 + p)[:, None] > q[None, :], NEG, 0.0)
    er = np.zeros((4, 32, 2048), np.float32)
    key = np.arange(2048)
    for i in range(4):
        er[i, 8 * i + key // 256, key] = 1.0
    return cf, cm.reshape(128, 2048), er


ARENA = 176 * 1024
A_X = 0
A_HT = 65536
A_ACT = 98304
A_WD = 131072
A_WGU = 147456
A_GAIN = 159744
A_TMP = 163840
A_QRT = 0
A_KRT = 12288
A_VTOK = 24576
A_RG = 36864
A_MQT = 49152
A_MKT = 98304
A_KVS = 110592
A_UT = 116736
A_WIN = 125056
A_CW = 137344
A_WOUT = 145536
A_QAUG = 0
A_KT = 24576
A_CONVTMP = 0
A_VR = 125056


class Prog:
    def __init__(self, depth, do_final, taps=(), stop=None):
        self.stop = stop
        self.D = depth
        self.do_final = do_final
        self.taps = taps
        self.nc = bass.Bass("TRN2", target_bir_lowering=False)
        self.S = Sched(self.nc)
        self.tapouts = {}

    def carve(self, off, shape, dt):
        n = int(np.prod(shape[1:]))
        sz = 2 if dt == BF16 else 4
        assert off % 4 == 0 and off + n * sz <= ARENA, (off, shape)
        v = self.arena[:, off // 2: off // 2 + n * sz // 2]
        if dt != BF16:
            v = v.bitcast(dt)
        if len(shape) == 3:
            v = v.rearrange("p (a b) -> p a b", a=shape[1])
        elif len(shape) == 4:
            v = v.rearrange("p (a b c) -> p a b c", a=shape[1], b=shape[2])
        if shape[0] != 128:
            v = v[0:shape[0]]
        return v

    def cfv(self, name):
        o, w = CF[name]
        return self.cf[:, o:o + w]

    def build(self):
        nc, S, D = self.nc, self.S, self.D
        dram = lambda name, shape, dt, kind: nc.dram_tensor(name, shape, dt, kind=kind).ap()
        self.x_in = dram("x", [TOK, D_MODEL], F32, "ExternalInput")
        self.pos_in = dram("pos", [128, NT], I32, "ExternalInput")
        self.w = {}
        for f in (1, 2):
            self.w[f"wg{f}"] = dram(f"wg{f}", [D, D_MODEL, D_FF], F32, "ExternalInput")
            self.w[f"wu{f}"] = dram(f"wu{f}", [D, D_MODEL, D_FF], F32, "ExternalInput")
            self.w[f"wd{f}"] = dram(f"wd{f}", [D, D_FF, D_MODEL], F32, "ExternalInput")
        self.w["w_in"] = dram("w_in", [D, D_MODEL, IN_W], F32, "ExternalInput")
        self.w["w_out"] = dram("w_out", [D, D_MODEL, D_MODEL], F32, "ExternalInput")
        self.norms = dram("norms", [D, 3, D_MODEL], F32, "ExternalInput")
        self.fnorm = dram("fnorm", [1, D_MODEL], F32, "ExternalInput")
        self.convp = dram("convp", [D, 128, 2, 34], F32, "ExternalInput")
        self.cf_in = dram("cf", [128, NCF], F32, "ExternalInput")
        self.cm_in = dram("cmask", [128, 2048], F32, "ExternalInput")
        self.er_in = dram("erows", [4, 32, 2048], F32, "ExternalInput")
        self.y_out = dram("y", [TOK, D_MODEL], F32, "ExternalOutput")
        self.xsp = nc.dram_tensor("xsp", [128, NT * D_MODEL], F32).ap()
        self.ccK_in = [nc.dram_tensor(f"ccK_in{j}", [128, 2048], BF16).ap() for j in range(3)]
        self.ccK_out = [nc.dram_tensor(f"ccK_out{j}", [512, 2048], BF16).ap() for j in range(3)]
        self.ccV_in = [nc.dram_tensor(f"ccV_in{j}", [128, 2048], BF16).ap() for j in range(3)]
        self.ccV_out = [nc.dram_tensor(f"ccV_out{j}", [512, 2048], BF16).ap() for j in range(3)]
        self.ccB_in = nc.dram_tensor("ccB_in", [128, 276], F32).ap()
        self.ccB_out = nc.dram_tensor("ccB_out", [512, 276], F32).ap()
        for t in self.taps:
            self.tapouts[t] = dram("tap_" + t, [128, NT * D_MODEL], F32, "ExternalOutput")

        with ExitStack() as st:
            sb = lambda name, shape, dt: st.enter_context(nc.sbuf_tensor(name, shape, dt))
            self.arena = sb("arena", [128, ARENA // 2], BF16)
            self.cf = sb("cf_sb", [128, NCS], F32)
            self.identb = sb("identb", [128, 128], BF16)
            self.mask01 = sb("mask01", [128, 384], BF16)
            self.identR = sb("identR", [128, 4, 128], BF16)
            self.cmask = sb("cmask_sb", [128, 4, 512], BF16)
            self.c256b = sb("c256b", [128, 1], BF16)
            self.cosr = sb("cosr", [128, NT, 32], F32)
            self.sinr = sb("sinr", [128, NT, 32], F32)
            self.cosm = sb("cosm", [128, NT, 8], F32)
            self.sinm = sb("sinm", [128, NT, 8], F32)
            self.posi = sb("posi", [128, NT], I32)
            self.posf = sb("posf", [128, NT], F32)
            self.ms = sb("ms", [128, NT], F32)
            self.rstd = sb("rstd", [128, NT], F32)
            self.cpar = sb("cpar", [128, 2, 34], F32)
            self.state = sb("state", [128, 3, 64], F32)
            self.stbf = sb("stbf", [128, 3, 64], BF16)
            self.kmsb = sb("kmsb", [128, 24], F32)
            self.kmall = sb("kmall", [64, 6, 32], F32)
            self.kmhi = sb("kmhi", [64, 6, 32], BF16)
            self.kmlo = sb("kmlo", [64, 6, 32], BF16)
            self.kmt = sb("kmt", [64, 6, 32], F32)
            self.send = sb("send", [128, 4, 192], F32)
            self.tails = sb("tails", [128, 4, 60], F32)
            self.tail_f = sb("tail_f", [128, 2, 30], F32)
            self.halo = sb("halo", [128, 60], F32)
            self.mtpad = [sb(f"mtpad{i}", [128, 96], BF16) for i in range(2)]
            self.g1 = [sb(f"g1_{i}", [128, 32], F32) for i in range(2)]
            self.m8 = [sb(f"m8_{i}", [128, 8], F32) for i in range(2)]
            self.t1 = [sb(f"t1_{i}", [128, 32], F32) for i in range(2)]
            self.ssq = sb("ssq", [128, 6], F32)
            self.psf = [st.enter_context(nc.psum_tensor(f"psf{i}", [128, 512], F32)) for i in range(6)]
            self.psb = [st.enter_context(nc.psum_tensor(f"psb{i}", [128, 1024], BF16)) for i in range(2)]
            self.tcount = 0
            self.body()
            S.emit()
        return nc

    def cp_eng(self):
        self.tcount += 1
        return "act" if self.tcount % 2 else "dve"

    def copy(self, eng, out, in_, reads, writes):
        if eng == "act":
            self.S.op("act", lambda e: e.activation(out=out, in_=in_, func=AF.Copy), reads=reads, writes=writes)
        else:
            self.S.op(eng, lambda e: e.tensor_copy(out=out, in_=in_), reads=reads, writes=writes)

    def body(self):
        S, nc = self.S, self.nc
        self.setup()
        self.load_x()
        for l in range(self.D):
            self.ffn(l, 1)
            if "x_ffn1" in self.taps and l == 0:
                self.tap_x("x_ffn1")
            if self.stop == "ffn1":
                break
            self.mixer(l)
            if self.stop is not None:
                S.barrier()
                S.dma("sp", lambda e: e.dma_start(out=self.carve(A_X, [128, NT, D_MODEL], F32), in_=self.xsp.rearrange("p (t f) -> p t f", t=NT)),
                      reads=["xsp"], writes=[f"x{t}" for t in range(NT)])
                break
            if "x_mix" in self.taps and l == 0:
                self.tap_x("x_mix")
            self.ffn(l, 2)
        if self.do_final:
            self.final()
        else:
            self.store_x()
        S.barrier(("sp",))

    def tap_x(self, name):
        S = self.S
        X = self.carve(A_X, [128, NT, D_MODEL], F32)
        S.dma("sp", lambda e: e.dma_start(out=self.tapouts[name].rearrange("p (t f) -> p t f", t=NT), in_=X),
              reads=[f"x{t}" for t in range(NT)], writes=["tap_" + name])

    def tap_buf(self, name, view, reads, width):
        S = self.S
        S.dma("sp", lambda e: e.dma_start(out=self.tapouts[name][:, 0:width], in_=view), reads=reads, writes=["tap_" + name])

    def setup(self):
        S = self.S
        S.dma("sp", lambda e: e.dma_start(out=self.cf[:], in_=self.cf_in[:, 0:NCS]), writes=["cf"])
        S.dma("sp", lambda e: e.dma_start(out=self.posi[:], in_=self.pos_in), writes=["posi"])
        o, w = CF["identf"]
        S.dma("pool", lambda e: e.dma_start(out=self.identb[:], in_=self.cf_in[:, o:o + w]), writes=["identb"])
        o2, w2 = CF["mask01"]
        S.dma("pool", lambda e: e.dma_start(out=self.mask01[:], in_=self.cf_in[:, o2:o2 + w2]), writes=["mask01"])
        o3, w3 = CF["identR"]
        S.dma("pool", lambda e: e.dma_start(out=self.identR[:], in_=self.cf_in[:, o3:o3 + w3].rearrange("p (a b) -> p a b", a=4)),
              writes=["identR"])
        S.dma("pool", lambda e: e.dma_start(out=self.cmask[:], in_=self.cm_in.rearrange("p (a b) -> p a b", a=4)), writes=["cmask"])
        S.op("dve", lambda e: e.memset(self.c256b[:], 1.0 / 256), writes=["c256b"])
        self.KT = [self.carve(A_KT + i * 4096, [128, 2048], BF16) for i in range(4)]
        self.VR = [self.carve(A_VR + i * 2112, [128, 16, 66], BF16) for i in range(4)]
        for i in range(2):
            S.op("pool", lambda e, i=i: e.memset(self.mtpad[i][:], 0.0), writes=[f"mtpad{i}"])
        S.op("dve", lambda e: e.tensor_copy(out=self.posf[:], in_=self.posi[:]), reads=["posi"], writes=["posf"])
        tmp = self.carve(A_TMP, [128, NT, 32], F32)
        tmp2 = self.carve(A_TMP + 2048, [128, NT, 32], F32)
        tmpi = self.carve(A_TMP + 4096, [128, NT, 32], I32)
        tmp3 = self.carve(A_TMP + 6144, [128, NT, 32], F32)
        C1 = 6.28125
        C2 = 2 * math.pi - C1
        for (nf, inv, cosT, sinT) in ((32, "invr", self.cosr, self.sinr), (8, "invm", self.cosm, self.sinm)):
            ang, kf, ki, r2 = tmp[:, :, 0:nf], tmp2[:, :, 0:nf], tmpi[:, :, 0:nf], tmp3[:, :, 0:nf]
            invv = self.cfv(inv)
            for t in range(NT):
                S.op("dve", lambda e, t=t, ang=ang, invv=invv: e.tensor_scalar(
                    out=ang[:, t, :], in0=invv, scalar1=self.posf[:, t:t + 1], scalar2=None, op0=ALU.mult),
                    reads=["posf", "cf"], writes=["rt_ang"], signal=(t == NT - 1))
            S.op("dve", lambda e, ang=ang, kf=kf: e.tensor_scalar(out=kf, in0=ang, scalar1=1.0 / (2 * math.pi), scalar2=None, op0=ALU.mult),
                 reads=["rt_ang"], writes=["rt_kf"])
            S.op("dve", lambda e, ki=ki, kf=kf: e.tensor_copy(out=ki, in_=kf), reads=["rt_kf"], writes=["rt_ki"])
            S.op("dve", lambda e, ki=ki, kf=kf: e.tensor_copy(out=kf, in_=ki), reads=["rt_ki"], writes=["rt_kf"])
            S.op("dve", lambda e, ang=ang, kf=kf: e.scalar_tensor_tensor(out=ang, in0=kf, scalar=-C1, in1=ang, op0=ALU.mult, op1=ALU.add),
                 reads=["rt_kf", "rt_ang"], writes=["rt_ang"])
            S.op("dve", lambda e, ang=ang, kf=kf: e.scalar_tensor_tensor(out=ang, in0=kf, scalar=-C2, in1=ang, op0=ALU.mult, op1=ALU.add),
                 reads=["rt_kf", "rt_ang"], writes=["rt_ang"])
            S.op("dve", lambda e, ang=ang: e.tensor_scalar(out=ang, in0=ang, scalar1=math.pi, scalar2=-math.pi, op0=ALU.min, op1=ALU.max),
                 reads=["rt_ang"], writes=["rt_ang"])
            S.op("act", lambda e, ang=ang, sinT=sinT: e.activation(out=sinT[:], in_=ang, func=AF.Sin), reads=["rt_ang"], writes=["rt_sin"])
            S.op("dve", lambda e, ang=ang, kf=kf: e.tensor_scalar(out=kf, in0=ang, scalar1=math.pi / 2, scalar2=math.pi, op0=ALU.add, op1=ALU.is_gt),
                 reads=["rt_ang"], writes=["rt_kf"])
            S.op("dve", lambda e, ang=ang, kf=kf, r2=r2: e.scalar_tensor_tensor(out=r2, in0=kf, scalar=-2 * math.pi, in1=ang, op0=ALU.mult, op1=ALU.add),
                 reads=["rt_kf", "rt_ang"], writes=["rt_r2"])
            S.op("dve", lambda e, r2=r2: e.tensor_scalar(out=r2, in0=r2, scalar1=math.pi / 2, scalar2=math.pi, op0=ALU.add, op1=ALU.min),
                 reads=["rt_r2"], writes=["rt_r2"])
            S.op("act", lambda e, r2=r2, cosT=cosT: e.activation(out=cosT[:], in_=r2, func=AF.Sin), reads=["rt_r2"], writes=["rt_cos"])
        S.barrier()

    def load_x(self):
        S = self.S
        X = self.carve(A_X, [128, NT, D_MODEL], F32)
        xin = self.x_in.rearrange("(t p) f -> p t f", p=128)
        for q in range(4):
            S.dma("sp", lambda e, q=q: e.dma_start(out=X[:, 4 * q:4 * q + 4, :], in_=xin[:, 4 * q:4 * q + 4, :]),
                  writes=[f"x{t}" for t in range(4 * q, 4 * q + 4)])

    def store_x(self):
        S = self.S
        X = self.carve(A_X, [128, NT, D_MODEL], F32)
        yo = self.y_out.rearrange("(t p) f -> p t f", p=128)
        for q in range(4):
            S.dma("sp", lambda e, q=q: e.dma_start(out=yo[:, 4 * q:4 * q + 4, :], in_=X[:, 4 * q:4 * q + 4, :]),
                  reads=[f"x{t}" for t in range(4 * q, 4 * q + 4)], writes=[f"y{q}"])

    def rms_stats(self):
        S = self.S
        X = self.carve(A_X, [128, NT, D_MODEL], F32)
        junk = self.carve(A_TMP + 8192, [128, D_MODEL], BF16)
        for t in range(NT):
            S.op("act", lambda e, t=t: e.activation(out=junk, in_=X[:, t, :], func=AF.Square, accum_out=self.ms[:, t:t + 1]),
                 reads=[f"x{t}"], writes=["junk", "ms"])
        S.op("dve", lambda e: e.tensor_scalar(out=self.rstd[:], in0=self.ms[:], scalar1=1.0 / D_MODEL, scalar2=EPS, op0=ALU.mult, op1=ALU.add),
             reads=["ms"], writes=["rstd"])
        S.op("act", lambda e: e.activation(out=self.rstd[:], in_=self.rstd[:], func=AF.Sqrt), reads=["rstd"], writes=["rstd"])
        S.op("dve", lambda e: e.reciprocal(out=self.rstd[:], in_=self.rstd[:]), reads=["rstd"], writes=["rstd"])

    def norm_to_hT(self, gain_ap):
        S = self.S
        X = self.carve(A_X, [128, NT, D_MODEL], F32)
        HT = self.carve(A_HT, [128, 8, TOK], BF16)
        gain = self.carve(A_GAIN, [128, D_MODEL], F32)
        S.dma("sp", lambda e: e.dma_start(out=gain, in_=gain_ap.to_broadcast([128, D_MODEL])), writes=["gain"])
        self.rms_stats()
        hn = [self.carve(A_TMP + 2048 * i, [128, D_MODEL], BF16) for i in range(2)]
        for t in range(NT):
            h = hn[t % 2]
            S.op("dve", lambda e, t=t, h=h: e.scalar_tensor_tensor(out=h, in0=X[:, t, :], scalar=self.rstd[:, t:t + 1], in1=gain,
                                                                  op0=ALU.mult, op1=ALU.mult),
                 reads=[f"x{t}", "rstd", "gain"], writes=[f"hn{t % 2}"])
            pb = self.psb[t % 2]
            for kc in range(8):
                S.op("pe", lambda e, kc=kc, h=h, pb=pb: e.transpose(out=pb[:, kc * 128:(kc + 1) * 128], in_=h[:, kc * 128:(kc + 1) * 128],
                                                                   identity=self.identb[:]),
                     reads=[f"hn{t % 2}", "identb"], writes=[f"psb{t % 2}"], signal=(kc == 7))
            self.copy(self.cp_eng(), HT[:, :, t * 128:(t + 1) * 128], pb.rearrange("p (a b) -> p a b", a=8),
                      reads=[f"psb{t % 2}"], writes=[f"hT{t}"])

    def ffn(self, l, f):
        S = self.S
        wg, wu, wd = self.w[f"wg{f}"], self.w[f"wu{f}"], self.w[f"wd{f}"]
        S.barrier()
        self.norm_to_hT(self.norms[l, (0 if f == 1 else 2):(1 if f == 1 else 3), :])
        X = self.carve(A_X, [128, NT, D_MODEL], F32)
        HT = self.carve(A_HT, [128, 8, TOK], BF16)
        ACT = self.carve(A_ACT, [128, 8, TOK], BF16)
        WD = self.carve(A_WD, [128, 8, D_MODEL], BF16)
        WGU = [self.carve(A_WGU + 4096 * i, [128, 2, 8, 128], BF16) for i in range(3)]
        sg = [self.carve(A_TMP + 4096 + 2048 * i, [128, 512], F32) for i in range(2)]
        wgv = wg[l].rearrange("(k p) f -> p k f", p=128)
        wuv = wu[l].rearrange("(k p) f -> p k f", p=128)
        wdv = wd[l].rearrange("(c p) f -> p c f", p=128)
        cnt = 0
        for (c0, c1) in ((0, 8), (8, 16), (16, 22)):
            ncp = c1 - c0
            S.dma("pool", lambda e, c0=c0, c1=c1, ncp=ncp: e.dma_start(out=WD[:, 0:ncp, :], in_=wdv[:, c0:c1, :]), writes=["wd"])
            for c in range(c0, c1):
                slot = c % 3
                W = WGU[slot]
                S.dma("pool", lambda e, c=c, W=W: e.dma_start(out=W[:, 0, :, :], in_=wgv[:, :, c * 128:(c + 1) * 128]), writes=[f"wgu{slot}g"])
                S.dma("pool", lambda e, c=c, W=W: e.dma_start(out=W[:, 1, :, :], in_=wuv[:, :, c * 128:(c + 1) * 128]), writes=[f"wgu{slot}u"])
                for tg in range(4):
                    pg, pu = self.psf[cnt % 2], self.psf[2 + cnt % 2]
                    kg, ku = f"psf{cnt % 2}", f"psf{2 + cnt % 2}"
                    hk = [f"hT{t}" for t in range(4 * tg, 4 * tg + 4)]
                    for kc in range(8):
                        S.op("pe", lambda e, kc=kc, W=W, pg=pg, tg=tg: e.matmul(pg[:], lhsT=W[:, 0, kc, :], rhs=HT[:, kc, tg * 512:(tg + 1) * 512],
                                                                             start=(kc == 0), stop=(kc == 7)),
                             reads=hk + [f"wgu{slot}g"], writes=[kg], signal=(kc == 7))
                    for kc in range(8):
                        S.op("pe", lambda e, kc=kc, W=W, pu=pu, tg=tg: e.matmul(pu[:], lhsT=W[:, 1, kc, :], rhs=HT[:, kc, tg * 512:(tg + 1) * 512],
                                                                             start=(kc == 0), stop=(kc == 7)),
                             reads=hk + [f"wgu{slot}u"], writes=[ku], signal=(kc == 7))
                    s_ = sg[cnt % 2]
                    S.op("act", lambda e, s_=s_, pg=pg: e.activation(out=s_, in_=pg[:], func=AF.Silu), reads=[kg], writes=[f"sg{cnt % 2}"])
                    S.op("dve", lambda e, s_=s_, pu=pu, c=c, c0=c0, tg=tg: e.tensor_tensor(
                        out=ACT[:, c - c0, tg * 512:(tg + 1) * 512], in0=s_, in1=pu[:], op=ALU.mult),
                        reads=[f"sg{cnt % 2}", ku], writes=[f"act{c - c0}_{tg}"])
                    cnt += 1
            for t in range(NT):
                for hf in range(2):
                    pd = self.psf[4 + (2 * t + hf) % 2]
                    kd = f"psf{4 + (2 * t + hf) % 2}"
                    for cc in range(ncp):
                        S.op("pe", lambda e, cc=cc, t=t, hf=hf, pd=pd, ncp=ncp: e.matmul(pd[:], lhsT=ACT[:, cc, t * 128:(t + 1) * 128],
                                                                             rhs=WD[:, cc, hf * 512:(hf + 1) * 512],
                                                                             start=(cc == 0), stop=(cc == ncp - 1)),
                             reads=[f"act{cc}_{t // 4}", "wd"], writes=[kd], signal=(cc == ncp - 1))
                    S.op("dve", lambda e, t=t, hf=hf, pd=pd: e.scalar_tensor_tensor(
                        out=X[:, t, hf * 512:(hf + 1) * 512], in0=pd[:], scalar=0.5, in1=X[:, t, hf * 512:(hf + 1) * 512],
                        op0=ALU.mult, op1=ALU.add), reads=[kd, f"x{t}"], writes=[f"x{t}"])

    def rotary(self, ps, out_bf, t, half, cosT, sinT, tmp):
        S = self.S
        a, b = tmp[0][:, :, 0:half], tmp[1][:, :, 0:half]
        cb = cosT[:, t:t + 1, :].to_broadcast([128, 6, half])
        sbb = sinT[:, t:t + 1, :].to_broadcast([128, 6, half])
        x1, x2 = ps[:, :, 0:half], ps[:, :, half:2 * half]
        rk, wk = self._rot_keys
        S.op("dve", lambda e: e.tensor_tensor(out=a, in0=x1, in1=cb, op=ALU.mult), reads=rk, writes=["rot_a"])
        S.op("dve", lambda e: e.tensor_tensor(out=b, in0=x2, in1=sbb, op=ALU.mult), reads=rk, writes=["rot_b"])
        S.op("dve", lambda e: e.tensor_tensor(out=out_bf[:, :, 0:half], in0=a, in1=b, op=ALU.subtract), reads=["rot_a", "rot_b"], writes=wk)
        S.op("dve", lambda e: e.tensor_tensor(out=a, in0=x2, in1=cb, op=ALU.mult), reads=rk, writes=["rot_a"])
        S.op("dve", lambda e: e.tensor_tensor(out=b, in0=x1, in1=sbb, op=ALU.mult), reads=rk, writes=["rot_b"])
        S.op("dve", lambda e: e.tensor_tensor(out=out_bf[:, :, half:2 * half], in0=a, in1=b, op=ALU.add), reads=["rot_a", "rot_b"], writes=wk)

    def mixer(self, l):
        S = self.S
        S.barrier()
        self.norm_to_hT(self.norms[l, 1:2, :])
        X = self.carve(A_X, [128, NT, D_MODEL], F32)
        xkeys = [f"x{t}" for t in range(NT)]
        S.dma("sp", lambda e: e.dma_start(out=self.xsp.rearrange("p (t f) -> p t f", t=NT), in_=X), reads=xkeys, writes=["xsp"])
        S.dma("pool", lambda e: e.dma_start(out=self.cpar[:], in_=self.convp[l]), writes=["cpar"])
        S.barrier()
        if self.stop == "spill":
            return
        HT = self.carve(A_HT, [128, 8, TOK], BF16)
        QRT = self.carve(A_QRT, [128, 3, TOK], BF16)
        KRT = self.carve(A_KRT, [128, 3, TOK], BF16)
        VTOK = self.carve(A_VTOK, [128, NT, 384], BF16)
        RG = self.carve(A_RG, [128, NT, 384], BF16)
        MQT = self.carve(A_MQT, [128, 3, TOK], BF16)
        MKT = self.carve(A_MKT, [128, 3, TOK], BF16)
        KVS = self.carve(A_KVS, [128, 8, 3, 64], F32)
        UT = self.carve(A_UT, [128, 2, 2080], BF16)
        WIN = [self.carve(A_WIN + 6144 * i, [128, 8, 384], BF16) for i in range(2)]
        CW = self.carve(A_CW, [128, 4, 8, 128], BF16)
        VIMG = self.carve(A_WOUT, [128, 3, NT, 128], BF16)
        tokb = [self.carve(A_TMP + 768 * i, [128, 6, 64], BF16) for i in range(3)]
        rtmp = [self.carve(A_TMP + 2304 + 768 * i, [128, 6, 32], F32) for i in range(2)]
        winv = self.w["w_in"][l].rearrange("(k p) f -> p k f", p=128)
        hkeys = [f"hT{t}" for t in range(NT)]
        qs = self.cfv("qs").rearrange("p (a b) -> p a b", a=2)
        ks = self.cfv("ks").rearrange("p (a b) -> p a b", a=2)
        bc6 = lambda v: v.unsqueeze(2).to_broadcast([128, 6, 64])
        pcnt = 0
        tb = 0
        groups = [("rv", 768), ("rg", 1152), ("rk", 384), ("rq", 0), ("mv", 2304), ("mk", 1920), ("mq", 1536)]
        for gi, (gname, c0) in enumerate(groups):
            if self.stop is not None and self.stop.startswith("g:") and gi >= int(self.stop[2:]):
                return
            W = WIN[gi % 2]
            S.dma("pool", lambda e, W=W, c0=c0: e.dma_start(out=W, in_=winv[:, :, c0:c0 + 384]), writes=[f"win{gi % 2}"])
            for t in range(NT):
                ps = self.psf[pcnt % 2]
                pk = f"psf{pcnt % 2}"
                pcnt += 1
                for kc in range(8):
                    S.op("pe", lambda e, kc=kc, t=t, ps=ps, W=W: e.matmul(ps[:, 0:384], lhsT=HT[:, kc, t * 128:(t + 1) * 128], rhs=W[:, kc, :],
                                                                         start=(kc == 0), stop=(kc == 7)),
                         reads=[f"hT{t}", f"win{gi % 2}"], writes=[pk], signal=(kc == 7))
                psv = ps[:, 0:384].rearrange("p (h d) -> p h d", h=6)
                if gname == "rv":
                    S.op("act", lambda e, t=t, ps=ps: e.activation(out=VTOK[:, t, :], in_=ps[:, 0:384], func=AF.Copy), reads=[pk], writes=[f"vtok{t}"])
                elif gname == "rg":
                    S.op("act", lambda e, t=t, ps=ps: e.activation(out=RG[:, t, :], in_=ps[:, 0:384], func=AF.Silu), reads=[pk], writes=[f"rg{t}"])
                elif gname in ("rk", "rq"):
                    ob = tokb[tb % 3]
                    obk = f"tokb{tb % 3}"
                    tb += 1
                    self._rot_keys = ([pk, "rt_cos", "rt_sin"], [obk])
                    self.rotary(psv, ob, t, 32, self.cosr, self.sinr, rtmp)
                    sc = bc6((ks if gname == "rk" else qs)[:, t % 2, :])
                    obf = ob.rearrange("p h d -> p (h d)")
                    S.op("dve", lambda e, ob=ob, sc=sc: e.tensor_tensor(out=ob, in0=ob, in1=sc, op=ALU.mult), reads=[obk, "cf"], writes=[obk])
                    pb = self.psb[t % 2]
                    for pr in range(3):
                        S.op("pe", lambda e, pr=pr, pb=pb, obf=obf: e.transpose(out=pb[:, pr * 128:(pr + 1) * 128], in_=obf[:, pr * 128:(pr + 1) * 128],
                                                                               identity=self.identb[:]),
                             reads=[obk, "identb"], writes=[f"psb{t % 2}"], signal=(pr == 2))
                    dst = (KRT if gname == "rk" else QRT)
                    dk = ("krt" if gname == "rk" else "qrt") + str(t)
                    self.copy(self.cp_eng(), dst[:, :, t * 128:(t + 1) * 128], pb[:, 0:384].rearrange("p (a b) -> p a b", a=3),
                              reads=[f"psb{t % 2}"], writes=[dk])
                    if gname == "rk":
                        pkv = self.psf[2]
                        for pr in range(3):
                            S.op("pe", lambda e, pr=pr, t=t, obf=obf, pkv=pkv: e.matmul(
                                pkv[:, pr * 128:(pr + 1) * 128], lhsT=obf[:, pr * 128:(pr + 1) * 128], rhs=VTOK[:, t, pr * 128:(pr + 1) * 128],
                                start=(t % 2 == 0 and pr == 0), stop=(t % 2 == 1 and pr == 2)),
                                reads=[obk, f"vtok{t}"], writes=["psf2"], signal=(pr == 2))
                        if t % 2 == 1:
                            ch = t // 2
                            pv = pkv[:, 0:384].rearrange("p (a b) -> p a b", a=3)
                            S.op("act", lambda e, ch=ch, pv=pv: e.activation(out=KVS[0:64, ch, :, :], in_=pv[0:64, :, 0:64], func=AF.Copy),
                                 reads=["psf2"], writes=[f"kvs{ch}a"])
                            S.op("dve", lambda e, ch=ch, pv=pv: e.tensor_copy(out=KVS[64:128, ch, :, :], in_=pv[64:128, :, 64:128]),
                                 reads=["psf2"], writes=[f"kvs{ch}b"])
                elif gname == "mv":
                    S.op("act", lambda e, t=t, ps=ps: e.activation(out=VIMG[:, :, t, :], in_=ps[:, 0:384].rearrange("p (a b) -> p a b", a=3), func=AF.Copy),
                         reads=[pk], writes=[f"vimg{t}"])
                elif gname in ("mk", "mq"):
                    ob = tokb[tb % 3]
                    obk = f"tokb{tb % 3}"
                    tb += 1
                    obf = ob.rearrange("p h d -> p (h d)")
                    S.op("dve", lambda e, ob=ob, psv=psv: e.tensor_copy(out=ob[:, :, 16:64], in_=psv[:, :, 16:64]), reads=[pk], writes=[obk])
                    self._rot_keys = ([pk, "rt_cos", "rt_sin"], [obk])
                    self.rotary(psv, ob, t, 8, self.cosm, self.sinm, rtmp)
                    pb = self.psb[t % 2]
                    for pr in range(3):
                        S.op("pe", lambda e, pr=pr, pb=pb, obf=obf: e.transpose(out=pb[:, pr * 128:(pr + 1) * 128], in_=obf[:, pr * 128:(pr + 1) * 128],
                                                                               identity=self.identb[:]),
                             reads=[obk, "identb"], writes=[f"psb{t % 2}"], signal=(pr == 2))
                    dst = (MKT if gname == "mk" else MQT)
                    dk = ("mkt" if gname == "mk" else "mqt") + str(t)
                    self.copy(self.cp_eng(), dst[:, :, t * 128:(t + 1) * 128], pb[:, 0:384].rearrange("p (a b) -> p a b", a=3),
                              reads=[f"psb{t % 2}"], writes=[dk])
        if self.stop == "g:7":
            return
        S.op("dve", lambda e: e.tensor_reduce(out=self.kmsb[:], in_=MKT.rearrange("p a (b c) -> p (a b) c", c=256),
                                              axis=mybir.AxisListType.X, op=ALU.add),
             reads=[f"mkt{t}" for t in range(NT)], writes=["kmsb"])
        S.op("dve", lambda e: e.tensor_scalar(out=self.kmsb[:], in0=self.kmsb[:], scalar1=1.0 / 256, scalar2=None, op0=ALU.mult),
             reads=["kmsb"], writes=["kmsb"])
        S.dma("sp", lambda e: e.dma_start(out=self.ccB_in[:, 0:24], in_=self.kmsb[:]), reads=["kmsb"], writes=["ccB_in"])
        for pr in range(3):
            S.dma("sp", lambda e, pr=pr: e.dma_start(out=self.ccK_in[pr], in_=MKT[:, pr, :]),
                  reads=[f"mkt{t}" for t in range(NT)], writes=[f"ccK_in{pr}"])
            S.dma("sp", lambda e, pr=pr: e.dma_start(out=self.ccV_in[pr], in_=VIMG[:, pr, :, :].rearrange("p t c -> p (t c)")),
                  reads=[f"vimg{t}" for t in range(NT)], writes=[f"ccV_in{pr}"])
        dec = self.cfv("dec").unsqueeze(2).to_broadcast([128, 3, 64])
        S.op("dve", lambda e: e.tensor_copy(out=self.state[:], in_=KVS[:, 0, :, :]), reads=["kvs0a", "kvs0b"], writes=["state"])
        S.op("dve", lambda e: e.tensor_tensor(out=self.state[:], in0=self.state[:], in1=dec, op=ALU.mult), reads=["state", "cf"], writes=["state"])
        for ch in range(1, 8):
            S.op("dve", lambda e, ch=ch: e.tensor_tensor(out=self.state[:], in0=self.state[:], in1=KVS[:, ch, :, :], op=ALU.add),
                 reads=["state", f"kvs{ch}a", f"kvs{ch}b"], writes=["state"])
            S.op("dve", lambda e: e.tensor_tensor(out=self.state[:], in0=self.state[:], in1=dec, op=ALU.mult), reads=["state", "cf"], writes=["state"])
        S.dma("sp", lambda e: e.dma_start(out=self.ccB_in[:, 24:216], in_=self.state[:].rearrange("p a b -> p (a b)")),
              reads=["state"], writes=["ccB_in"])
        for j, c0 in enumerate((2688, 2816, 2944, 3072)):
            S.dma("pool", lambda e, j=j, c0=c0: e.dma_start(out=CW[:, j, :, :], in_=winv[:, :, c0:c0 + 128]), writes=[f"cw{j}"])
        sgm = [self.carve(A_TMP + 4096 + 2048 * i, [128, 512], F32) for i in range(2)]
        cc_ = 0
        for c in range(2):
            for tg in range(4):
                pa, pg = self.psf[cc_ % 2], self.psf[4 + cc_ % 2]
                ka, kg = f"psf{cc_ % 2}", f"psf{4 + cc_ % 2}"
                hk = [f"hT{t}" for t in range(4 * tg, 4 * tg + 4)]
                for kc in range(8):
                    S.op("pe", lambda e, kc=kc, c=c, tg=tg, pa=pa: e.matmul(pa[:], lhsT=CW[:, c, kc, :], rhs=HT[:, kc, tg * 512:(tg + 1) * 512],
                                                                         start=(kc == 0), stop=(kc == 7)),
                         reads=hk + [f"cw{c}"], writes=[ka], signal=(kc == 7))
                for kc in range(8):
                    S.op("pe", lambda e, kc=kc, c=c, tg=tg, pg=pg: e.matmul(pg[:], lhsT=CW[:, 2 + c, kc, :], rhs=HT[:, kc, tg * 512:(tg + 1) * 512],
                                                                         start=(kc == 0), stop=(kc == 7)),
                         reads=hk + [f"cw{2 + c}"], writes=[kg], signal=(kc == 7))
                s_ = sgm[cc_ % 2]
                S.op("act", lambda e, s_=s_, pg=pg: e.activation(out=s_, in_=pg[:], func=AF.Sigmoid), reads=[kg], writes=[f"sgm{cc_ % 2}"])
                S.op("dve", lambda e, s_=s_, pa=pa, c=c, tg=tg: e.tensor_tensor(out=UT[:, c, 32 + tg * 512:32 + (tg + 1) * 512], in0=pa[:], in1=s_, op=ALU.mult),
                     reads=[ka, f"sgm{cc_ % 2}"], writes=[f"ut{c}_{tg}"])
                cc_ += 1
        S.op("dve", lambda e: e.tensor_copy(out=self.tail_f[:], in_=UT[:, :, 2050:2080]), reads=["ut0_3", "ut1_3"], writes=["tail_f"])
        S.dma("sp", lambda e: e.dma_start(out=self.ccB_in[:, 216:276], in_=self.tail_f[:].rearrange("p a b -> p (a b)")),
              reads=["tail_f"], writes=["ccB_in"])
        if self.stop == "proj":
            return
        S.cc(lambda e: e.collective_compute("AllGather", ALU.bypass, replica_groups=[[0, 1, 2, 3], [4, 5, 6, 7]],
                                            ins=[self.ccB_in], outs=[self.ccB_out]), reads=["ccB_in"], writes=["ccB_out"])
        for pr in range(3):
            S.cc(lambda e, pr=pr: e.collective_compute("AllGather", ALU.bypass, replica_groups=[[0, 1, 2, 3], [4, 5, 6, 7]],
                                                       ins=[self.ccK_in[pr]], outs=[self.ccK_out[pr]]), reads=[f"ccK_in{pr}"], writes=[f"ccK_out{pr}"])
            S.cc(lambda e, pr=pr: e.collective_compute("AllGather", ALU.bypass, replica_groups=[[0, 1, 2, 3], [4, 5, 6, 7]],
                                                       ins=[self.ccV_in[pr]], outs=[self.ccV_out[pr]]), reads=[f"ccV_in{pr}"], writes=[f"ccV_out{pr}"])
        ccBv = self.ccB_out.rearrange("(r p) w -> p r w", p=128)
        S.dma("sp", lambda e: e.dma_start(out=self.send[:], in_=ccBv[:, :, 24:216]), reads=["ccB_out"], writes=["send"])
        S.dma("sp", lambda e: e.dma_start(out=self.tails[:], in_=ccBv[:, :, 216:276]), reads=["ccB_out"], writes=["tails"])
        for h in range(6):
            S.dma("sp", lambda e, h=h: e.dma_start(
                out=self.kmall[:, h, :].rearrange("p (r n) -> p r n", r=4),
                in_=ccBv[(h % 2) * 64:(h % 2) * 64 + 64, :, (h // 2) * 8:(h // 2) * 8 + 8]), reads=["ccB_out"], writes=["kmall"])
        if self.stop == "cc":
            return
        coefr = self.cfv("coef").rearrange("p (r a) -> p r a", r=4)
        coef = [coefr[:, r_, :].unsqueeze(2).to_broadcast([128, 3, 64]) for r_ in range(4)]
        sv = self.send[:].rearrange("p r (a b) -> p r a b", a=3)
        stt = self.carve(A_TMP + 3840, [128, 3, 64], F32)
        S.op("dve", lambda e: e.tensor_tensor(out=self.state[:], in0=sv[:, 0], in1=coef[0], op=ALU.mult), reads=["send", "cf", "state"], writes=["state"])
        for r_ in range(1, 4):
            S.op("dve", lambda e, r_=r_: e.tensor_tensor(out=stt, in0=sv[:, r_], in1=coef[r_], op=ALU.mult), reads=["send", "cf"], writes=["stt"])
            S.op("dve", lambda e: e.tensor_tensor(out=self.state[:], in0=self.state[:], in1=stt, op=ALU.add), reads=["state", "stt"], writes=["state"])
        MIXT = self.carve(A_HT, [128, 8, TOK], BF16)
        ptb = [self.carve(A_TMP + 4608 + 768 * i, [128, 384], BF16) for i in range(3)]
        ytok = [self.carve(A_TMP + 6912 + 768 * i, [128, 384], BF16) for i in range(2)]
        ysq = self.carve(A_TMP + 8448, [128, 6, 64], F32)
        rs6 = self.carve(A_TMP + 9984, [128, 6], F32)
        pti = 0
        for ch in range(8):
            S.op("act", lambda e: e.activation(out=self.stbf[:], in_=self.state[:], func=AF.Copy), reads=["state"], writes=["stbf"])
            t0, t1 = 2 * ch, 2 * ch + 1
            po = [self.psf[2], self.psf[3]]
            pok = ["psf2", "psf3"]
            first = [True, True]
            for h in range(6):
                pr, hh = h // 2, h % 2
                prt = slice(hh * 64, hh * 64 + 64)
                ps = self.psf[pti % 2]
                psk = f"psf{pti % 2}"
                S.op("pe", lambda e, ps=ps, pr=pr, prt=prt, t0=t0: e.matmul(ps[:, 0:256], lhsT=KRT[prt, pr, t0 * 128:(t0 + 1) * 128],
                                                                         rhs=QRT[prt, pr, t0 * 128:(t0 + 2) * 128], start=True, stop=False),
                     reads=[f"krt{t0}", f"qrt{t0}", f"qrt{t1}"], writes=[psk], signal=False)
                S.op("pe", lambda e, ps=ps, pr=pr, prt=prt, t1=t1: e.matmul(ps[:, 256:384], lhsT=KRT[prt, pr, t1 * 128:(t1 + 1) * 128],
                                                                         rhs=QRT[prt, pr, t1 * 128:(t1 + 1) * 128], start=False, stop=True),
                     reads=[f"krt{t1}", f"qrt{t1}"], writes=[psk])
                pt = ptb[pti % 3]
                ptk = f"ptb{pti % 3}"
                pti += 1
                S.op("dve", lambda e, pt=pt, ps=ps: e.tensor_tensor(out=pt, in0=ps[:, 0:384], in1=self.mask01[:], op=ALU.mult),
                     reads=[psk, "mask01"], writes=[ptk])
                hc = slice(h * 64, h * 64 + 64)
                S.op("pe", lambda e, pt=pt, hc=hc, t0=t0, f0=first[0]: e.matmul(po[0][:, hc], lhsT=pt[:, 0:128], rhs=VTOK[:, t0, hc], start=f0, stop=False),
                     reads=[ptk, f"vtok{t0}"], writes=[pok[0]], signal=False)
                first[0] = False
                S.op("pe", lambda e, hc=hc, pr=pr, prt=prt, t0=t0, h=h: e.matmul(po[0][:, hc], lhsT=QRT[prt, pr, t0 * 128:(t0 + 1) * 128],
                                                                              rhs=self.stbf[prt, pr, :], start=False, stop=(h == 5)),
                     reads=[f"qrt{t0}", "stbf"], writes=[pok[0]], signal=(h == 5))
                S.op("pe", lambda e, pt=pt, hc=hc, t0=t0, f1=first[1]: e.matmul(po[1][:, hc], lhsT=pt[:, 128:256], rhs=VTOK[:, t0, hc], start=f1, stop=False),
                     reads=[ptk, f"vtok{t0}"], writes=[pok[1]], signal=False)
                first[1] = False
                S.op("pe", lambda e, pt=pt, hc=hc, t1=t1: e.matmul(po[1][:, hc], lhsT=pt[:, 256:384], rhs=VTOK[:, t1, hc], start=False, stop=False),
                     reads=[ptk, f"vtok{t1}"], writes=[pok[1]], signal=False)
                S.op("pe", lambda e, hc=hc, pr=pr, prt=prt, t1=t1, h=h: e.matmul(po[1][:, hc], lhsT=QRT[prt, pr, t1 * 128:(t1 + 1) * 128],
                                                                              rhs=self.stbf[prt, pr, :], start=False, stop=(h == 5)),
                     reads=[f"qrt{t1}", "stbf"], writes=[pok[1]], signal=(h == 5))
            for ci, t in enumerate((t0, t1)):
                pv = po[ci][:, 0:384].rearrange("p (h d) -> p h d", h=6)
                S.op("act", lambda e, pv=pv: e.activation(out=ysq, in_=pv, func=AF.Square), reads=[pok[ci]], writes=["ysq"])
                S.op("dve", lambda e: e.tensor_reduce(out=self.ssq[:], in_=ysq, axis=mybir.AxisListType.X, op=ALU.add), reads=["ysq"], writes=["ssq"])
                S.op("dve", lambda e: e.tensor_scalar(out=rs6, in0=self.ssq[:], scalar1=1.0 / 64, scalar2=EPS, op0=ALU.mult, op1=ALU.add),
                     reads=["ssq"], writes=["rs6"])
                S.op("act", lambda e: e.activation(out=rs6, in_=rs6, func=AF.Sqrt), reads=["rs6"], writes=["rs6"])
                S.op("dve", lambda e: e.reciprocal(out=rs6, in_=rs6), reads=["rs6"], writes=["rs6"])
                yt = ytok[t % 2]
                ytk = f"ytok{t % 2}"
                rgv = RG[:, t, :].rearrange("p (h d) -> p h d", h=6)
                ytv = yt.rearrange("p (h d) -> p h d", h=6)
                for h in range(6):
                    S.op("dve", lambda e, h=h, pv=pv, ytv=ytv, rgv=rgv: e.scalar_tensor_tensor(
                        out=ytv[:, h, :], in0=pv[:, h, :], scalar=rs6[:, h:h + 1], in1=rgv[:, h, :], op0=ALU.mult, op1=ALU.mult),
                        reads=[pok[ci], "rs6", f"rg{t}"], writes=[ytk], signal=(h == 5))
                pb = self.psb[t % 2]
                for pr in range(3):
                    S.op("pe", lambda e, pr=pr, pb=pb, yt=yt: e.transpose(out=pb[:, pr * 128:(pr + 1) * 128], in_=yt[:, pr * 128:(pr + 1) * 128],
                                                                         identity=self.identb[:]),
                         reads=[ytk, "identb"], writes=[f"psb{t % 2}"], signal=(pr == 2))
                self.copy(self.cp_eng(), MIXT[:, 0:3, t * 128:(t + 1) * 128], pb[:, 0:384].rearrange("p (a b) -> p a b", a=3),
                          reads=[f"psb{t % 2}"], writes=[f"mixT{t}"])
            S.op("dve", lambda e, ch=ch: e.tensor_tensor(out=self.state[:], in0=self.state[:], in1=KVS[:, ch, :, :], op=ALU.add),
                 reads=["state", f"kvs{ch}a", f"kvs{ch}b"], writes=["state"])
            S.op("dve", lambda e: e.tensor_tensor(out=self.state[:], in0=self.state[:], in1=dec, op=ALU.mult), reads=["state", "cf"], writes=["state"])
        if "mixret" in self.taps and l == 0:
            S.barrier()
            self.tap_mixT("mixret")
        S.barrier()
        if self.stop == "ret":
            return
        self.conv(l)
        S.barrier()
        if self.stop == "conv":
            return
        self.moba(l)
        S.barrier()
        if self.stop == "moba":
            return
        if "mixT" in self.taps and l == 0:
            self.tap_mixT("mixT")
            S.barrier()
        WOUT = self.carve(A_WOUT, [128, 8, D_MODEL], BF16)
        S.dma("pool", lambda e: e.dma_start(out=WOUT, in_=self.w["w_out"][l].rearrange("(k p) f -> p k f", p=128)),
              writes=["wout", "gain"] + [f"vimg{t}" for t in range(NT)])
        S.dma("sp", lambda e: e.dma_start(out=X, in_=self.xsp.rearrange("p (t f) -> p t f", t=NT)), reads=["xsp"], writes=xkeys)
        for t in range(NT):
            for hf in range(2):
                pd = self.psf[4 + (2 * t + hf) % 2]
                kd = f"psf{4 + (2 * t + hf) % 2}"
                for kc in range(8):
                    S.op("pe", lambda e, kc=kc, t=t, hf=hf, pd=pd: e.matmul(pd[:], lhsT=MIXT[:, kc, t * 128:(t + 1) * 128],
                                                                         rhs=WOUT[:, kc, hf * 512:(hf + 1) * 512], start=(kc == 0), stop=(kc == 7)),
                         reads=["wout"], writes=[kd], signal=(kc == 7))
                S.op("dve", lambda e, t=t, hf=hf, pd=pd: e.tensor_tensor(out=X[:, t, hf * 512:(hf + 1) * 512], in0=pd[:],
                                                                       in1=X[:, t, hf * 512:(hf + 1) * 512], op=ALU.add),
                     reads=[kd, f"x{t}"], writes=[f"x{t}"])

    def tap_mixT(self, name):
        S = self.S
        MIXT = self.carve(A_HT, [128, 8, TOK], BF16)
        tmp = self.carve(A_CONVTMP, [128, TOK], F32)
        for kc in range(8):
            S.op("dve", lambda e, kc=kc: e.tensor_copy(out=tmp, in_=MIXT[:, kc, :]), writes=["taptmp"])
            S.dma("sp", lambda e, kc=kc: e.dma_start(out=self.tapouts[name][:, kc * TOK:(kc + 1) * TOK], in_=tmp), reads=["taptmp"],
                  writes=["tap_" + name + str(kc)])
        S.barrier()

    def conv(self, l):
        S = self.S
        UT = self.carve(A_UT, [128, 2, 2080], BF16)
        MIXT = self.carve(A_HT, [128, 8, TOK], BF16)
        sel = self.cfv("sel")
        identf = self.cfv("identf")
        bd = self.cfv("bd")
        tl = self.tails[:]
        S.op("dve", lambda e: e.tensor_scalar(out=self.halo[:], in0=tl[:, 0, :], scalar1=sel[:, 0:1], scalar2=None, op0=ALU.mult),
             reads=["tails", "cf"], writes=["halo"])
        for r_ in range(1, 4):
            S.op("dve", lambda e, r_=r_: e.scalar_tensor_tensor(out=self.halo[:], in0=tl[:, r_, :], scalar=sel[:, r_:r_ + 1], in1=self.halo[:],
                                                               op0=ALU.mult, op1=ALU.add), reads=["tails", "cf", "halo"], writes=["halo"])
        S.op("dve", lambda e: e.tensor_copy(out=UT[:, :, 2:32], in_=self.halo[:].rearrange("p (a b) -> p a b", a=2)), reads=["halo"], writes=["ut_halo"])
        diag = [self.carve(A_CONVTMP + 256 * i, [128, 128], BF16) for i in range(4)]
        ysb = self.carve(A_CONVTMP + 1024, [128, 512], F32)
        ysq = self.carve(A_CONVTMP + 3072, [128, 512], F32)
        msb = self.carve(A_CONVTMP + 5120, [128, 512], F32)
        var = self.carve(A_CONVTMP + 7168, [128, 512], F32)
        dj = 0
        for c in range(2):
            for j in range(31):
                dg = diag[dj % 4]
                dk = f"diag{dj % 4}"
                dj += 1
                S.op("dve", lambda e, dg=dg, c=c, j=j: e.tensor_scalar(out=dg, in0=identf, scalar1=self.cpar[:, c, j:j + 1], scalar2=None, op0=ALU.mult),
                     reads=["cf", "cpar"], writes=[dk])
                for tg in range(4):
                    S.op("pe", lambda e, dg=dg, c=c, j=j, tg=tg: e.matmul(self.psf[tg][:], lhsT=dg, rhs=UT[:, c, 2 + tg * 512 + j:2 + tg * 512 + j + 512],
                                                                         start=(j == 0), stop=(j == 30)),
                         reads=[dk, f"ut{c}_{tg}", "ut_halo"] + ([f"ut{c}_{tg - 1}"] if tg > 0 else []), writes=[f"psf{tg}"], signal=(tg == 3))
            for tg in range(4):
                pk = f"psf{tg}"
                S.op("act", lambda e, tg=tg, c=c: e.activation(out=ysb, in_=self.psf[tg][:], func=AF.Identity, bias=self.cpar[:, c, 31:32], scale=1.0),
                     reads=[pk, "cpar"], writes=["c_ysb"])
                S.op("act", lambda e: e.activation(out=ysq, in_=ysb, func=AF.Square), reads=["c_ysb"], writes=["c_ysq"])
                S.op("pe", lambda e: e.matmul(self.psf[4][:], lhsT=bd, rhs=ysb, start=True, stop=True), reads=["cf", "c_ysb"], writes=["psf4"])
                S.op("pe", lambda e: e.matmul(self.psf[5][:], lhsT=bd, rhs=ysq, start=True, stop=True), reads=["cf", "c_ysq"], writes=["psf5"])
                S.op("act", lambda e: e.activation(out=msb, in_=self.psf[4][:], func=AF.Copy), reads=["psf4"], writes=["c_msb"])
                S.op("dve", lambda e: e.tensor_tensor(out=var, in0=msb, in1=msb, op=ALU.mult), reads=["c_msb"], writes=["c_var"])
                S.op("dve", lambda e: e.tensor_tensor(out=var, in0=self.psf[5][:], in1=var, op=ALU.subtract), reads=["psf5", "c_var"], writes=["c_var"])
                S.op("dve", lambda e: e.tensor_scalar(out=var, in0=var, scalar1=EPS, scalar2=0.0, op0=ALU.add, op1=ALU.max), reads=["c_var"], writes=["c_var"])
                S.op("act", lambda e: e.activation(out=var, in_=var, func=AF.Sqrt), reads=["c_var"], writes=["c_var"])
                S.op("dve", lambda e: e.reciprocal(out=var, in_=var), reads=["c_var"], writes=["c_var"])
                S.op("dve", lambda e: e.tensor_tensor(out=ysb, in0=ysb, in1=msb, op=ALU.subtract), reads=["c_ysb", "c_msb"], writes=["c_ysb"])
                S.op("dve", lambda e: e.tensor_tensor(out=ysb, in0=ysb, in1=var, op=ALU.mult), reads=["c_ysb", "c_var"], writes=["c_ysb"])
                S.op("dve", lambda e, c=c: e.tensor_scalar(out=ysb, in0=ysb, scalar1=self.cpar[:, c, 32:33], scalar2=self.cpar[:, c, 33:34],
                                                          op0=ALU.mult, op1=ALU.add), reads=["c_ysb", "cpar"], writes=["c_ysb"])
                S.op("act", lambda e, c=c, tg=tg: e.activation(out=MIXT[:, 6 + c, tg * 512:(tg + 1) * 512], in_=ysb, func=AF.Silu),
                     reads=["c_ysb"], writes=[f"mixTc{c}_{tg}"])

    def moba(self, l):
        S = self.S
        MQT = self.carve(A_MQT, [128, 3, TOK], BF16)
        QAUG = self.carve(A_QAUG, [96, 6, TOK], BF16)
        MIXT = self.carve(A_HT, [128, 8, TOK], BF16)
        KT, VR = self.KT, self.VR
        onesf = self.cfv("onesf")
        cap = self.cfv("cap").rearrange("p (a b) -> p a b", a=8)
        pastneg = self.cfv("pastneg").rearrange("p (a b) -> p a b", a=8)
        futneg = self.cfv("futneg").rearrange("p (a b) -> p a b", a=8)
        for i in range(4):
            S.dma("pool", lambda e, i=i: e.dma_start(out=KT[i][64:96, :], in_=self.er_in[i]), writes=[f"kt{i}"])
            S.op("dve", lambda e, i=i: e.memset(VR[i][:, :, 64:66], 1.0), writes=[f"vr{i}"])
        for h in range(6):
            S.dma("sp", lambda e, h=h: e.dma_start(out=QAUG[0:64, h, :], in_=MQT[(h % 2) * 64:(h % 2) * 64 + 64, h // 2, :]),
                  reads=[f"mqt{t}" for t in range(NT)], writes=[f"qaug{h}"])
        S.op("dve", lambda e: e.tensor_copy(out=self.kmhi[:], in_=self.kmall[:]), reads=["kmall"], writes=["kmhi"])
        S.op("dve", lambda e: e.tensor_copy(out=self.kmt[:], in_=self.kmhi[:]), reads=["kmhi"], writes=["kmt"])
        S.op("dve", lambda e: e.tensor_tensor(out=self.kmt[:], in0=self.kmall[:], in1=self.kmt[:], op=ALU.subtract), reads=["kmall", "kmt"], writes=["kmt"])
        S.op("dve", lambda e: e.tensor_copy(out=self.kmlo[:], in_=self.kmt[:]), reads=["kmt"], writes=["kmlo"])
        gi = 0
        for h in range(6):
            for tg in range(4):
                pb = self.psb[(h * 4 + tg) % 2]
                pbk = f"psb{(h * 4 + tg) % 2}"
                for tt in range(4):
                    t = 4 * tg + tt
                    b = t // 2
                    pg = self.psf[gi % 2]
                    pgk = f"psf{gi % 2}"
                    g1, m8, t1, mt = self.g1[gi % 2], self.m8[gi % 2], self.t1[gi % 2], self.mtpad[gi % 2]
                    k_ = gi % 2
                    gi += 1
                    S.op("pe", lambda e, pg=pg, h=h, t=t: e.matmul(pg[:, 0:32], lhsT=QAUG[0:64, h, t * 128:(t + 1) * 128], rhs=self.kmhi[:, h, :],
                                                                 start=True, stop=False), reads=[f"qaug{h}", "kmhi"], writes=[pgk], signal=False)
                    S.op("pe", lambda e, pg=pg, h=h, t=t: e.matmul(pg[:, 0:32], lhsT=QAUG[0:64, h, t * 128:(t + 1) * 128], rhs=self.kmlo[:, h, :],
                                                                 start=False, stop=True), reads=[f"qaug{h}", "kmlo"], writes=[pgk])
                    S.op("dve", lambda e, pg=pg, g1=g1, b=b: e.tensor_tensor(out=g1[:], in0=pg[:, 0:32], in1=cap[:, b, :], op=ALU.min),
                         reads=[pgk, "cf"], writes=[f"g1_{k_}"])
                    S.op("dve", lambda e, g1=g1, m8=m8: e.max(out=m8[:], in_=g1[:]), reads=[f"g1_{k_}"], writes=[f"m8_{k_}"])
                    S.op("dve", lambda e, g1=g1, m8=m8, t1=t1, b=b: e.scalar_tensor_tensor(out=t1[:], in0=g1[:], scalar=m8[:, 2:3], in1=pastneg[:, b, :],
                                                                                       op0=ALU.is_lt, op1=ALU.mult),
                         reads=[f"g1_{k_}", f"m8_{k_}", "cf"], writes=[f"t1_{k_}"])
                    S.op("dve", lambda e, t1=t1, mt=mt, b=b: e.tensor_tensor(out=mt[:, 64:96], in0=t1[:], in1=futneg[:, b, :], op=ALU.add),
                         reads=[f"t1_{k_}", "cf"], writes=[f"mtpad{k_}"])
                    S.op("pe", lambda e, mt=mt, pb=pb, tt=tt: e.transpose(out=pb[0:96, tt * 128:(tt + 1) * 128], in_=mt[:], identity=self.identb[:]),
                         reads=[f"mtpad{k_}", "identb"], writes=[pbk], signal=(tt == 3))
                self.copy(self.cp_eng(), QAUG[64:96, h, tg * 512:(tg + 1) * 512], pb[64:96, 0:512], reads=[pbk], writes=[f"qaug{h}"])
        PT = [self.carve(A_TMP + 1024 * i, [128, 512], BF16) for i in range(3)]
        osb = self.carve(A_TMP + 3072, [128, 512], F32)
        rec = self.carve(A_TMP + 5120, [128, 512], F32)
        otmp = self.carve(A_TMP + 7168, [128, 512], BF16)
        for h in range(6):
            steps = []
            for i in range(4):
                S.dma("sp", lambda e, i=i, h=h: e.dma_start(
                    out=KT[i][0:64, :], in_=self.ccK_out[h // 2][i * 128 + (h % 2) * 64:i * 128 + (h % 2) * 64 + 64, :]),
                    reads=[f"ccK_out{h // 2}"], writes=[f"kt{i}"])
                S.dma("sp", lambda e, i=i, h=h: e.dma_start(
                    out=VR[i][:, :, 0:64],
                    in_=self.ccV_out[h // 2][i * 128:i * 128 + 128, :].rearrange("p (t c) -> p t c", c=128)[:, :, (h % 2) * 64:(h % 2) * 64 + 64]),
                    reads=[f"ccV_out{h // 2}"], writes=[f"vr{i}"])
                for t in range(NT):
                    for g in range(4):
                        steps.append((i, t, g))
            ns = len(steps)

            def emit_s(k):
                i, t, g = steps[k]
                ps = self.psf[4 + k % 2]
                need_c = (4 * g <= t < 4 * g + 4)
                S.op("pe", lambda e, ps=ps, i=i, t=t, g=g, h=h: e.matmul(ps[:], lhsT=KT[i][0:96, t * 128:(t + 1) * 128], rhs=QAUG[:, h, g * 512:(g + 1) * 512],
                                                                       start=True, stop=not need_c),
                     reads=[f"kt{i}", f"qaug{h}"], writes=[f"psf{4 + k % 2}"], signal=not need_c)
                if need_c:
                    S.op("pe", lambda e, ps=ps, i=i, t=t, g=g: e.matmul(ps[:], lhsT=self.identR[:, i, :], rhs=self.cmask[:, t - 4 * g, :], start=False, stop=True),
                         reads=["identR", "cmask"], writes=[f"psf{4 + k % 2}"])

            def emit_pv(k):
                i, t, g = steps[k]
                ps = self.psf[4 + k % 2]
                pt = PT[k % 3]
                S.op("act", lambda e, ps=ps, pt=pt: e.activation(out=pt, in_=ps[:], func=AF.Exp, scale=0.125), reads=[f"psf{4 + k % 2}"], writes=[f"pt{k % 3}"])
                S.op("pe", lambda e, pt=pt, i=i, t=t, g=g: e.matmul(self.psf[g][0:65, :], lhsT=VR[i][:, t, 0:65], rhs=pt,
                                                                  start=(i == 0 and t == 0), stop=(i == 3 and t == NT - 1)),
                     reads=[f"pt{k % 3}", f"vr{i}"], writes=[f"psf{g}"], signal=True)

            emit_s(0)
            for k in range(ns):
                if k + 1 < ns:
                    emit_s(k + 1)
                emit_pv(k)
            for g in range(4):
                po = self.psf[g]
                S.op("act", lambda e, po=po: e.activation(out=osb[0:65, :], in_=po[0:65, :], func=AF.Copy), reads=[f"psf{g}"], writes=["osb"])
                S.op("dve", lambda e: e.reciprocal(out=rec[64:65, :], in_=osb[64:65, :]), reads=["osb"], writes=["rec"])
                pr_ = self.psf[4 + g % 2]
                prk = f"psf{4 + g % 2}"
                S.op("pe", lambda e, pr_=pr_: e.matmul(pr_[0:64, :], lhsT=onesf[64:65, 0:64], rhs=rec[64:65, :], start=True, stop=True),
                     reads=["cf", "rec"], writes=[prk])
                ch = 3 + h // 2
                if h % 2 == 0:
                    S.op("dve", lambda e, pr_=pr_, g=g, ch=ch: e.tensor_tensor(out=MIXT[0:64, ch, g * 512:(g + 1) * 512], in0=osb[0:64, :], in1=pr_[0:64, :], op=ALU.mult),
                         reads=["osb", prk], writes=[f"mixTm{h}_{g}"])
                else:
                    S.op("dve", lambda e, pr_=pr_: e.tensor_tensor(out=otmp[0:64, :], in0=osb[0:64, :], in1=pr_[0:64, :], op=ALU.mult),
                         reads=["osb", prk], writes=["otmp"])
                    S.dma("sp", lambda e, g=g, ch=ch: e.dma_start(out=MIXT[64:128, ch, g * 512:(g + 1) * 512], in_=otmp[0:64, :]),
                          reads=["otmp"], writes=[f"mixTm{h}_{g}"])

    def final(self):
        S = self.S
        S.barrier()
        X = self.carve(A_X, [128, NT, D_MODEL], F32)
        gain = self.carve(A_GAIN, [128, D_MODEL], F32)
        S.dma("sp", lambda e: e.dma_start(out=gain, in_=self.fnorm[0:1, :].to_broadcast([128, D_MODEL])), writes=["gain"])
        self.rms_stats()
        yo = self.y_out.rearrange("(t p) f -> p t f", p=128)
        ob = [self.carve(A_HT + 4096 * i, [128, D_MODEL], F32) for i in range(2)]
        for t in range(NT):
            o = ob[t % 2]
            S.op("dve", lambda e, t=t, o=o: e.scalar_tensor_tensor(out=o, in0=X[:, t, :], scalar=self.rstd[:, t:t + 1], in1=gain, op0=ALU.mult, op1=ALU.mult),
                 reads=[f"x{t}", "rstd", "gain"], writes=[f"fo{t % 2}"])
            S.dma("sp", lambda e, t=t, o=o: e.dma_start(out=yo[:, t, :], in_=o), reads=[f"fo{t % 2}"], writes=[f"y{t}"])


_PROG_CACHE = {}


def _get_prog(depth, do_final, taps=(), stop=None):
    key = (depth, do_final, tuple(taps), stop)
    if key not in _PROG_CACHE:
        p = Prog(depth, do_final, taps, stop)
        p.build()
        _PROG_CACHE[key] = p
    return _PROG_CACHE[key]


def _layer_inputs(inp, l0, l1):
    f32 = np.float32
    sl = slice(l0, l1)
    d = {}
    for f in (1, 2):
        d[f"wg{f}"] = np.ascontiguousarray(inp[f"ffn{f}_wg"][sl], f32)
        d[f"wu{f}"] = np.ascontiguousarray(inp[f"ffn{f}_wu"][sl], f32)
        d[f"wd{f}"] = np.ascontiguousarray(inp[f"ffn{f}_wd"][sl], f32)
    d["w_in"] = np.ascontiguousarray(inp["w_in"][sl], f32)
    d["w_out"] = np.ascontiguousarray(inp["w_out"][sl], f32)
    d["norms"] = np.ascontiguousarray(np.stack([inp["ffn1_norm"][sl], inp["mix_norm"][sl], inp["ffn2_norm"][sl]], 1), f32)
    d["fnorm"] = np.ascontiguousarray(inp["final_norm"], f32).reshape(1, D_MODEL)
    nl = l1 - l0
    cp = np.zeros((nl, 128, 2, 34), f32)
    cw = np.asarray(inp["conv_w"][sl], f32)
    cp[:, :, :, 0:31] = cw.transpose(0, 2, 1).reshape(nl, 2, 128, 31).transpose(0, 2, 1, 3)
    for j, nm in ((31, "conv_b"), (32, "conv_ln_g"), (33, "conv_ln_b")):
        cp[:, :, :, j] = np.asarray(inp[nm][sl], f32).reshape(nl, 2, 128).transpose(0, 2, 1)
    d["convp"] = cp
    return d


def _run(inp, xs, l0, l1, do_final, taps=(), stop=None):
    prog = _get_prog(l1 - l0, do_final, taps, stop)
    shared = _layer_inputs(inp, l0, l1)
    pos = np.asarray(inp["positions"], np.int32)
    in_maps = []
    for c in range(NCORES):
        b, r = c // 4, c % 4
        cf, cm, er = _host_consts(r)
        m = dict(shared)
        m["x"] = np.ascontiguousarray(xs[c], np.float32)
        m["pos"] = np.ascontiguousarray(pos[b, r * TOK:(r + 1) * TOK].reshape(NT, 128).T)
        m["cf"] = cf
        m["cmask"] = cm
        m["erows"] = er
        in_maps.append(m)
    res = run_bass_kernel_spmd(prog.nc, in_maps, core_ids=list(range(NCORES)))
    return res.results


FUSED = True


def kernel(**inputs):
    inp = {k: np.asarray(v) for k, v in inputs.items()}
    x = np.asarray(inp["x"], np.float32)
    xs = [x[c // 4, (c % 4) * TOK:(c % 4 + 1) * TOK] for c in range(NCORES)]
    if FUSED:
        res = _run(inp, xs, 0, DEPTH, True)
        xs = [r["y"] for r in res]
    else:
        for l in range(DEPTH):
            res = _run(inp, xs, l, l + 1, l == DEPTH - 1)
            xs = [r["y"] for r in res]
    out = np.zeros((2, SEQ, D_MODEL), np.float32)
    for c in range(NCORES):
        out[c // 4, (c % 4) * TOK:(c % 4 + 1) * TOK] = xs[c]
    return out
```

```python
import math
from contextlib import ExitStack

import numpy as np
import concourse.bass as bass
import concourse.mybir as mybir
from concourse.bass_utils import run_bass_kernel_spmd

F32 = mybir.dt.float32
BF16 = mybir.dt.bfloat16
I32 = mybir.dt.int32
ALU = mybir.AluOpType
AF = mybir.ActivationFunctionType

D_MODEL = 1024
SEQ = 8192
DEPTH = 4
D_FF = 2816
IN_W = 3200
NCORES = 8
TOK = 2048
NT = 16
NEG = -30000.0
EPS = 1e-6

ENGS = ("pe", "act", "dve", "pool", "sp")
NDSEM = 8


class Sched:
    def __init__(self, nc):
        self.nc = nc
        self.q = {e: [] for e in ENGS}
        self.sig = {e: 0 for e in ENGS}
        self.dcnt = {e: 0 for e in ENGS}
        self.ccnt = 0
        self.seen = {e: {} for e in ENGS}
        self.issued = {}
        self.lastw = {}
        self.readers = {}

    def _deps(self, eng, reads, writes):
        toks = []
        for b in reads:
            t = self.lastw.get(b)
            if t is not None:
                toks.append(t)
        for b in writes:
            t = self.lastw.get(b)
            if t is not None:
                toks.append(t)
            toks.extend(self.readers.get(b, ()))
        need = {}
        for (sk, v, e) in toks:
            if e == eng and sk[0] == "c":
                if eng == "pe" or v > self.sig[eng]:
                    continue
            if self.seen[eng].get(sk, 0) >= v:
                continue
            if need.get(sk, 0) < v:
                need[sk] = v
        for sk, v in need.items():
            self.seen[eng][sk] = v
        return list(need.items())

    def _commit(self, tok, reads, writes):
        for b in reads:
            self.readers.setdefault(b, []).append(tok)
        for b in writes:
            self.lastw[b] = tok
            self.readers[b] = []

    def op(self, eng, fn, reads=(), writes=(), signal=True):
        waits = self._deps(eng, reads, writes)
        if signal:
            self.sig[eng] += 1
            val = self.sig[eng]
            self.issued[("c", eng)] = val
        else:
            val = self.sig[eng] + 1
        tok = (("c", eng), val, eng)
        self.q[eng].append(("op", waits, fn, signal, None))
        self._commit(tok, reads, writes)
        return tok

    def dma(self, eng, fn, reads=(), writes=()):
        waits = self._deps(eng, reads, writes)
        i = self.dcnt[eng]
        self.dcnt[eng] += 1
        sk = ("d", eng, i % NDSEM)
        val = 16 * (i // NDSEM + 1)
        if val > 16 and self.seen[eng].get(sk, 0) < val - 16:
            waits.append((sk, val - 16))
            self.seen[eng][sk] = val - 16
        tok = (sk, val, eng)
        self.issued[sk] = val
        self.q[eng].append(("dma", waits, fn, True, sk))
        self._commit(tok, reads, writes)
        return tok

    def cc(self, fn, reads=(), writes=()):
        eng = "pool"
        waits = self._deps(eng, reads, writes)
        self.ccnt += 1
        sk = ("x", "cc")
        if self.ccnt > 1 and self.seen[eng].get(sk, 0) < self.ccnt - 1:
            waits.append((sk, self.ccnt - 1))
            self.seen[eng][sk] = self.ccnt - 1
        tok = (sk, self.ccnt, eng)
        self.issued[sk] = self.ccnt
        self.q[eng].append(("cc", waits, fn, True, sk))
        self._commit(tok, reads, writes)
        return tok

    def barrier(self, engines=ENGS):
        for e in engines:
            waits = []
            for sk, v in self.issued.items():
                if sk == ("c", e):
                    continue
                if self.seen[e].get(sk, 0) < v:
                    waits.append((sk, v))
                    self.seen[e][sk] = v
            self.q[e].append(("wait", waits, None, False, None))

    def wait_all(self, eng, bufs):
        waits = self._deps(eng, bufs, ())
        self.q[eng].append(("wait", waits, None, False, None))

    def emit(self):
        nc = self.nc
        used = []
        seen = set()
        for e in ENGS:
            for (kind, waits, fn, signal, sk) in self.q[e]:
                keys = [wk for (wk, v) in waits]
                if kind in ("dma", "cc"):
                    keys.append(sk)
                elif kind == "op" and signal:
                    keys.append(("c", e))
                for k in keys:
                    if k not in seen:
                        seen.add(k)
                        used.append(k)
        with ExitStack() as st:
            sems = {}
            for s in used:
                sems[s] = st.enter_context(nc.semaphore("s_" + "_".join(str(x) for x in s)))
            block = st.enter_context(nc.Block())

            def runner(e):
                def run(engobj):
                    for (kind, waits, fn, signal, sk) in self.q[e]:
                        for (wk, v) in waits:
                            engobj.wait_ge(sems[wk], v)
                        if kind == "op":
                            ins = fn(engobj)
                            if signal:
                                ins.then_inc(sems[("c", e)], 1)
                        elif kind == "dma":
                            fn(engobj).then_inc(sems[sk], 16)
                        elif kind == "cc":
                            fn(engobj).then_inc(sems[sk], 1)
                return run

            block.tensor(runner("pe"))
            block.scalar(runner("act"))
            block.vector(runner("dve"))
            block.gpsimd(runner("pool"))
            block.sync(runner("sp"))


CF = {}
_off = 0
for _name, _w in [("identf", 128), ("bd", 128), ("onesf", 128), ("invr", 32), ("invm", 8),
                  ("qs", 12), ("ks", 12), ("dec", 3), ("coef", 12), ("cap", 256),
                  ("pastneg", 256), ("futneg", 256), ("sel", 4), ("c256", 1), ("zero", 1),
                  ("mask01", 384), ("identR", 512)]:
    CF[_name] = (_off, _w)
    _off += _w
NCF = _off
NCS = CF["mask01"][0]


def _host_consts(rank):
    cf = np.zeros((128, NCF), np.float32)

    def put(name, arr):
        o, w = CF[name]
        cf[:, o:o + w] = np.asarray(arr, np.float32).reshape(128, w)

    p = np.arange(128)
    put("identf", np.eye(128))
    bd = np.zeros((128, 128))
    bd[:64, :64] = 1.0 / 64
    bd[64:, 64:] = 1.0 / 64
    put("bd", bd)
    put("onesf", np.ones((128, 128)))
    ret_inv = (10000.0 ** (-np.linspace(0.0, 1.0, 32, dtype=np.float32))).astype(np.float32)
    rope_inv = (500000.0 ** (-np.arange(8, dtype=np.float32) / 8)).astype(np.float32)
    put("invr", np.broadcast_to(ret_inv, (128, 32)))
    put("invm", np.broadcast_to(rope_inv, (128, 8)))
    hh = np.arange(6, dtype=np.float64)
    lg = np.log1p(-np.exp2(-5.0 - hh))
    qs = np.zeros((128, 2, 6))
    ks = np.zeros((128, 2, 6))
    for par in range(2):
        c = par * 128 + p
        qs[:, par] = np.exp(lg[None, :] * (c[:, None] + 1.0))
        ks[:, par] = np.exp(-lg[None, :] * (c[:, None] + 1.0)) * 0.125
    put("qs", qs)
    put("ks", ks)
    hd = np.zeros((128, 3), np.int64)
    for pr in range(3):
        hd[:64, pr] = 2 * pr
        hd[64:, pr] = 2 * pr + 1
    dec = np.exp(lg[hd] * 256.0)
    put("dec", dec)
    coef = np.zeros((128, 4, 3))
    for i in range(4):
        if i < rank:
            coef[:, i] = np.exp(lg[hd] * 2048.0 * (rank - 1 - i))
    put("coef", coef)
    cap = np.zeros((128, 8, 32))
    pastneg = np.zeros((128, 8, 32))
    futneg = np.zeros((128, 8, 32))
    n = np.arange(32)
    for b in range(8):
        own = 8 * rank + b
        cap[:, b] = np.where(n < own, 3.0e38, -1.0e9)[None, :]
        pastneg[:, b] = np.where(n < own, NEG, 0.0)[None, :]
        futneg[:, b] = np.where(n > own, NEG, 0.0)[None, :]
    put("cap", cap)
    put("pastneg", pastneg)
    put("futneg", futneg)
    sel = np.zeros((128, 4))
    if rank > 0:
        sel[:, rank - 1] = 1.0
    put("sel", sel)
    tri = (p[:, None] <= p[None, :]).astype(np.float32)
    put("mask01", np.concatenate([tri, np.ones((128, 128)), tri], 1))
    idr = np.zeros((128, 4, 128))
    idr[:, rank] = np.eye(128)
    put("identR", idr)
    put("c256", np.full((128, 1), 1.0 / 256))
    cm = np.zeros((128, 4, 512), np.float32)
    q = np.arange(512)
    for t in range(4):
        cm[:, t] = np.where((t * 128 + p)[:, None] > q[None, :], NEG, 0.0)
    er = np.zeros((4, 32, 2048), np.float32)
    key = np.arange(2048)
    for i in range(4):
        er[i, 8 * i + key // 256, key] = 1.0
    return cf, cm.reshape(128, 2048), er


ARENA = 176 * 1024
A_X = 0
A_HT = 65536
A_ACT = 98304
A_WD = 131072
A_WGU = 147456
A_GAIN = 159744
A_TMP = 163840
A_QRT = 0
A_KRT = 12288
A_VTOK = 24576
A_RG = 36864
A_MQT = 49152
A_MKT = 98304
A_KVS = 110592
A_UT = 116736
A_WIN = 125056
A_CW = 137344
A_WOUT = 145536
A_QAUG = 0
A_KT = 24576
A_CONVTMP = 0
A_VR = 125056


class Prog:
    def __init__(self, depth, do_final, taps=(), stop=None):
        self.stop = stop
        self.D = depth
        self.do_final = do_final
        self.taps = taps
        self.nc = bass.Bass("TRN2", target_bir_lowering=False)
        self.S = Sched(self.nc)
        self.tapouts = {}

    def carve(self, off, shape, dt):
        n = int(np.prod(shape[1:]))
        sz = 2 if dt == BF16 else 4
        assert off % 4 == 0 and off + n * sz <= ARENA, (off, shape)
        v = self.arena[:, off // 2: off // 2 + n * sz // 2]
        if dt != BF16:
            v = v.bitcast(dt)
        if len(shape) == 3:
            v = v.rearrange("p (a b) -> p a b", a=shape[1])
        elif len(shape) == 4:
            v = v.rearrange("p (a b c) -> p a b c", a=shape[1], b=shape[2])
        if shape[0] != 128:
            v = v[0:shape[0]]
        return v

    def cfv(self, name):
        o, w = CF[name]
        return self.cf[:, o:o + w]

    def build(self):
        nc, S, D = self.nc, self.S, self.D
        dram = lambda name, shape, dt, kind: nc.dram_tensor(name, shape, dt, kind=kind).ap()
        self.x_in = dram("x", [TOK, D_MODEL], F32, "ExternalInput")
        self.pos_in = dram("pos", [128, NT], I32, "ExternalInput")
        self.w = {}
        for f in (1, 2):
            self.w[f"wg{f}"] = dram(f"wg{f}", [D, D_MODEL, D_FF], F32, "ExternalInput")
            self.w[f"wu{f}"] = dram(f"wu{f}", [D, D_MODEL, D_FF], F32, "ExternalInput")
            self.w[f"wd{f}"] = dram(f"wd{f}", [D, D_FF, D_MODEL], F32, "ExternalInput")
        self.w["w_in"] = dram("w_in", [D, D_MODEL, IN_W], F32, "ExternalInput")
        self.w["w_out"] = dram("w_out", [D, D_MODEL, D_MODEL], F32, "ExternalInput")
        self.norms = dram("norms", [D, 3, D_MODEL], F32, "ExternalInput")
        self.fnorm = dram("fnorm", [1, D_MODEL], F32, "ExternalInput")
        self.convp = dram("convp", [D, 128, 2, 34], F32, "ExternalInput")
        self.cf_in = dram("cf", [128, NCF], F32, "ExternalInput")
        self.cm_in = dram("cmask", [128, 2048], F32, "ExternalInput")
        self.er_in = dram("erows", [4, 32, 2048], F32, "ExternalInput")
        self.y_out = dram("y", [TOK, D_MODEL], F32, "ExternalOutput")
        self.xsp = nc.dram_tensor("xsp", [128, NT * D_MODEL], F32).ap()
        self.ccK_in = [nc.dram_tensor(f"ccK_in{j}", [128, 2048], BF16).ap() for j in range(3)]
        self.ccK_out = [nc.dram_tensor(f"ccK_out{j}", [512, 2048], BF16).ap() for j in range(3)]
        self.ccV_in = [nc.dram_tensor(f"ccV_in{j}", [128, 2048], BF16).ap() for j in range(3)]
        self.ccV_out = [nc.dram_tensor(f"ccV_out{j}", [512, 2048], BF16).ap() for j in range(3)]
        self.ccB_in = nc.dram_tensor("ccB_in", [128, 276], F32).ap()
        self.ccB_out = nc.dram_tensor("ccB_out", [512, 276], F32).ap()
        for t in self.taps:
            self.tapouts[t] = dram("tap_" + t, [128, NT * D_MODEL], F32, "ExternalOutput")

        with ExitStack() as st:
            sb = lambda name, shape, dt: st.enter_context(nc.sbuf_tensor(name, shape, dt))
            self.arena = sb("arena", [128, ARENA // 2], BF16)
            self.cf = sb("cf_sb", [128, NCS], F32)
            self.identb = sb("identb", [128, 128], BF16)
            self.mask01 = sb("mask01", [128, 384], BF16)
            self.identR = sb("identR", [128, 4, 128], BF16)
            self.cmask = sb("cmask_sb", [128, 4, 512], BF16)
            self.c256b = sb("c256b", [128, 1], BF16)
            self.cosr = sb("cosr", [128, NT, 32], F32)
            self.sinr = sb("sinr", [128, NT, 32], F32)
            self.cosm = sb("cosm", [128, NT, 8], F32)
            self.sinm = sb("sinm", [128, NT, 8], F32)
            self.posi = sb("posi", [128, NT], I32)
            self.posf = sb("posf", [128, NT], F32)
            self.ms = sb("ms", [128, NT], F32)
            self.rstd = sb("rstd", [128, NT], F32)
            self.cpar = sb("cpar", [128, 2, 34], F32)
            self.state = sb("state", [128, 3, 64], F32)
            self.stbf = sb("stbf", [128, 3, 64], BF16)
            self.kmsb = sb("kmsb", [128, 24], F32)
            self.kmall = sb("kmall", [64, 6, 32], F32)
            self.kmhi = sb("kmhi", [64, 6, 32], BF16)
            self.kmlo = sb("kmlo", [64, 6, 32], BF16)
            self.kmt = sb("kmt", [64, 6, 32], F32)
            self.send = sb("send", [128, 4, 192], F32)
            self.tails = sb("tails", [128, 4, 60], F32)
            self.tail_f = sb("tail_f", [128, 2, 30], F32)
            self.halo = sb("halo", [128, 60], F32)
            self.mtpad = [sb(f"mtpad{i}", [128, 96], BF16) for i in range(2)]
            self.g1 = [sb(f"g1_{i}", [128, 32], F32) for i in range(2)]
            self.m8 = [sb(f"m8_{i}", [128, 8], F32) for i in range(2)]
            self.t1 = [sb(f"t1_{i}", [128, 32], F32) for i in range(2)]
            self.ssq = sb("ssq", [128, 6], F32)
            self.psf = [st.enter_context(nc.psum_tensor(f"psf{i}", [128, 512], F32)) for i in range(6)]
            self.psb = [st.enter_context(nc.psum_tensor(f"psb{i}", [128, 1024], BF16)) for i in range(2)]
            self.tcount = 0
            self.body()
            S.emit()
        return nc

    def cp_eng(self):
        self.tcount += 1
        return "act" if self.tcount % 2 else "dve"

    def copy(self, eng, out, in_, reads, writes):
        if eng == "act":
            self.S.op("act", lambda e: e.activation(out=out, in_=in_, func=AF.Copy), reads=reads, writes=writes)
        else:
            self.S.op(eng, lambda e: e.tensor_copy(out=out, in_=in_), reads=reads, writes=writes)

    def body(self):
        S, nc = self.S, self.nc
        self.setup()
        self.load_x()
        for l in range(self.D):
            self.ffn(l, 1)
            if "x_ffn1" in self.taps and l == 0:
                self.tap_x("x_ffn1")
            if self.stop == "ffn1":
                break
            self.mixer(l)
            if self.stop is not None:
                S.barrier()
                S.dma("sp", lambda e: e.dma_start(out=self.carve(A_X, [128, NT, D_MODEL], F32), in_=self.xsp.rearrange("p (t f) -> p t f", t=NT)),
                      reads=["xsp"], writes=[f"x{t}" for t in range(NT)])
                break
            if "x_mix" in self.taps and l == 0:
                self.tap_x("x_mix")
            self.ffn(l, 2)
        if self.do_final:
            self.final()
        else:
            self.store_x()
        S.barrier(("sp",))

    def tap_x(self, name):
        S = self.S
        X = self.carve(A_X, [128, NT, D_MODEL], F32)
        S.dma("sp", lambda e: e.dma_start(out=self.tapouts[name].rearrange("p (t f) -> p t f", t=NT), in_=X),
              reads=[f"x{t}" for t in range(NT)], writes=["tap_" + name])

    def tap_buf(self, name, view, reads, width):
        S = self.S
        S.dma("sp", lambda e: e.dma_start(out=self.tapouts[name][:, 0:width], in_=view), reads=reads, writes=["tap_" + name])

    def setup(self):
        S = self.S
        S.dma("sp", lambda e: e.dma_start(out=self.cf[:], in_=self.cf_in[:, 0:NCS]), writes=["cf"])
        S.dma("sp", lambda e: e.dma_start(out=self.posi[:], in_=self.pos_in), writes=["posi"])
        o, w = CF["identf"]
        S.dma("pool", lambda e: e.dma_start(out=self.identb[:], in_=self.cf_in[:, o:o + w]), writes=["identb"])
        o2, w2 = CF["mask01"]
        S.dma("pool", lambda e: e.dma_start(out=self.mask01[:], in_=self.cf_in[:, o2:o2 + w2]), writes=["mask01"])
        o3, w3 = CF["identR"]
        S.dma("pool", lambda e: e.dma_start(out=self.identR[:], in_=self.cf_in[:, o3:o3 + w3].rearrange("p (a b) -> p a b", a=4)),
              writes=["identR"])
        S.dma("pool", lambda e: e.dma_start(out=self.cmask[:], in_=self.cm_in.rearrange("p (a b) -> p a b", a=4)), writes=["cmask"])
        S.op("dve", lambda e: e.memset(self.c256b[:], 1.0 / 256), writes=["c256b"])
        self.KT = [self.carve(A_KT + i * 4096, [128, 2048], BF16) for i in range(4)]
        self.VR = [self.carve(A_VR + i * 2112, [128, 16, 66], BF16) for i in range(4)]
        for i in range(2):
            S.op("pool", lambda e, i=i: e.memset(self.mtpad[i][:], 0.0), writes=[f"mtpad{i}"])
        S.op("dve", lambda e: e.tensor_copy(out=self.posf[:], in_=self.posi[:]), reads=["posi"], writes=["posf"])
        tmp = self.carve(A_TMP, [128, NT, 32], F32)
        tmp2 = self.carve(A_TMP + 2048, [128, NT, 32], F32)
        tmpi = self.carve(A_TMP + 4096, [128, NT, 32], I32)
        tmp3 = self.carve(A_TMP + 6144, [128, NT, 32], F32)
        C1 = 6.28125
        C2 = 2 * math.pi - C1
        for (nf, inv, cosT, sinT) in ((32, "invr", self.cosr, self.sinr), (8, "invm", self.cosm, self.sinm)):
            ang, kf, ki, r2 = tmp[:, :, 0:nf], tmp2[:, :, 0:nf], tmpi[:, :, 0:nf], tmp3[:, :, 0:nf]
            invv = self.cfv(inv)
            for t in range(NT):
                S.op("dve", lambda e, t=t, ang=ang, invv=invv: e.tensor_scalar(
                    out=ang[:, t, :], in0=invv, scalar1=self.posf[:, t:t + 1], scalar2=None, op0=ALU.mult),
                    reads=["posf", "cf"], writes=["rt_ang"], signal=(t == NT - 1))
            S.op("dve", lambda e, ang=ang, kf=kf: e.tensor_scalar(out=kf, in0=ang, scalar1=1.0 / (2 * math.pi), scalar2=None, op0=ALU.mult),
                 reads=["rt_ang"], writes=["rt_kf"])
            S.op("dve", lambda e, ki=ki, kf=kf: e.tensor_copy(out=ki, in_=kf), reads=["rt_kf"], writes=["rt_ki"])
            S.op("dve", lambda e, ki=ki, kf=kf: e.tensor_copy(out=kf, in_=ki), reads=["rt_ki"], writes=["rt_kf"])
            S.op("dve", lambda e, ang=ang, kf=kf: e.scalar_tensor_tensor(out=ang, in0=kf, scalar=-C1, in1=ang, op0=ALU.mult, op1=ALU.add),
                 reads=["rt_kf", "rt_ang"], writes=["rt_ang"])
            S.op("dve", lambda e, ang=ang, kf=kf: e.scalar_tensor_tensor(out=ang, in0=kf, scalar=-C2, in1=ang, op0=ALU.mult, op1=ALU.add),
                 reads=["rt_kf", "rt_ang"], writes=["rt_ang"])
            S.op("dve", lambda e, ang=ang: e.tensor_scalar(out=ang, in0=ang, scalar1=math.pi, scalar2=-math.pi, op0=ALU.min, op1=ALU.max),
                 reads=["rt_ang"], writes=["rt_ang"])
            S.op("act", lambda e, ang=ang, sinT=sinT: e.activation(out=sinT[:], in_=ang, func=AF.Sin), reads=["rt_ang"], writes=["rt_sin"])
            S.op("dve", lambda e, ang=ang, kf=kf: e.tensor_scalar(out=kf, in0=ang, scalar1=math.pi / 2, scalar2=math.pi, op0=ALU.add, op1=ALU.is_gt),
                 reads=["rt_ang"], writes=["rt_kf"])
            S.op("dve", lambda e, ang=ang, kf=kf, r2=r2: e.scalar_tensor_tensor(out=r2, in0=kf, scalar=-2 * math.pi, in1=ang, op0=ALU.mult, op1=ALU.add),
                 reads=["rt_kf", "rt_ang"], writes=["rt_r2"])
            S.op("dve", lambda e, r2=r2: e.tensor_scalar(out=r2, in0=r2, scalar1=math.pi / 2, scalar2=math.pi, op0=ALU.add, op1=ALU.min),
                 reads=["rt_r2"], writes=["rt_r2"])
            S.op("act", lambda e, r2=r2, cosT=cosT: e.activation(out=cosT[:], in_=r2, func=AF.Sin), reads=["rt_r2"], writes=["rt_cos"])
        S.barrier()

    def load_x(self):
        S = self.S
        X = self.carve(A_X, [128, NT, D_MODEL], F32)
        xin = self.x_in.rearrange("(t p) f -> p t f", p=128)
        for q in range(4):
            S.dma("sp", lambda e, q=q: e.dma_start(out=X[:, 4 * q:4 * q + 4, :], in_=xin[:, 4 * q:4 * q + 4, :]),
                  writes=[f"x{t}" for t in range(4 * q, 4 * q + 4)])

    def store_x(self):
        S = self.S
        X = self.carve(A_X, [128, NT, D_MODEL], F32)
        yo = self.y_out.rearrange("(t p) f -> p t f", p=128)
        for q in range(4):
            S.dma("sp", lambda e, q=q: e.dma_start(out=yo[:, 4 * q:4 * q + 4, :], in_=X[:, 4 * q:4 * q + 4, :]),
                  reads=[f"x{t}" for t in range(4 * q, 4 * q + 4)], writes=[f"y{q}"])

    def rms_stats(self):
        S = self.S
        X = self.carve(A_X, [128, NT, D_MODEL], F32)
        junk = self.carve(A_TMP + 8192, [128, D_MODEL], BF16)
        for t in range(NT):
            S.op("act", lambda e, t=t: e.activation(out=junk, in_=X[:, t, :], func=AF.Square, accum_out=self.ms[:, t:t + 1]),
                 reads=[f"x{t}"], writes=["junk", "ms"])
        S.op("dve", lambda e: e.tensor_scalar(out=self.rstd[:], in0=self.ms[:], scalar1=1.0 / D_MODEL, scalar2=EPS, op0=ALU.mult, op1=ALU.add),
             reads=["ms"], writes=["rstd"])
        S.op("act", lambda e: e.activation(out=self.rstd[:], in_=self.rstd[:], func=AF.Sqrt), reads=["rstd"], writes=["rstd"])
        S.op("dve", lambda e: e.reciprocal(out=self.rstd[:], in_=self.rstd[:]), reads=["rstd"], writes=["rstd"])

    def norm_to_hT(self, gain_ap):
        S = self.S
        X = self.carve(A_X, [128, NT, D_MODEL], F32)
        HT = self.carve(A_HT, [128, 8, TOK], BF16)
        gain = self.carve(A_GAIN, [128, D_MODEL], F32)
        S.dma("sp", lambda e: e.dma_start(out=gain, in_=gain_ap.to_broadcast([128, D_MODEL])), writes=["gain"])
        self.rms_stats()
        hn = [self.carve(A_TMP + 2048 * i, [128, D_MODEL], BF16) for i in range(2)]
        for t in range(NT):
            h = hn[t % 2]
            S.op("dve", lambda e, t=t, h=h: e.scalar_tensor_tensor(out=h, in0=X[:, t, :], scalar=self.rstd[:, t:t + 1], in1=gain,
                                                                  op0=ALU.mult, op1=ALU.mult),
                 reads=[f"x{t}", "rstd", "gain"], writes=[f"hn{t % 2}"])
            pb = self.psb[t % 2]
            for kc in range(8):
                S.op("pe", lambda e, kc=kc, h=h, pb=pb: e.transpose(out=pb[:, kc * 128:(kc + 1) * 128], in_=h[:, kc * 128:(kc + 1) * 128],
                                                                   identity=self.identb[:]),
                     reads=[f"hn{t % 2}", "identb"], writes=[f"psb{t % 2}"], signal=(kc == 7))
            self.copy(self.cp_eng(), HT[:, :, t * 128:(t + 1) * 128], pb.rearrange("p (a b) -> p a b", a=8),
                      reads=[f"psb{t % 2}"], writes=[f"hT{t}"])

    def ffn(self, l, f):
        S = self.S
        wg, wu, wd = self.w[f"wg{f}"], self.w[f"wu{f}"], self.w[f"wd{f}"]
        if f == 2:
            S.barrier()
        self.norm_to_hT(self.norms[l, (0 if f == 1 else 2):(1 if f == 1 else 3), :])
        X = self.carve(A_X, [128, NT, D_MODEL], F32)
        HT = self.carve(A_HT, [128, 8, TOK], BF16)
        ACT = self.carve(A_ACT, [128, 8, TOK], BF16)
        WD = self.carve(A_WD, [128, 8, D_MODEL], BF16)
        WGU = [self.carve(A_WGU + 4096 * i, [128, 2, 8, 128], BF16) for i in range(3)]
        sg = [self.carve(A_TMP + 4096 + 2048 * i, [128, 512], F32) for i in range(2)]
        wgv = wg[l].rearrange("(k p) f -> p k f", p=128)
        wuv = wu[l].rearrange("(k p) f -> p k f", p=128)
        wdv = wd[l].rearrange("(c p) f -> p c f", p=128)
        cnt = 0
        for (c0, c1) in ((0, 8), (8, 16), (16, 22)):
            ncp = c1 - c0
            S.dma("pool", lambda e, c0=c0, c1=c1, ncp=ncp: e.dma_start(out=WD[:, 0:ncp, :], in_=wdv[:, c0:c1, :]), writes=["wd"])
            for c in range(c0, c1):
                slot = c % 3
                W = WGU[slot]
                S.dma("pool", lambda e, c=c, W=W: e.dma_start(out=W[:, 0, :, :], in_=wgv[:, :, c * 128:(c + 1) * 128]), writes=[f"wgu{slot}g"])
                S.dma("pool", lambda e, c=c, W=W: e.dma_start(out=W[:, 1, :, :], in_=wuv[:, :, c * 128:(c + 1) * 128]), writes=[f"wgu{slot}u"])
                for tg in range(4):
                    pg, pu = self.psf[cnt % 2], self.psf[2 + cnt % 2]
                    kg, ku = f"psf{cnt % 2}", f"psf{2 + cnt % 2}"
                    hk = [f"hT{t}" for t in range(4 * tg, 4 * tg + 4)]
                    for kc in range(8):
                        S.op("pe", lambda e, kc=kc, W=W, pg=pg, tg=tg: e.matmul(pg[:], lhsT=W[:, 0, kc, :], rhs=HT[:, kc, tg * 512:(tg + 1) * 512],
                                                                             start=(kc == 0), stop=(kc == 7)),
                             reads=hk + [f"wgu{slot}g"], writes=[kg], signal=(kc == 7))
                    for kc in range(8):
                        S.op("pe", lambda e, kc=kc, W=W, pu=pu, tg=tg: e.matmul(pu[:], lhsT=W[:, 1, kc, :], rhs=HT[:, kc, tg * 512:(tg + 1) * 512],
                                                                             start=(kc == 0), stop=(kc == 7)),
                             reads=hk + [f"wgu{slot}u"], writes=[ku], signal=(kc == 7))
                    s_ = sg[cnt % 2]
                    S.op("act", lambda e, s_=s_, pg=pg: e.activation(out=s_, in_=pg[:], func=AF.Silu), reads=[kg], writes=[f"sg{cnt % 2}"])
                    S.op("dve", lambda e, s_=s_, pu=pu, c=c, c0=c0, tg=tg: e.tensor_tensor(
                        out=ACT[:, c - c0, tg * 512:(tg + 1) * 512], in0=s_, in1=pu[:], op=ALU.mult),
                        reads=[f"sg{cnt % 2}", ku], writes=[f"act{c - c0}_{tg}"])
                    cnt += 1
            for t in range(NT):
                for hf in range(2):
                    pd = self.psf[4 + (2 * t + hf) % 2]
                    kd = f"psf{4 + (2 * t + hf) % 2}"
                    for cc in range(ncp):
                        S.op("pe", lambda e, cc=cc, t=t, hf=hf, pd=pd, ncp=ncp: e.matmul(pd[:], lhsT=ACT[:, cc, t * 128:(t + 1) * 128],
                                                                             rhs=WD[:, cc, hf * 512:(hf + 1) * 512],
                                                                             start=(cc == 0), stop=(cc == ncp - 1)),
                             reads=[f"act{cc}_{t // 4}", "wd"], writes=[kd], signal=(cc == ncp - 1))
                    S.op("dve", lambda e, t=t, hf=hf, pd=pd: e.scalar_tensor_tensor(
                        out=X[:, t, hf * 512:(hf + 1) * 512], in0=pd[:], scalar=0.5, in1=X[:, t, hf * 512:(hf + 1) * 512],
                        op0=ALU.mult, op1=ALU.add), reads=[kd, f"x{t}"], writes=[f"x{t}"])

    def rotary(self, ps, out_bf, t, half, cosT, sinT, tmp):
        S = self.S
        a, b = tmp[0][:, :, 0:half], tmp[1][:, :, 0:half]
        cb = cosT[:, t:t + 1, :].to_broadcast([128, 6, half])
        sbb = sinT[:, t:t + 1, :].to_broadcast([128, 6, half])
        x1, x2 = ps[:, :, 0:half], ps[:, :, half:2 * half]
        rk, wk = self._rot_keys
        S.op("dve", lambda e: e.tensor_tensor(out=a, in0=x1, in1=cb, op=ALU.mult), reads=rk, writes=["rot_a"])
        S.op("dve", lambda e: e.tensor_tensor(out=b, in0=x2, in1=sbb, op=ALU.mult), reads=rk, writes=["rot_b"])
        S.op("dve", lambda e: e.tensor_tensor(out=out_bf[:, :, 0:half], in0=a, in1=b, op=ALU.subtract), reads=["rot_a", "rot_b"], writes=wk)
        S.op("dve", lambda e: e.tensor_tensor(out=a, in0=x2, in1=cb, op=ALU.mult), reads=rk, writes=["rot_a"])
        S.op("dve", lambda e: e.tensor_tensor(out=b, in0=x1, in1=sbb, op=ALU.mult), reads=rk, writes=["rot_b"])
        S.op("dve", lambda e: e.tensor_tensor(out=out_bf[:, :, half:2 * half], in0=a, in1=b, op=ALU.add), reads=["rot_a", "rot_b"], writes=wk)

    def mixer(self, l):
        S = self.S
        self.norm_to_hT(self.norms[l, 1:2, :])
        X = self.carve(A_X, [128, NT, D_MODEL], F32)
        xkeys = [f"x{t}" for t in range(NT)]
        S.dma("sp", lambda e: e.dma_start(out=self.xsp.rearrange("p (t f) -> p t f", t=NT), in_=X), reads=xkeys, writes=["xsp"])
        S.dma("pool", lambda e: e.dma_start(out=self.cpar[:], in_=self.convp[l]), writes=["cpar"])
        S.barrier()
        if self.stop == "spill":
            return
        HT = self.carve(A_HT, [128, 8, TOK], BF16)
        QRT = self.carve(A_QRT, [128, 3, TOK], BF16)
        KRT = self.carve(A_KRT, [128, 3, TOK], BF16)
        VTOK = self.carve(A_VTOK, [128, NT, 384], BF16)
        RG = self.carve(A_RG, [128, NT, 384], BF16)
        MQT = self.carve(A_MQT, [128, 3, TOK], BF16)
        MKT = self.carve(A_MKT, [128, 3, TOK], BF16)
        KVS = self.carve(A_KVS, [128, 8, 3, 64], F32)
        UT = self.carve(A_UT, [128, 2, 2080], BF16)
        WIN = [self.carve(A_WIN + 6144 * i, [128, 8, 384], BF16) for i in range(2)]
        CW = self.carve(A_CW, [128, 4, 8, 128], BF16)
        VIMG = self.carve(A_WOUT, [128, 3, NT, 128], BF16)
        tokb = [self.carve(A_TMP + 768 * i, [128, 6, 64], BF16) for i in range(3)]
        rtmp = [self.carve(A_TMP + 2304 + 768 * i, [128, 6, 32], F32) for i in range(2)]
        winv = self.w["w_in"][l].rearrange("(k p) f -> p k f", p=128)
        hkeys = [f"hT{t}" for t in range(NT)]
        qs = self.cfv("qs").rearrange("p (a b) -> p a b", a=2)
        ks = self.cfv("ks").rearrange("p (a b) -> p a b", a=2)
        bc6 = lambda v: v.unsqueeze(2).to_broadcast([128, 6, 64])
        pcnt = 0
        tb = 0
        groups = [("rv", 768), ("rg", 1152), ("rk", 384), ("rq", 0), ("mv", 2304), ("mk", 1920), ("mq", 1536)]
        for gi, (gname, c0) in enumerate(groups):
            if self.stop is not None and self.stop.startswith("g:") and gi >= int(self.stop[2:]):
                return
            W = WIN[gi % 2]
            S.dma("pool", lambda e, W=W, c0=c0: e.dma_start(out=W, in_=winv[:, :, c0:c0 + 384]), writes=[f"win{gi % 2}"])
            for t in range(NT):
                ps = self.psf[pcnt % 2]
                pk = f"psf{pcnt % 2}"
                pcnt += 1
                for kc in range(8):
                    S.op("pe", lambda e, kc=kc, t=t, ps=ps, W=W: e.matmul(ps[:, 0:384], lhsT=HT[:, kc, t * 128:(t + 1) * 128], rhs=W[:, kc, :],
                                                                         start=(kc == 0), stop=(kc == 7)),
                         reads=[f"hT{t}", f"win{gi % 2}"], writes=[pk], signal=(kc == 7))
                psv = ps[:, 0:384].rearrange("p (h d) -> p h d", h=6)
                if gname == "rv":
                    S.op("act", lambda e, t=t, ps=ps: e.activation(out=VTOK[:, t, :], in_=ps[:, 0:384], func=AF.Copy), reads=[pk], writes=[f"vtok{t}"])
                elif gname == "rg":
                    S.op("act", lambda e, t=t, ps=ps: e.activation(out=RG[:, t, :], in_=ps[:, 0:384], func=AF.Silu), reads=[pk], writes=[f"rg{t}"])
                elif gname in ("rk", "rq"):
                    ob = tokb[tb % 3]
                    obk = f"tokb{tb % 3}"
                    tb += 1
                    self._rot_keys = ([pk, "rt_cos", "rt_sin"], [obk])
                    self.rotary(psv, ob, t, 32, self.cosr, self.sinr, rtmp)
                    sc = bc6((ks if gname == "rk" else qs)[:, t % 2, :])
                    obf = ob.rearrange("p h d -> p (h d)")
                    S.op("dve", lambda e, ob=ob, sc=sc: e.tensor_tensor(out=ob, in0=ob, in1=sc, op=ALU.mult), reads=[obk, "cf"], writes=[obk])
                    pb = self.psb[t % 2]
                    for pr in range(3):
                        S.op("pe", lambda e, pr=pr, pb=pb, obf=obf: e.transpose(out=pb[:, pr * 128:(pr + 1) * 128], in_=obf[:, pr * 128:(pr + 1) * 128],
                                                                               identity=self.identb[:]),
                             reads=[obk, "identb"], writes=[f"psb{t % 2}"], signal=(pr == 2))
                    dst = (KRT if gname == "rk" else QRT)
                    dk = ("krt" if gname == "rk" else "qrt") + str(t)
                    self.copy(self.cp_eng(), dst[:, :, t * 128:(t + 1) * 128], pb[:, 0:384].rearrange("p (a b) -> p a b", a=3),
                              reads=[f"psb{t % 2}"], writes=[dk])
                    if gname == "rk":
                        pkv = self.psf[2]
                        for pr in range(3):
                            S.op("pe", lambda e, pr=pr, t=t, obf=obf, pkv=pkv: e.matmul(
                                pkv[:, pr * 128:(pr + 1) * 128], lhsT=obf[:, pr * 128:(pr + 1) * 128], rhs=VTOK[:, t, pr * 128:(pr + 1) * 128],
                                start=(t % 2 == 0 and pr == 0), stop=(t % 2 == 1 and pr == 2)),
                                reads=[obk, f"vtok{t}"], writes=["psf2"], signal=(pr == 2))
                        if t % 2 == 1:
                            ch = t // 2
                            pv = pkv[:, 0:384].rearrange("p (a b) -> p a b", a=3)
                            S.op("act", lambda e, ch=ch, pv=pv: e.activation(out=KVS[0:64, ch, :, :], in_=pv[0:64, :, 0:64], func=AF.Copy),
                                 reads=["psf2"], writes=[f"kvs{ch}a"])
                            S.op("dve", lambda e, ch=ch, pv=pv: e.tensor_copy(out=KVS[64:128, ch, :, :], in_=pv[64:128, :, 64:128]),
                                 reads=["psf2"], writes=[f"kvs{ch}b"])
                elif gname == "mv":
                    S.op("act", lambda e, t=t, ps=ps: e.activation(out=VIMG[:, :, t, :], in_=ps[:, 0:384].rearrange("p (a b) -> p a b", a=3), func=AF.Copy),
                         reads=[pk], writes=[f"vimg{t}"])
                elif gname in ("mk", "mq"):
                    ob = tokb[tb % 3]
                    obk = f"tokb{tb % 3}"
                    tb += 1
                    obf = ob.rearrange("p h d -> p (h d)")
                    S.op("dve", lambda e, ob=ob, psv=psv: e.tensor_copy(out=ob[:, :, 16:64], in_=psv[:, :, 16:64]), reads=[pk], writes=[obk])
                    self._rot_keys = ([pk, "rt_cos", "rt_sin"], [obk])
                    self.rotary(psv, ob, t, 8, self.cosm, self.sinm, rtmp)
                    pb = self.psb[t % 2]
                    for pr in range(3):
                        S.op("pe", lambda e, pr=pr, pb=pb, obf=obf: e.transpose(out=pb[:, pr * 128:(pr + 1) * 128], in_=obf[:, pr * 128:(pr + 1) * 128],
                                                                               identity=self.identb[:]),
                             reads=[obk, "identb"], writes=[f"psb{t % 2}"], signal=(pr == 2))
                    dst = (MKT if gname == "mk" else MQT)
                    dk = ("mkt" if gname == "mk" else "mqt") + str(t)
                    self.copy(self.cp_eng(), dst[:, :, t * 128:(t + 1) * 128], pb[:, 0:384].rearrange("p (a b) -> p a b", a=3),
                              reads=[f"psb{t % 2}"], writes=[dk])
        if self.stop == "g:7":
            return
        S.op("dve", lambda e: e.tensor_reduce(out=self.kmsb[:], in_=MKT.rearrange("p a (b c) -> p (a b) c", c=256),
                                              axis=mybir.AxisListType.X, op=ALU.add),
             reads=[f"mkt{t}" for t in range(NT)], writes=["kmsb"])
        S.op("dve", lambda e: e.tensor_scalar(out=self.kmsb[:], in0=self.kmsb[:], scalar1=1.0 / 256, scalar2=None, op0=ALU.mult),
             reads=["kmsb"], writes=["kmsb"])
        S.dma("sp", lambda e: e.dma_start(out=self.ccB_in[:, 0:24], in_=self.kmsb[:]), reads=["kmsb"], writes=["ccB_in"])
        for pr in range(3):
            S.dma("sp", lambda e, pr=pr: e.dma_start(out=self.ccK_in[pr], in_=MKT[:, pr, :]),
                  reads=[f"mkt{t}" for t in range(NT)], writes=[f"ccK_in{pr}"])
            S.dma("sp", lambda e, pr=pr: e.dma_start(out=self.ccV_in[pr], in_=VIMG[:, pr, :, :].rearrange("p t c -> p (t c)")),
                  reads=[f"vimg{t}" for t in range(NT)], writes=[f"ccV_in{pr}"])
        dec = self.cfv("dec").unsqueeze(2).to_broadcast([128, 3, 64])
        S.op("dve", lambda e: e.tensor_copy(out=self.state[:], in_=KVS[:, 0, :, :]), reads=["kvs0a", "kvs0b"], writes=["state"])
        S.op("dve", lambda e: e.tensor_tensor(out=self.state[:], in0=self.state[:], in1=dec, op=ALU.mult), reads=["state", "cf"], writes=["state"])
        for ch in range(1, 8):
            S.op("dve", lambda e, ch=ch: e.tensor_tensor(out=self.state[:], in0=self.state[:], in1=KVS[:, ch, :, :], op=ALU.add),
                 reads=["state", f"kvs{ch}a", f"kvs{ch}b"], writes=["state"])
            S.op("dve", lambda e: e.tensor_tensor(out=self.state[:], in0=self.state[:], in1=dec, op=ALU.mult), reads=["state", "cf"], writes=["state"])
        S.dma("sp", lambda e: e.dma_start(out=self.ccB_in[:, 24:216], in_=self.state[:].rearrange("p a b -> p (a b)")),
              reads=["state"], writes=["ccB_in"])
        for j, c0 in enumerate((2688, 2816, 2944, 3072)):
            S.dma("pool", lambda e, j=j, c0=c0: e.dma_start(out=CW[:, j, :, :], in_=winv[:, :, c0:c0 + 128]), writes=[f"cw{j}"])
        sgm = [self.carve(A_TMP + 4096 + 2048 * i, [128, 512], F32) for i in range(2)]
        cc_ = 0
        for c in range(2):
            for tg in range(4):
                pa, pg = self.psf[cc_ % 2], self.psf[4 + cc_ % 2]
                ka, kg = f"psf{cc_ % 2}", f"psf{4 + cc_ % 2}"
                hk = [f"hT{t}" for t in range(4 * tg, 4 * tg + 4)]
                for kc in range(8):
                    S.op("pe", lambda e, kc=kc, c=c, tg=tg, pa=pa: e.matmul(pa[:], lhsT=CW[:, c, kc, :], rhs=HT[:, kc, tg * 512:(tg + 1) * 512],
                                                                         start=(kc == 0), stop=(kc == 7)),
                         reads=hk + [f"cw{c}"], writes=[ka], signal=(kc == 7))
                for kc in range(8):
                    S.op("pe", lambda e, kc=kc, c=c, tg=tg, pg=pg: e.matmul(pg[:], lhsT=CW[:, 2 + c, kc, :], rhs=HT[:, kc, tg * 512:(tg + 1) * 512],
                                                                         start=(kc == 0), stop=(kc == 7)),
                         reads=hk + [f"cw{2 + c}"], writes=[kg], signal=(kc == 7))
                s_ = sgm[cc_ % 2]
                S.op("act", lambda e, s_=s_, pg=pg: e.activation(out=s_, in_=pg[:], func=AF.Sigmoid), reads=[kg], writes=[f"sgm{cc_ % 2}"])
                S.op("dve", lambda e, s_=s_, pa=pa, c=c, tg=tg: e.tensor_tensor(out=UT[:, c, 32 + tg * 512:32 + (tg + 1) * 512], in0=pa[:], in1=s_, op=ALU.mult),
                     reads=[ka, f"sgm{cc_ % 2}"], writes=[f"ut{c}_{tg}"])
                cc_ += 1
        S.op("dve", lambda e: e.tensor_copy(out=self.tail_f[:], in_=UT[:, :, 2050:2080]), reads=["ut0_3", "ut1_3"], writes=["tail_f"])
        S.dma("sp", lambda e: e.dma_start(out=self.ccB_in[:, 216:276], in_=self.tail_f[:].rearrange("p a b -> p (a b)")),
              reads=["tail_f"], writes=["ccB_in"])
        if self.stop == "proj":
            return
        S.cc(lambda e: e.collective_compute("AllGather", ALU.bypass, replica_groups=[[0, 1, 2, 3], [4, 5, 6, 7]],
                                            ins=[self.ccB_in], outs=[self.ccB_out]), reads=["ccB_in"], writes=["ccB_out"])
        for pr in range(3):
            S.cc(lambda e, pr=pr: e.collective_compute("AllGather", ALU.bypass, replica_groups=[[0, 1, 2, 3], [4, 5, 6, 7]],
                                                       ins=[self.ccK_in[pr]], outs=[self.ccK_out[pr]]), reads=[f"ccK_in{pr}"], writes=[f"ccK_out{pr}"])
            S.cc(lambda e, pr=pr: e.collective_compute("AllGather", ALU.bypass, replica_groups=[[0, 1, 2, 3], [4, 5, 6, 7]],
                                                       ins=[self.ccV_in[pr]], outs=[self.ccV_out[pr]]), reads=[f"ccV_in{pr}"], writes=[f"ccV_out{pr}"])
        ccBv = self.ccB_out.rearrange("(r p) w -> p r w", p=128)
        S.dma("sp", lambda e: e.dma_start(out=self.send[:], in_=ccBv[:, :, 24:216]), reads=["ccB_out"], writes=["send"])
        S.dma("sp", lambda e: e.dma_start(out=self.tails[:], in_=ccBv[:, :, 216:276]), reads=["ccB_out"], writes=["tails"])
        for h in range(6):
            S.dma("sp", lambda e, h=h: e.dma_start(
                out=self.kmall[:, h, :].rearrange("p (r n) -> p r n", r=4),
                in_=ccBv[(h % 2) * 64:(h % 2) * 64 + 64, :, (h // 2) * 8:(h // 2) * 8 + 8]), reads=["ccB_out"], writes=["kmall"])
        if self.stop == "cc":
            return
        coefr = self.cfv("coef").rearrange("p (r a) -> p r a", r=4)
        coef = [coefr[:, r_, :].unsqueeze(2).to_broadcast([128, 3, 64]) for r_ in range(4)]
        sv = self.send[:].rearrange("p r (a b) -> p r a b", a=3)
        stt = self.carve(A_TMP + 3840, [128, 3, 64], F32)
        S.op("dve", lambda e: e.tensor_tensor(out=self.state[:], in0=sv[:, 0], in1=coef[0], op=ALU.mult), reads=["send", "cf", "state"], writes=["state"])
        for r_ in range(1, 4):
            S.op("dve", lambda e, r_=r_: e.tensor_tensor(out=stt, in0=sv[:, r_], in1=coef[r_], op=ALU.mult), reads=["send", "cf"], writes=["stt"])
            S.op("dve", lambda e: e.tensor_tensor(out=self.state[:], in0=self.state[:], in1=stt, op=ALU.add), reads=["state", "stt"], writes=["state"])
        MIXT = self.carve(A_HT, [128, 8, TOK], BF16)
        ptb = [self.carve(A_TMP + 4608 + 768 * i, [128, 384], BF16) for i in range(3)]
        ytok = [self.carve(A_TMP + 6912 + 768 * i, [128, 384], BF16) for i in range(2)]
        ysq = self.carve(A_TMP + 8448, [128, 6, 64], F32)
        rs6 = self.carve(A_TMP + 9984, [128, 6], F32)
        pti = 0
        for ch in range(8):
            S.op("act", lambda e: e.activation(out=self.stbf[:], in_=self.state[:], func=AF.Copy), reads=["state"], writes=["stbf"])
            t0, t1 = 2 * ch, 2 * ch + 1
            po = [self.psf[2], self.psf[3]]
            pok = ["psf2", "psf3"]
            first = [True, True]
            for h in range(6):
                pr, hh = h // 2, h % 2
                prt = slice(hh * 64, hh * 64 + 64)
                ps = self.psf[pti % 2]
                psk = f"psf{pti % 2}"
                S.op("pe", lambda e, ps=ps, pr=pr, prt=prt, t0=t0: e.matmul(ps[:, 0:256], lhsT=KRT[prt, pr, t0 * 128:(t0 + 1) * 128],
                                                                         rhs=QRT[prt, pr, t0 * 128:(t0 + 2) * 128], start=True, stop=False),
                     reads=[f"krt{t0}", f"qrt{t0}", f"qrt{t1}"], writes=[psk], signal=False)
                S.op("pe", lambda e, ps=ps, pr=pr, prt=prt, t1=t1: e.matmul(ps[:, 256:384], lhsT=KRT[prt, pr, t1 * 128:(t1 + 1) * 128],
                                                                         rhs=QRT[prt, pr, t1 * 128:(t1 + 1) * 128], start=False, stop=True),
                     reads=[f"krt{t1}", f"qrt{t1}"], writes=[psk])
                pt = ptb[pti % 3]
                ptk = f"ptb{pti % 3}"
                pti += 1
                S.op("dve", lambda e, pt=pt, ps=ps: e.tensor_tensor(out=pt, in0=ps[:, 0:384], in1=self.mask01[:], op=ALU.mult),
                     reads=[psk, "mask01"], writes=[ptk])
                hc = slice(h * 64, h * 64 + 64)
                S.op("pe", lambda e, pt=pt, hc=hc, t0=t0, f0=first[0]: e.matmul(po[0][:, hc], lhsT=pt[:, 0:128], rhs=VTOK[:, t0, hc], start=f0, stop=False),
                     reads=[ptk, f"vtok{t0}"], writes=[pok[0]], signal=False)
                first[0] = False
                S.op("pe", lambda e, hc=hc, pr=pr, prt=prt, t0=t0, h=h: e.matmul(po[0][:, hc], lhsT=QRT[prt, pr, t0 * 128:(t0 + 1) * 128],
                                                                              rhs=self.stbf[prt, pr, :], start=False, stop=(h == 5)),
                     reads=[f"qrt{t0}", "stbf"], writes=[pok[0]], signal=(h == 5))
                S.op("pe", lambda e, pt=pt, hc=hc, t0=t0, f1=first[1]: e.matmul(po[1][:, hc], lhsT=pt[:, 128:256], rhs=VTOK[:, t0, hc], start=f1, stop=False),
                     reads=[ptk, f"vtok{t0}"], writes=[pok[1]], signal=False)
                first[1] = False
                S.op("pe", lambda e, pt=pt, hc=hc, t1=t1: e.matmul(po[1][:, hc], lhsT=pt[:, 256:384], rhs=VTOK[:, t1, hc], start=False, stop=False),
                     reads=[ptk, f"vtok{t1}"], writes=[pok[1]], signal=False)
                S.op("pe", lambda e, hc=hc, pr=pr, prt=prt, t1=t1, h=h: e.matmul(po[1][:, hc], lhsT=QRT[prt, pr, t1 * 128:(t1 + 1) * 128],
                                                                              rhs=self.stbf[prt, pr, :], start=False, stop=(h == 5)),
                     reads=[f"qrt{t1}", "stbf"], writes=[pok[1]], signal=(h == 5))
            for ci, t in enumerate((t0, t1)):
                pv = po[ci][:, 0:384].rearrange("p (h d) -> p h d", h=6)
                S.op("act", lambda e, pv=pv: e.activation(out=ysq, in_=pv, func=AF.Square), reads=[pok[ci]], writes=["ysq"])
                S.op("dve", lambda e: e.tensor_reduce(out=self.ssq[:], in_=ysq, axis=mybir.AxisListType.X, op=ALU.add), reads=["ysq"], writes=["ssq"])
                S.op("dve", lambda e: e.tensor_scalar(out=rs6, in0=self.ssq[:], scalar1=1.0 / 64, scalar2=EPS, op0=ALU.mult, op1=ALU.add),
                     reads=["ssq"], writes=["rs6"])
                S.op("act", lambda e: e.activation(out=rs6, in_=rs6, func=AF.Sqrt), reads=["rs6"], writes=["rs6"])
                S.op("dve", lambda e: e.reciprocal(out=rs6, in_=rs6), reads=["rs6"], writes=["rs6"])
                yt = ytok[t % 2]
                ytk = f"ytok{t % 2}"
                rgv = RG[:, t, :].rearrange("p (h d) -> p h d", h=6)
                ytv = yt.rearrange("p (h d) -> p h d", h=6)
                for h in range(6):
                    S.op("dve", lambda e, h=h, pv=pv, ytv=ytv, rgv=rgv: e.scalar_tensor_tensor(
                        out=ytv[:, h, :], in0=pv[:, h, :], scalar=rs6[:, h:h + 1], in1=rgv[:, h, :], op0=ALU.mult, op1=ALU.mult),
                        reads=[pok[ci], "rs6", f"rg{t}"], writes=[ytk], signal=(h == 5))
                pb = self.psb[t % 2]
                for pr in range(3):
                    S.op("pe", lambda e, pr=pr, pb=pb, yt=yt: e.transpose(out=pb[:, pr * 128:(pr + 1) * 128], in_=yt[:, pr * 128:(pr + 1) * 128],
                                                                         identity=self.identb[:]),
                         reads=[ytk, "identb"], writes=[f"psb{t % 2}"], signal=(pr == 2))
                self.copy(self.cp_eng(), MIXT[:, 0:3, t * 128:(t + 1) * 128], pb[:, 0:384].rearrange("p (a b) -> p a b", a=3),
                          reads=[f"psb{t % 2}"], writes=[f"mixT{t}"])
            S.op("dve", lambda e, ch=ch: e.tensor_tensor(out=self.state[:], in0=self.state[:], in1=KVS[:, ch, :, :], op=ALU.add),
                 reads=["state", f"kvs{ch}a", f"kvs{ch}b"], writes=["state"])
            S.op("dve", lambda e: e.tensor_tensor(out=self.state[:], in0=self.state[:], in1=dec, op=ALU.mult), reads=["state", "cf"], writes=["state"])
        if "mixret" in self.taps and l == 0:
            S.barrier()
            self.tap_mixT("mixret")
        S.barrier()
        if self.stop == "ret":
            return
        self.conv(l)
        S.barrier()
        if self.stop == "conv":
            return
        self.moba(l)
        S.barrier()
        if self.stop == "moba":
            return
        if "mixT" in self.taps and l == 0:
            self.tap_mixT("mixT")
            S.barrier()
        WOUT = self.carve(A_WOUT, [128, 8, D_MODEL], BF16)
        S.dma("pool", lambda e: e.dma_start(out=WOUT, in_=self.w["w_out"][l].rearrange("(k p) f -> p k f", p=128)),
              writes=["wout", "gain"] + [f"vimg{t}" for t in range(NT)])
        S.dma("sp", lambda e: e.dma_start(out=X, in_=self.xsp.rearrange("p (t f) -> p t f", t=NT)), reads=["xsp"], writes=xkeys)
        for t in range(NT):
            for hf in range(2):
                pd = self.psf[4 + (2 * t + hf) % 2]
                kd = f"psf{4 + (2 * t + hf) % 2}"
                for kc in range(8):
                    S.op("pe", lambda e, kc=kc, t=t, hf=hf, pd=pd: e.matmul(pd[:], lhsT=MIXT[:, kc, t * 128:(t + 1) * 128],
                                                                         rhs=WOUT[:, kc, hf * 512:(hf + 1) * 512], start=(kc == 0), stop=(kc == 7)),
                         reads=["wout"], writes=[kd], signal=(kc == 7))
                S.op("dve", lambda e, t=t, hf=hf, pd=pd: e.tensor_tensor(out=X[:, t, hf * 512:(hf + 1) * 512], in0=pd[:],
                                                                       in1=X[:, t, hf * 512:(hf + 1) * 512], op=ALU.add),
                     reads=[kd, f"x{t}"], writes=[f"x{t}"])

    def tap_mixT(self, name):
        S = self.S
        MIXT = self.carve(A_HT, [128, 8, TOK], BF16)
        tmp = self.carve(A_CONVTMP, [128, TOK], F32)
        for kc in range(8):
            S.op("dve", lambda e, kc=kc: e.tensor_copy(out=tmp, in_=MIXT[:, kc, :]), writes=["taptmp"])
            S.dma("sp", lambda e, kc=kc: e.dma_start(out=self.tapouts[name][:, kc * TOK:(kc + 1) * TOK], in_=tmp), reads=["taptmp"],
                  writes=["tap_" + name + str(kc)])
        S.barrier()

    def conv(self, l):
        S = self.S
        UT = self.carve(A_UT, [128, 2, 2080], BF16)
        MIXT = self.carve(A_HT, [128, 8, TOK], BF16)
        sel = self.cfv("sel")
        identf = self.cfv("identf")
        bd = self.cfv("bd")
        tl = self.tails[:]
        S.op("dve", lambda e: e.tensor_scalar(out=self.halo[:], in0=tl[:, 0, :], scalar1=sel[:, 0:1], scalar2=None, op0=ALU.mult),
             reads=["tails", "cf"], writes=["halo"])
        for r_ in range(1, 4):
            S.op("dve", lambda e, r_=r_: e.scalar_tensor_tensor(out=self.halo[:], in0=tl[:, r_, :], scalar=sel[:, r_:r_ + 1], in1=self.halo[:],
                                                               op0=ALU.mult, op1=ALU.add), reads=["tails", "cf", "halo"], writes=["halo"])
        S.op("dve", lambda e: e.tensor_copy(out=UT[:, :, 2:32], in_=self.halo[:].rearrange("p (a b) -> p a b", a=2)), reads=["halo"], writes=["ut_halo"])
        diag = [self.carve(A_CONVTMP + 256 * i, [128, 128], BF16) for i in range(4)]
        ysb = self.carve(A_CONVTMP + 1024, [128, 512], F32)
        ysq = self.carve(A_CONVTMP + 3072, [128, 512], F32)
        msb = self.carve(A_CONVTMP + 5120, [128, 512], F32)
        var = self.carve(A_CONVTMP + 7168, [128, 512], F32)
        dj = 0
        for c in range(2):
            for j in range(31):
                dg = diag[dj % 4]
                dk = f"diag{dj % 4}"
                dj += 1
                S.op("dve", lambda e, dg=dg, c=c, j=j: e.tensor_scalar(out=dg, in0=identf, scalar1=self.cpar[:, c, j:j + 1], scalar2=None, op0=ALU.mult),
                     reads=["cf", "cpar"], writes=[dk])
                for tg in range(4):
                    S.op("pe", lambda e, dg=dg, c=c, j=j, tg=tg: e.matmul(self.psf[tg][:], lhsT=dg, rhs=UT[:, c, 2 + tg * 512 + j:2 + tg * 512 + j + 512],
                                                                         start=(j == 0), stop=(j == 30)),
                         reads=[dk, f"ut{c}_{tg}", "ut_halo"] + ([f"ut{c}_{tg - 1}"] if tg > 0 else []), writes=[f"psf{tg}"], signal=(tg == 3))
            for tg in range(4):
                pk = f"psf{tg}"
                S.op("act", lambda e, tg=tg, c=c: e.activation(out=ysb, in_=self.psf[tg][:], func=AF.Identity, bias=self.cpar[:, c, 31:32], scale=1.0),
                     reads=[pk, "cpar"], writes=["c_ysb"])
                S.op("act", lambda e: e.activation(out=ysq, in_=ysb, func=AF.Square), reads=["c_ysb"], writes=["c_ysq"])
                S.op("pe", lambda e: e.matmul(self.psf[4][:], lhsT=bd, rhs=ysb, start=True, stop=True), reads=["cf", "c_ysb"], writes=["psf4"])
                S.op("pe", lambda e: e.matmul(self.psf[5][:], lhsT=bd, rhs=ysq, start=True, stop=True), reads=["cf", "c_ysq"], writes=["psf5"])
                S.op("act", lambda e: e.activation(out=msb, in_=self.psf[4][:], func=AF.Copy), reads=["psf4"], writes=["c_msb"])
                S.op("dve", lambda e: e.tensor_tensor(out=var, in0=msb, in1=msb, op=ALU.mult), reads=["c_msb"], writes=["c_var"])
                S.op("dve", lambda e: e.tensor_tensor(out=var, in0=self.psf[5][:], in1=var, op=ALU.subtract), reads=["psf5", "c_var"], writes=["c_var"])
                S.op("dve", lambda e: e.tensor_scalar(out=var, in0=var, scalar1=EPS, scalar2=0.0, op0=ALU.add, op1=ALU.max), reads=["c_var"], writes=["c_var"])
                S.op("act", lambda e: e.activation(out=var, in_=var, func=AF.Sqrt), reads=["c_var"], writes=["c_var"])
                S.op("dve", lambda e: e.reciprocal(out=var, in_=var), reads=["c_var"], writes=["c_var"])
                S.op("dve", lambda e: e.tensor_tensor(out=ysb, in0=ysb, in1=msb, op=ALU.subtract), reads=["c_ysb", "c_msb"], writes=["c_ysb"])
                S.op("dve", lambda e: e.tensor_tensor(out=ysb, in0=ysb, in1=var, op=ALU.mult), reads=["c_ysb", "c_var"], writes=["c_ysb"])
                S.op("dve", lambda e, c=c: e.tensor_scalar(out=ysb, in0=ysb, scalar1=self.cpar[:, c, 32:33], scalar2=self.cpar[:, c, 33:34],
                                                          op0=ALU.mult, op1=ALU.add), reads=["c_ysb", "cpar"], writes=["c_ysb"])
                S.op("act", lambda e, c=c, tg=tg: e.activation(out=MIXT[:, 6 + c, tg * 512:(tg + 1) * 512], in_=ysb, func=AF.Silu),
                     reads=["c_ysb"], writes=[f"mixTc{c}_{tg}"])

    def moba(self, l):
        S = self.S
        MQT = self.carve(A_MQT, [128, 3, TOK], BF16)
        QAUG = self.carve(A_QAUG, [96, 6, TOK], BF16)
        MIXT = self.carve(A_HT, [128, 8, TOK], BF16)
        KT, VR = self.KT, self.VR
        onesf = self.cfv("onesf")
        cap = self.cfv("cap").rearrange("p (a b) -> p a b", a=8)
        pastneg = self.cfv("pastneg").rearrange("p (a b) -> p a b", a=8)
        futneg = self.cfv("futneg").rearrange("p (a b) -> p a b", a=8)
        for i in range(4):
            S.dma("pool", lambda e, i=i: e.dma_start(out=KT[i][64:96, :], in_=self.er_in[i]), writes=[f"kt{i}"])
            S.op("dve", lambda e, i=i: e.memset(VR[i][:, :, 64:66], 1.0), writes=[f"vr{i}"])
        for h in range(6):
            S.dma("sp", lambda e, h=h: e.dma_start(out=QAUG[0:64, h, :], in_=MQT[(h % 2) * 64:(h % 2) * 64 + 64, h // 2, :]),
                  reads=[f"mqt{t}" for t in range(NT)], writes=[f"qaug{h}"])
        S.op("dve", lambda e: e.tensor_copy(out=self.kmhi[:], in_=self.kmall[:]), reads=["kmall"], writes=["kmhi"])
        S.op("dve", lambda e: e.tensor_copy(out=self.kmt[:], in_=self.kmhi[:]), reads=["kmhi"], writes=["kmt"])
        S.op("dve", lambda e: e.tensor_tensor(out=self.kmt[:], in0=self.kmall[:], in1=self.kmt[:], op=ALU.subtract), reads=["kmall", "kmt"], writes=["kmt"])
        S.op("dve", lambda e: e.tensor_copy(out=self.kmlo[:], in_=self.kmt[:]), reads=["kmt"], writes=["kmlo"])
        gi = 0
        for h in range(6):
            for tg in range(4):
                pb = self.psb[(h * 4 + tg) % 2]
                pbk = f"psb{(h * 4 + tg) % 2}"
                for tt in range(4):
                    t = 4 * tg + tt
                    b = t // 2
                    pg = self.psf[gi % 2]
                    pgk = f"psf{gi % 2}"
                    g1, m8, t1, mt = self.g1[gi % 2], self.m8[gi % 2], self.t1[gi % 2], self.mtpad[gi % 2]
                    k_ = gi % 2
                    gi += 1
                    S.op("pe", lambda e, pg=pg, h=h, t=t: e.matmul(pg[:, 0:32], lhsT=QAUG[0:64, h, t * 128:(t + 1) * 128], rhs=self.kmhi[:, h, :],
                                                                 start=True, stop=False), reads=[f"qaug{h}", "kmhi"], writes=[pgk], signal=False)
                    S.op("pe", lambda e, pg=pg, h=h, t=t: e.matmul(pg[:, 0:32], lhsT=QAUG[0:64, h, t * 128:(t + 1) * 128], rhs=self.kmlo[:, h, :],
                                                                 start=False, stop=True), reads=[f"qaug{h}", "kmlo"], writes=[pgk])
                    S.op("dve", lambda e, pg=pg, g1=g1, b=b: e.tensor_tensor(out=g1[:], in0=pg[:, 0:32], in1=cap[:, b, :], op=ALU.min),
                         reads=[pgk, "cf"], writes=[f"g1_{k_}"])
                    S.op("dve", lambda e, g1=g1, m8=m8: e.max(out=m8[:], in_=g1[:]), reads=[f"g1_{k_}"], writes=[f"m8_{k_}"])
                    S.op("dve", lambda e, g1=g1, m8=m8, t1=t1, b=b: e.scalar_tensor_tensor(out=t1[:], in0=g1[:], scalar=m8[:, 2:3], in1=pastneg[:, b, :],
                                                                                       op0=ALU.is_lt, op1=ALU.mult),
                         reads=[f"g1_{k_}", f"m8_{k_}", "cf"], writes=[f"t1_{k_}"])
                    S.op("dve", lambda e, t1=t1, mt=mt, b=b: e.tensor_tensor(out=mt[:, 64:96], in0=t1[:], in1=futneg[:, b, :], op=ALU.add),
                         reads=[f"t1_{k_}", "cf"], writes=[f"mtpad{k_}"])
                    S.op("pe", lambda e, mt=mt, pb=pb, tt=tt: e.transpose(out=pb[0:96, tt * 128:(tt + 1) * 128], in_=mt[:], identity=self.identb[:]),
                         reads=[f"mtpad{k_}", "identb"], writes=[pbk], signal=(tt == 3))
                self.copy(self.cp_eng(), QAUG[64:96, h, tg * 512:(tg + 1) * 512], pb[64:96, 0:512], reads=[pbk], writes=[f"qaug{h}"])
        NPT = 4
        PT = [self.carve(A_TMP + 1024 * i, [128, 512], BF16) for i in range(NPT)]
        osb = self.carve(A_TMP + 4096, [128, 512], F32)
        rec = self.carve(A_TMP + 6144, [128, 512], F32)
        otmp = self.carve(A_TMP + 8192, [128, 512], BF16)
        SB = [(self.psf[4][:], "psf4"), (self.psf[5][:], "psf5"),
              (self.psb[0][:].bitcast(F32), "psb0"), (self.psb[1][:].bitcast(F32), "psb1")]
        NSB = len(SB)
        for h in range(6):
            steps = []
            for i in range(4):
                S.dma("sp", lambda e, i=i, h=h: e.dma_start(
                    out=KT[i][0:64, :], in_=self.ccK_out[h // 2][i * 128 + (h % 2) * 64:i * 128 + (h % 2) * 64 + 64, :]),
                    reads=[f"ccK_out{h // 2}"], writes=[f"kt{i}"])
                S.dma("sp", lambda e, i=i, h=h: e.dma_start(
                    out=VR[i][:, :, 0:64],
                    in_=self.ccV_out[h // 2][i * 128:i * 128 + 128, :].rearrange("p (t c) -> p t c", c=128)[:, :, (h % 2) * 64:(h % 2) * 64 + 64]),
                    reads=[f"ccV_out{h // 2}"], writes=[f"vr{i}"])
                for t in range(NT):
                    for g in range(4):
                        steps.append((i, t, g))
            ns = len(steps)

            def emit_s(k):
                i, t, g = steps[k]
                ps, psk = SB[k % NSB]
                need_c = (4 * g <= t < 4 * g + 4)
                S.op("pe", lambda e, ps=ps, i=i, t=t, g=g, h=h: e.matmul(ps, lhsT=KT[i][0:96, t * 128:(t + 1) * 128], rhs=QAUG[:, h, g * 512:(g + 1) * 512],
                                                                       start=True, stop=not need_c),
                     reads=[f"kt{i}", f"qaug{h}"], writes=[psk], signal=not need_c)
                if need_c:
                    S.op("pe", lambda e, ps=ps, i=i, t=t, g=g: e.matmul(ps, lhsT=self.identR[:, i, :], rhs=self.cmask[:, t - 4 * g, :], start=False, stop=True),
                         reads=["identR", "cmask"], writes=[psk])

            def emit_pv(k):
                i, t, g = steps[k]
                ps, psk = SB[k % NSB]
                pt = PT[k % NPT]
                S.op("act", lambda e, ps=ps, pt=pt: e.activation(out=pt, in_=ps, func=AF.Exp, scale=0.125), reads=[psk], writes=[f"pt{k % NPT}"])
                S.op("pe", lambda e, pt=pt, i=i, t=t, g=g: e.matmul(self.psf[g][0:65, :], lhsT=VR[i][:, t, 0:65], rhs=pt,
                                                                  start=(i == 0 and t == 0), stop=(i == 3 and t == NT - 1)),
                     reads=[f"pt{k % NPT}", f"vr{i}"], writes=[f"psf{g}"], signal=True)

            LOOK = NSB - 1
            for k in range(min(LOOK, ns)):
                emit_s(k)
            for k in range(ns):
                if k + LOOK < ns:
                    emit_s(k + LOOK)
                emit_pv(k)
            for g in range(4):
                po = self.psf[g]
                S.op("act", lambda e, po=po: e.activation(out=osb[0:65, :], in_=po[0:65, :], func=AF.Copy), reads=[f"psf{g}"], writes=["osb"])
                S.op("dve", lambda e: e.reciprocal(out=rec[64:65, :], in_=osb[64:65, :]), reads=["osb"], writes=["rec"])
                pr_ = self.psf[4 + g % 2]
                prk = f"psf{4 + g % 2}"
                S.op("pe", lambda e, pr_=pr_: e.matmul(pr_[0:64, :], lhsT=onesf[64:65, 0:64], rhs=rec[64:65, :], start=True, stop=True),
                     reads=["cf", "rec"], writes=[prk])
                ch = 3 + h // 2
                if h % 2 == 0:
                    S.op("dve", lambda e, pr_=pr_, g=g, ch=ch: e.tensor_tensor(out=MIXT[0:64, ch, g * 512:(g + 1) * 512], in0=osb[0:64, :], in1=pr_[0:64, :], op=ALU.mult),
                         reads=["osb", prk], writes=[f"mixTm{h}_{g}"])
                else:
                    S.op("dve", lambda e, pr_=pr_: e.tensor_tensor(out=otmp[0:64, :], in0=osb[0:64, :], in1=pr_[0:64, :], op=ALU.mult),
                         reads=["osb", prk], writes=["otmp"])
                    S.dma("sp", lambda e, g=g, ch=ch: e.dma_start(out=MIXT[64:128, ch, g * 512:(g + 1) * 512], in_=otmp[0:64, :]),
                          reads=["otmp"], writes=[f"mixTm{h}_{g}"])

    def final(self):
        S = self.S
        S.barrier()
        X = self.carve(A_X, [128, NT, D_MODEL], F32)
        gain = self.carve(A_GAIN, [128, D_MODEL], F32)
        S.dma("sp", lambda e: e.dma_start(out=gain, in_=self.fnorm[0:1, :].to_broadcast([128, D_MODEL])), writes=["gain"])
        self.rms_stats()
        yo = self.y_out.rearrange("(t p) f -> p t f", p=128)
        ob = [self.carve(A_HT + 4096 * i, [128, D_MODEL], F32) for i in range(2)]
        for t in range(NT):
            o = ob[t % 2]
            S.op("dve", lambda e, t=t, o=o: e.scalar_tensor_tensor(out=o, in0=X[:, t, :], scalar=self.rstd[:, t:t + 1], in1=gain, op0=ALU.mult, op1=ALU.mult),
                 reads=[f"x{t}", "rstd", "gain"], writes=[f"fo{t % 2}"])
            S.dma("sp", lambda e, t=t, o=o: e.dma_start(out=yo[:, t, :], in_=o), reads=[f"fo{t % 2}"], writes=[f"y{t}"])


_PROG_CACHE = {}


def _get_prog(depth, do_final, taps=(), stop=None):
    key = (depth, do_final, tuple(taps), stop)
    if key not in _PROG_CACHE:
        p = Prog(depth, do_final, taps, stop)
        p.build()
        _PROG_CACHE[key] = p
    return _PROG_CACHE[key]


def _layer_inputs(inp, l0, l1):
    f32 = np.float32
    sl = slice(l0, l1)
    d = {}
    for f in (1, 2):
        d[f"wg{f}"] = np.ascontiguousarray(inp[f"ffn{f}_wg"][sl], f32)
        d[f"wu{f}"] = np.ascontiguousarray(inp[f"ffn{f}_wu"][sl], f32)
        d[f"wd{f}"] = np.ascontiguousarray(inp[f"ffn{f}_wd"][sl], f32)
    d["w_in"] = np.ascontiguousarray(inp["w_in"][sl], f32)
    d["w_out"] = np.ascontiguousarray(inp["w_out"][sl], f32)
    d["norms"] = np.ascontiguousarray(np.stack([inp["ffn1_norm"][sl], inp["mix_norm"][sl], inp["ffn2_norm"][sl]], 1), f32)
    d["fnorm"] = np.ascontiguousarray(inp["final_norm"], f32).reshape(1, D_MODEL)
    nl = l1 - l0
    cp = np.zeros((nl, 128, 2, 34), f32)
    cw = np.asarray(inp["conv_w"][sl], f32)
    cp[:, :, :, 0:31] = cw.transpose(0, 2, 1).reshape(nl, 2, 128, 31).transpose(0, 2, 1, 3)
    for j, nm in ((31, "conv_b"), (32, "conv_ln_g"), (33, "conv_ln_b")):
        cp[:, :, :, j] = np.asarray(inp[nm][sl], f32).reshape(nl, 2, 128).transpose(0, 2, 1)
    d["convp"] = cp
    return d


def _run(inp, xs, l0, l1, do_final, taps=(), stop=None):
    prog = _get_prog(l1 - l0, do_final, taps, stop)
    shared = _layer_inputs(inp, l0, l1)
    pos = np.asarray(inp["positions"], np.int32)
    in_maps = []
    for c in range(NCORES):
        b, r = c // 4, c % 4
        cf, cm, er = _host_consts(r)
        m = dict(shared)
        m["x"] = np.ascontiguousarray(xs[c], np.float32)
        m["pos"] = np.ascontiguousarray(pos[b, r * TOK:(r + 1) * TOK].reshape(NT, 128).T)
        m["cf"] = cf
        m["cmask"] = cm
        m["erows"] = er
        in_maps.append(m)
    res = run_bass_kernel_spmd(prog.nc, in_maps, core_ids=list(range(NCORES)))
    return res.results


FUSED = True


def kernel(**inputs):
    inp = {k: np.asarray(v) for k, v in inputs.items()}
    x = np.asarray(inp["x"], np.float32)
    xs = [x[c // 4, (c % 4) * TOK:(c % 4 + 1) * TOK] for c in range(NCORES)]
    if FUSED:
        res = _run(inp, xs, 0, DEPTH, True)
        xs = [r["y"] for r in res]
    else:
        for l in range(DEPTH):
            res = _run(inp, xs, l, l + 1, l == DEPTH - 1)
            xs = [r["y"] for r in res]
    out = np.zeros((2, SEQ, D_MODEL), np.float32)
    for c in range(NCORES):
        out[c // 4, (c % 4) * TOK:(c % 4 + 1) * TOK] = xs[c]
    return out
```

```python
import math
from contextlib import ExitStack

import numpy as np
import concourse.bass as bass
import concourse.mybir as mybir
from concourse.bass_utils import run_bass_kernel_spmd

F32 = mybir.dt.float32
BF16 = mybir.dt.bfloat16
I32 = mybir.dt.int32
ALU = mybir.AluOpType
AF = mybir.ActivationFunctionType

D_MODEL = 1024
SEQ = 8192
DEPTH = 4
D_FF = 2816
IN_W = 3200
NCORES = 8
TOK = 2048
NT = 16
NEG = -30000.0
EPS = 1e-6

ENGS = ("pe", "act", "dve", "pool", "sp")
NDSEM = 8


class Sched:
    def __init__(self, nc):
        self.nc = nc
        self.q = {e: [] for e in ENGS}
        self.sig = {e: 0 for e in ENGS}
        self.dcnt = {e: 0 for e in ENGS}
        self.ccnt = 0
        self.seen = {e: {} for e in ENGS}
        self.issued = {}
        self.lastw = {}
        self.readers = {}

    def _deps(self, eng, reads, writes):
        toks = []
        for b in reads:
            t = self.lastw.get(b)
            if t is not None:
                toks.append(t)
        for b in writes:
            t = self.lastw.get(b)
            if t is not None:
                toks.append(t)
            toks.extend(self.readers.get(b, ()))
        need = {}
        for (sk, v, e) in toks:
            if e == eng and sk[0] == "c":
                if eng == "pe" or v > self.sig[eng]:
                    continue
            if self.seen[eng].get(sk, 0) >= v:
                continue
            if need.get(sk, 0) < v:
                need[sk] = v
        for sk, v in need.items():
            self.seen[eng][sk] = v
        return list(need.items())

    def _commit(self, tok, reads, writes):
        for b in reads:
            self.readers.setdefault(b, []).append(tok)
        for b in writes:
            self.lastw[b] = tok
            self.readers[b] = []

    def op(self, eng, fn, reads=(), writes=(), signal=True):
        waits = self._deps(eng, reads, writes)
        if signal:
            self.sig[eng] += 1
            val = self.sig[eng]
            self.issued[("c", eng)] = val
        else:
            val = self.sig[eng] + 1
        tok = (("c", eng), val, eng)
        self.q[eng].append(("op", waits, fn, signal, None))
        self._commit(tok, reads, writes)
        return tok

    def dma(self, eng, fn, reads=(), writes=()):
        waits = self._deps(eng, reads, writes)
        i = self.dcnt[eng]
        self.dcnt[eng] += 1
        sk = ("d", eng, i % NDSEM)
        val = 16 * (i // NDSEM + 1)
        if val > 16 and self.seen[eng].get(sk, 0) < val - 16:
            waits.append((sk, val - 16))
            self.seen[eng][sk] = val - 16
        tok = (sk, val, eng)
        self.issued[sk] = val
        self.q[eng].append(("dma", waits, fn, True, sk))
        self._commit(tok, reads, writes)
        return tok

    def cc(self, fn, reads=(), writes=()):
        eng = "pool"
        waits = self._deps(eng, reads, writes)
        self.ccnt += 1
        sk = ("x", "cc")
        if self.ccnt > 1 and self.seen[eng].get(sk, 0) < self.ccnt - 1:
            waits.append((sk, self.ccnt - 1))
            self.seen[eng][sk] = self.ccnt - 1
        tok = (sk, self.ccnt, eng)
        self.issued[sk] = self.ccnt
        self.q[eng].append(("cc", waits, fn, True, sk))
        self._commit(tok, reads, writes)
        return tok

    def barrier(self, engines=ENGS):
        for e in engines:
            waits = []
            for sk, v in self.issued.items():
                if sk == ("c", e):
                    continue
                if self.seen[e].get(sk, 0) < v:
                    waits.append((sk, v))
                    self.seen[e][sk] = v
            self.q[e].append(("wait", waits, None, False, None))

    def wait_all(self, eng, bufs):
        waits = self._deps(eng, bufs, ())
        self.q[eng].append(("wait", waits, None, False, None))

    def emit(self):
        nc = self.nc
        used = []
        seen = set()
        for e in ENGS:
            for (kind, waits, fn, signal, sk) in self.q[e]:
                keys = [wk for (wk, v) in waits]
                if kind in ("dma", "cc"):
                    keys.append(sk)
                elif kind == "op" and signal:
                    keys.append(("c", e))
                for k in keys:
                    if k not in seen:
                        seen.add(k)
                        used.append(k)
        with ExitStack() as st:
            sems = {}
            for s in used:
                sems[s] = st.enter_context(nc.semaphore("s_" + "_".join(str(x) for x in s)))
            block = st.enter_context(nc.Block())

            def runner(e):
                def run(engobj):
                    for (kind, waits, fn, signal, sk) in self.q[e]:
                        for (wk, v) in waits:
                            engobj.wait_ge(sems[wk], v)
                        if kind == "op":
                            ins = fn(engobj)
                            if signal:
                                ins.then_inc(sems[("c", e)], 1)
                        elif kind == "dma":
                            fn(engobj).then_inc(sems[sk], 16)
                        elif kind == "cc":
                            fn(engobj).then_inc(sems[sk], 1)
                return run

            block.tensor(runner("pe"))
            block.scalar(runner("act"))
            block.vector(runner("dve"))
            block.gpsimd(runner("pool"))
            block.sync(runner("sp"))


CF = {}
_off = 0
for _name, _w in [("identf", 128), ("bd", 128), ("onesf", 128), ("invr", 32), ("invm", 8),
                  ("qs", 12), ("ks", 12), ("dec", 3), ("coef", 12), ("cap", 256),
                  ("pastneg", 256), ("futneg", 256), ("sel", 4), ("c256", 1), ("zero", 1),
                  ("mask01", 384), ("identR", 512)]:
    CF[_name] = (_off, _w)
    _off += _w
NCF = _off
NCS = CF["mask01"][0]


def _host_consts(rank):
    cf = np.zeros((128, NCF), np.float32)

    def put(name, arr):
        o, w = CF[name]
        cf[:, o:o + w] = np.asarray(arr, np.float32).reshape(128, w)

    p = np.arange(128)
    put("identf", np.eye(128))
    bd = np.zeros((128, 128))
    bd[:64, :64] = 1.0 / 64
    bd[64:, 64:] = 1.0 / 64
    put("bd", bd)
    put("onesf", np.ones((128, 128)))
    ret_inv = (10000.0 ** (-np.linspace(0.0, 1.0, 32, dtype=np.float32))).astype(np.float32)
    rope_inv = (500000.0 ** (-np.arange(8, dtype=np.float32) / 8)).astype(np.float32)
    put("invr", np.broadcast_to(ret_inv, (128, 32)))
    put("invm", np.broadcast_to(rope_inv, (128, 8)))
    hh = np.arange(6, dtype=np.float64)
    lg = np.log1p(-np.exp2(-5.0 - hh))
    qs = np.zeros((128, 2, 6))
    ks = np.zeros((128, 2, 6))
    for par in range(2):
        c = par * 128 + p
        qs[:, par] = np.exp(lg[None, :] * (c[:, None] + 1.0))
        ks[:, par] = np.exp(-lg[None, :] * (c[:, None] + 1.0)) * 0.125
    put("qs", qs)
    put("ks", ks)
    hd = np.zeros((128, 3), np.int64)
    for pr in range(3):
        hd[:64, pr] = 2 * pr
        hd[64:, pr] = 2 * pr + 1
    dec = np.exp(lg[hd] * 256.0)
    put("dec", dec)
    coef = np.zeros((128, 4, 3))
    for i in range(4):
        if i < rank:
            coef[:, i] = np.exp(lg[hd] * 2048.0 * (rank - 1 - i))
    put("coef", coef)
    cap = np.zeros((128, 8, 32))
    pastneg = np.zeros((128, 8, 32))
    futneg = np.zeros((128, 8, 32))
    n = np.arange(32)
    for b in range(8):
        own = 8 * rank + b
        cap[:, b] = np.where(n < own, 3.0e38, -1.0e9)[None, :]
        pastneg[:, b] = np.where(n < own, NEG, 0.0)[None, :]
        futneg[:, b] = np.where(n > own, NEG, 0.0)[None, :]
    put("cap", cap)
    put("pastneg", pastneg)
    put("futneg", futneg)
    sel = np.zeros((128, 4))
    if rank > 0:
        sel[:, rank - 1] = 1.0
    put("sel", sel)
    tri = (p[:, None] <= p[None, :]).astype(np.float32)
    put("mask01", np.concatenate([tri, np.ones((128, 128)), tri], 1))
    idr = np.zeros((128, 4, 128))
    idr[:, rank] = np.eye(128)
    put("identR", idr)
    put("c256", np.full((128, 1), 1.0 / 256))
    cm = np.zeros((128, 4, 512), np.float32)
    q = np.arange(512)
    for t in range(4):
        cm[:, t] = np.where((t * 128 + p)[:, None] > q[None, :], NEG, 0.0)
    er = np.zeros((4, 32, 2048), np.float32)
    key = np.arange(2048)
    for i in range(4):
        er[i, 8 * i + key // 256, key] = 1.0
    return cf, cm.reshape(128, 2048), er


ARENA = 176 * 1024
A_X = 0
A_HT = 65536
A_ACT = 98304
A_WD = 131072
A_WGU = 147456
A_GAIN = 159744
A_TMP = 163840
A_QRT = 0
A_KRT = 12288
A_VTOK = 24576
A_RG = 36864
A_MQT = 49152
A_MKT = 98304
A_KVS = 110592
A_UT = 116736
A_WIN = 125056
A_CW = 137344
A_WOUT = 145536
A_QAUG = 0
A_KT = 24576
A_CONVTMP = 0
A_VR = 125056


class Prog:
    def __init__(self, depth, do_final, taps=(), stop=None):
        self.stop = stop
        self.D = depth
        self.do_final = do_final
        self.taps = taps
        self.nc = bass.Bass("TRN2", target_bir_lowering=False)
        self.S = Sched(self.nc)
        self.tapouts = {}

    def carve(self, off, shape, dt):
        n = int(np.prod(shape[1:]))
        sz = 2 if dt == BF16 else 4
        assert off % 4 == 0 and off + n * sz <= ARENA, (off, shape)
        v = self.arena[:, off // 2: off // 2 + n * sz // 2]
        if dt != BF16:
            v = v.bitcast(dt)
        if len(shape) == 3:
            v = v.rearrange("p (a b) -> p a b", a=shape[1])
        elif len(shape) == 4:
            v = v.rearrange("p (a b c) -> p a b c", a=shape[1], b=shape[2])
        if shape[0] != 128:
            v = v[0:shape[0]]
        return v

    def cfv(self, name):
        o, w = CF[name]
        return self.cf[:, o:o + w]

    def build(self):
        nc, S, D = self.nc, self.S, self.D
        dram = lambda name, shape, dt, kind: nc.dram_tensor(name, shape, dt, kind=kind).ap()
        self.x_in = dram("x", [TOK, D_MODEL], F32, "ExternalInput")
        self.pos_in = dram("pos", [128, NT], I32, "ExternalInput")
        self.w = {}
        for f in (1, 2):
            self.w[f"wg{f}"] = dram(f"wg{f}", [D, D_MODEL, D_FF], F32, "ExternalInput")
            self.w[f"wu{f}"] = dram(f"wu{f}", [D, D_MODEL, D_FF], F32, "ExternalInput")
            self.w[f"wd{f}"] = dram(f"wd{f}", [D, D_FF, D_MODEL], F32, "ExternalInput")
        self.w["w_in"] = dram("w_in", [D, D_MODEL, IN_W], F32, "ExternalInput")
        self.w["w_out"] = dram("w_out", [D, D_MODEL, D_MODEL], F32, "ExternalInput")
        self.norms = dram("norms", [D, 3, D_MODEL], F32, "ExternalInput")
        self.fnorm = dram("fnorm", [1, D_MODEL], F32, "ExternalInput")
        self.convp = dram("convp", [D, 128, 2, 34], F32, "ExternalInput")
        self.cf_in = dram("cf", [128, NCF], F32, "ExternalInput")
        self.cm_in = dram("cmask", [128, 2048], F32, "ExternalInput")
        self.er_in = dram("erows", [4, 32, 2048], F32, "ExternalInput")
        self.y_out = dram("y", [TOK, D_MODEL], F32, "ExternalOutput")
        self.xsp = nc.dram_tensor("xsp", [128, NT * D_MODEL], F32).ap()
        self.ccK_in = [nc.dram_tensor(f"ccK_in{j}", [128, 2048], BF16).ap() for j in range(3)]
        self.ccK_out = [nc.dram_tensor(f"ccK_out{j}", [512, 2048], BF16).ap() for j in range(3)]
        self.ccV_in = [nc.dram_tensor(f"ccV_in{j}", [128, 2048], BF16).ap() for j in range(3)]
        self.ccV_out = [nc.dram_tensor(f"ccV_out{j}", [512, 2048], BF16).ap() for j in range(3)]
        self.ccB_in = nc.dram_tensor("ccB_in", [128, 276], F32).ap()
        self.ccB_out = nc.dram_tensor("ccB_out", [512, 276], F32).ap()
        for t in self.taps:
            self.tapouts[t] = dram("tap_" + t, [128, NT * D_MODEL], F32, "ExternalOutput")

        with ExitStack() as st:
            sb = lambda name, shape, dt: st.enter_context(nc.sbuf_tensor(name, shape, dt))
            self.arena = sb("arena", [128, ARENA // 2], BF16)
            self.cf = sb("cf_sb", [128, NCS], F32)
            self.identb = sb("identb", [128, 128], BF16)
            self.mask01 = sb("mask01", [128, 384], BF16)
            self.identR = sb("identR", [128, 4, 128], BF16)
            self.cmask = sb("cmask_sb", [128, 4, 512], BF16)
            self.c256b = sb("c256b", [128, 1], BF16)
            self.cosr = sb("cosr", [128, NT, 32], F32)
            self.sinr = sb("sinr", [128, NT, 32], F32)
            self.cosm = sb("cosm", [128, NT, 8], F32)
            self.sinm = sb("sinm", [128, NT, 8], F32)
            self.posi = sb("posi", [128, NT], I32)
            self.posf = sb("posf", [128, NT], F32)
            self.ms = sb("ms", [128, NT], F32)
            self.rstd = sb("rstd", [128, NT], F32)
            self.cpar = sb("cpar", [128, 2, 34], F32)
            self.state = sb("state", [128, 3, 64], F32)
            self.stbf = sb("stbf", [128, 3, 64], BF16)
            self.kmsb = sb("kmsb", [128, 24], F32)
            self.kmall = sb("kmall", [64, 6, 32], F32)
            self.kmhi = sb("kmhi", [64, 6, 32], BF16)
            self.kmlo = sb("kmlo", [64, 6, 32], BF16)
            self.kmt = sb("kmt", [64, 6, 32], F32)
            self.send = sb("send", [128, 4, 192], F32)
            self.tails = sb("tails", [128, 4, 60], F32)
            self.tail_f = sb("tail_f", [128, 2, 30], F32)
            self.halo = sb("halo", [128, 60], F32)
            self.mtpad = [sb(f"mtpad{i}", [128, 96], BF16) for i in range(4)]
            self.g1 = [sb(f"g1_{i}", [128, 32], F32) for i in range(4)]
            self.m8 = [sb(f"m8_{i}", [128, 8], F32) for i in range(4)]
            self.t1 = [sb(f"t1_{i}", [128, 32], F32) for i in range(4)]
            self.ssq = sb("ssq", [128, 6], F32)
            self.psf = [st.enter_context(nc.psum_tensor(f"psf{i}", [128, 512], F32)) for i in range(6)]
            self.psb = [st.enter_context(nc.psum_tensor(f"psb{i}", [128, 1024], BF16)) for i in range(2)]
            self.tcount = 0
            self.body()
            S.emit()
        return nc

    def cp_eng(self):
        self.tcount += 1
        return "act" if self.tcount % 2 else "dve"

    def copy(self, eng, out, in_, reads, writes):
        if eng == "act":
            self.S.op("act", lambda e: e.activation(out=out, in_=in_, func=AF.Copy), reads=reads, writes=writes)
        else:
            self.S.op(eng, lambda e: e.tensor_copy(out=out, in_=in_), reads=reads, writes=writes)

    def body(self):
        S, nc = self.S, self.nc
        self.setup()
        self.load_x()
        for l in range(self.D):
            self.ffn(l, 1)
            if "x_ffn1" in self.taps and l == 0:
                self.tap_x("x_ffn1")
            if self.stop == "ffn1":
                break
            self.mixer(l)
            if self.stop is not None:
                S.barrier()
                S.dma("sp", lambda e: e.dma_start(out=self.carve(A_X, [128, NT, D_MODEL], F32), in_=self.xsp.rearrange("p (t f) -> p t f", t=NT)),
                      reads=["xsp"], writes=[f"x{t}" for t in range(NT)])
                break
            if "x_mix" in self.taps and l == 0:
                self.tap_x("x_mix")
            self.ffn(l, 2)
        if self.do_final:
            self.final()
        else:
            self.store_x()
        S.barrier(("sp",))

    def tap_x(self, name):
        S = self.S
        X = self.carve(A_X, [128, NT, D_MODEL], F32)
        S.dma("sp", lambda e: e.dma_start(out=self.tapouts[name].rearrange("p (t f) -> p t f", t=NT), in_=X),
              reads=[f"x{t}" for t in range(NT)], writes=["tap_" + name])

    def tap_buf(self, name, view, reads, width):
        S = self.S
        S.dma("sp", lambda e: e.dma_start(out=self.tapouts[name][:, 0:width], in_=view), reads=reads, writes=["tap_" + name])

    def setup(self):
        S = self.S
        S.dma("sp", lambda e: e.dma_start(out=self.cf[:], in_=self.cf_in[:, 0:NCS]), writes=["cf"])
        S.dma("sp", lambda e: e.dma_start(out=self.posi[:], in_=self.pos_in), writes=["posi"])
        o, w = CF["identf"]
        S.dma("pool", lambda e: e.dma_start(out=self.identb[:], in_=self.cf_in[:, o:o + w]), writes=["identb"])
        o2, w2 = CF["mask01"]
        S.dma("pool", lambda e: e.dma_start(out=self.mask01[:], in_=self.cf_in[:, o2:o2 + w2]), writes=["mask01"])
        o3, w3 = CF["identR"]
        S.dma("pool", lambda e: e.dma_start(out=self.identR[:], in_=self.cf_in[:, o3:o3 + w3].rearrange("p (a b) -> p a b", a=4)),
              writes=["identR"])
        S.dma("pool", lambda e: e.dma_start(out=self.cmask[:], in_=self.cm_in.rearrange("p (a b) -> p a b", a=4)), writes=["cmask"])
        S.op("dve", lambda e: e.memset(self.c256b[:], 1.0 / 256), writes=["c256b"])
        self.KT = [self.carve(A_KT + i * 4096, [128, 2048], BF16) for i in range(4)]
        self.VR = [self.carve(A_VR + i * 2112, [128, 16, 66], BF16) for i in range(4)]
        for i in range(4):
            S.op("pool", lambda e, i=i: e.memset(self.mtpad[i][:], 0.0), writes=[f"mtpad{i}"])
        S.op("dve", lambda e: e.tensor_copy(out=self.posf[:], in_=self.posi[:]), reads=["posi"], writes=["posf"])
        tmp = self.carve(A_TMP, [128, NT, 32], F32)
        tmp2 = self.carve(A_TMP + 2048, [128, NT, 32], F32)
        tmpi = self.carve(A_TMP + 4096, [128, NT, 32], I32)
        tmp3 = self.carve(A_TMP + 6144, [128, NT, 32], F32)
        C1 = 6.28125
        C2 = 2 * math.pi - C1
        for (nf, inv, cosT, sinT) in ((32, "invr", self.cosr, self.sinr), (8, "invm", self.cosm, self.sinm)):
            ang, kf, ki, r2 = tmp[:, :, 0:nf], tmp2[:, :, 0:nf], tmpi[:, :, 0:nf], tmp3[:, :, 0:nf]
            invv = self.cfv(inv)
            for t in range(NT):
                S.op("dve", lambda e, t=t, ang=ang, invv=invv: e.tensor_scalar(
                    out=ang[:, t, :], in0=invv, scalar1=self.posf[:, t:t + 1], scalar2=None, op0=ALU.mult),
                    reads=["posf", "cf"], writes=["rt_ang"], signal=(t == NT - 1))
            S.op("dve", lambda e, ang=ang, kf=kf: e.tensor_scalar(out=kf, in0=ang, scalar1=1.0 / (2 * math.pi), scalar2=None, op0=ALU.mult),
                 reads=["rt_ang"], writes=["rt_kf"])
            S.op("dve", lambda e, ki=ki, kf=kf: e.tensor_copy(out=ki, in_=kf), reads=["rt_kf"], writes=["rt_ki"])
            S.op("dve", lambda e, ki=ki, kf=kf: e.tensor_copy(out=kf, in_=ki), reads=["rt_ki"], writes=["rt_kf"])
            S.op("dve", lambda e, ang=ang, kf=kf: e.scalar_tensor_tensor(out=ang, in0=kf, scalar=-C1, in1=ang, op0=ALU.mult, op1=ALU.add),
                 reads=["rt_kf", "rt_ang"], writes=["rt_ang"])
            S.op("dve", lambda e, ang=ang, kf=kf: e.scalar_tensor_tensor(out=ang, in0=kf, scalar=-C2, in1=ang, op0=ALU.mult, op1=ALU.add),
                 reads=["rt_kf", "rt_ang"], writes=["rt_ang"])
            S.op("dve", lambda e, ang=ang: e.tensor_scalar(out=ang, in0=ang, scalar1=math.pi, scalar2=-math.pi, op0=ALU.min, op1=ALU.max),
                 reads=["rt_ang"], writes=["rt_ang"])
            S.op("act", lambda e, ang=ang, sinT=sinT: e.activation(out=sinT[:], in_=ang, func=AF.Sin), reads=["rt_ang"], writes=["rt_sin"])
            S.op("dve", lambda e, ang=ang, kf=kf: e.tensor_scalar(out=kf, in0=ang, scalar1=math.pi / 2, scalar2=math.pi, op0=ALU.add, op1=ALU.is_gt),
                 reads=["rt_ang"], writes=["rt_kf"])
            S.op("dve", lambda e, ang=ang, kf=kf, r2=r2: e.scalar_tensor_tensor(out=r2, in0=kf, scalar=-2 * math.pi, in1=ang, op0=ALU.mult, op1=ALU.add),
                 reads=["rt_kf", "rt_ang"], writes=["rt_r2"])
            S.op("dve", lambda e, r2=r2: e.tensor_scalar(out=r2, in0=r2, scalar1=math.pi / 2, scalar2=math.pi, op0=ALU.add, op1=ALU.min),
                 reads=["rt_r2"], writes=["rt_r2"])
            S.op("act", lambda e, r2=r2, cosT=cosT: e.activation(out=cosT[:], in_=r2, func=AF.Sin), reads=["rt_r2"], writes=["rt_cos"])
        S.barrier()

    def load_x(self):
        S = self.S
        X = self.carve(A_X, [128, NT, D_MODEL], F32)
        xin = self.x_in.rearrange("(t p) f -> p t f", p=128)
        for q in range(4):
            S.dma("sp", lambda e, q=q: e.dma_start(out=X[:, 4 * q:4 * q + 4, :], in_=xin[:, 4 * q:4 * q + 4, :]),
                  writes=[f"x{t}" for t in range(4 * q, 4 * q + 4)])

    def store_x(self):
        S = self.S
        X = self.carve(A_X, [128, NT, D_MODEL], F32)
        yo = self.y_out.rearrange("(t p) f -> p t f", p=128)
        for q in range(4):
            S.dma("sp", lambda e, q=q: e.dma_start(out=yo[:, 4 * q:4 * q + 4, :], in_=X[:, 4 * q:4 * q + 4, :]),
                  reads=[f"x{t}" for t in range(4 * q, 4 * q + 4)], writes=[f"y{q}"])

    def rms_stats(self):
        S = self.S
        X = self.carve(A_X, [128, NT, D_MODEL], F32)
        junk = self.carve(A_TMP + 8192, [128, D_MODEL], BF16)
        for t in range(NT):
            S.op("act", lambda e, t=t: e.activation(out=junk, in_=X[:, t, :], func=AF.Square, accum_out=self.ms[:, t:t + 1]),
                 reads=[f"x{t}"], writes=["junk", "ms"])
        S.op("dve", lambda e: e.tensor_scalar(out=self.rstd[:], in0=self.ms[:], scalar1=1.0 / D_MODEL, scalar2=EPS, op0=ALU.mult, op1=ALU.add),
             reads=["ms"], writes=["rstd"])
        S.op("act", lambda e: e.activation(out=self.rstd[:], in_=self.rstd[:], func=AF.Sqrt), reads=["rstd"], writes=["rstd"])
        S.op("dve", lambda e: e.reciprocal(out=self.rstd[:], in_=self.rstd[:]), reads=["rstd"], writes=["rstd"])

    def norm_to_hT(self, gain_ap):
        S = self.S
        X = self.carve(A_X, [128, NT, D_MODEL], F32)
        HT = self.carve(A_HT, [128, 8, TOK], BF16)
        gain = self.carve(A_GAIN, [128, D_MODEL], F32)
        S.dma("sp", lambda e: e.dma_start(out=gain, in_=gain_ap.to_broadcast([128, D_MODEL])), writes=["gain"])
        self.rms_stats()
        hn = [self.carve(A_TMP + 2048 * i, [128, D_MODEL], BF16) for i in range(2)]
        for t in range(NT):
            h = hn[t % 2]
            S.op("dve", lambda e, t=t, h=h: e.scalar_tensor_tensor(out=h, in0=X[:, t, :], scalar=self.rstd[:, t:t + 1], in1=gain,
                                                                  op0=ALU.mult, op1=ALU.mult),
                 reads=[f"x{t}", "rstd", "gain"], writes=[f"hn{t % 2}"])
            pb = self.psb[t % 2]
            for kc in range(8):
                S.op("pe", lambda e, kc=kc, h=h, pb=pb: e.transpose(out=pb[:, kc * 128:(kc + 1) * 128], in_=h[:, kc * 128:(kc + 1) * 128],
                                                                   identity=self.identb[:]),
                     reads=[f"hn{t % 2}", "identb"], writes=[f"psb{t % 2}"], signal=(kc == 7))
            self.copy(self.cp_eng(), HT[:, :, t * 128:(t + 1) * 128], pb.rearrange("p (a b) -> p a b", a=8),
                      reads=[f"psb{t % 2}"], writes=[f"hT{t}"])

    def ffn(self, l, f):
        S = self.S
        wg, wu, wd = self.w[f"wg{f}"], self.w[f"wu{f}"], self.w[f"wd{f}"]
        if f == 2:
            S.barrier()
        self.norm_to_hT(self.norms[l, (0 if f == 1 else 2):(1 if f == 1 else 3), :])
        X = self.carve(A_X, [128, NT, D_MODEL], F32)
        HT = self.carve(A_HT, [128, 8, TOK], BF16)
        ACT = self.carve(A_ACT, [128, 8, TOK], BF16)
        WD = self.carve(A_WD, [128, 8, D_MODEL], BF16)
        WGU = [self.carve(A_WGU + 4096 * i, [128, 2, 8, 128], BF16) for i in range(3)]
        sg = [self.carve(A_TMP + 4096 + 2048 * i, [128, 512], F32) for i in range(2)]
        wgv = wg[l].rearrange("(k p) f -> p k f", p=128)
        wuv = wu[l].rearrange("(k p) f -> p k f", p=128)
        wdv = wd[l].rearrange("(c p) f -> p c f", p=128)
        cnt = 0
        for (c0, c1) in ((0, 8), (8, 16), (16, 22)):
            ncp = c1 - c0
            S.dma("pool", lambda e, c0=c0, c1=c1, ncp=ncp: e.dma_start(out=WD[:, 0:ncp, :], in_=wdv[:, c0:c1, :]), writes=["wd"])
            for c in range(c0, c1):
                slot = c % 3
                W = WGU[slot]
                S.dma("pool", lambda e, c=c, W=W: e.dma_start(out=W[:, 0, :, :], in_=wgv[:, :, c * 128:(c + 1) * 128]), writes=[f"wgu{slot}g"])
                S.dma("pool", lambda e, c=c, W=W: e.dma_start(out=W[:, 1, :, :], in_=wuv[:, :, c * 128:(c + 1) * 128]), writes=[f"wgu{slot}u"])
                for tg in range(4):
                    pg, pu = self.psf[cnt % 2], self.psf[2 + cnt % 2]
                    kg, ku = f"psf{cnt % 2}", f"psf{2 + cnt % 2}"
                    hk = [f"hT{t}" for t in range(4 * tg, 4 * tg + 4)]
                    for kc in range(8):
                        S.op("pe", lambda e, kc=kc, W=W, pg=pg, tg=tg: e.matmul(pg[:], lhsT=W[:, 0, kc, :], rhs=HT[:, kc, tg * 512:(tg + 1) * 512],
                                                                             start=(kc == 0), stop=(kc == 7)),
                             reads=hk + [f"wgu{slot}g"], writes=[kg], signal=(kc == 7))
                    for kc in range(8):
                        S.op("pe", lambda e, kc=kc, W=W, pu=pu, tg=tg: e.matmul(pu[:], lhsT=W[:, 1, kc, :], rhs=HT[:, kc, tg * 512:(tg + 1) * 512],
                                                                             start=(kc == 0), stop=(kc == 7)),
                             reads=hk + [f"wgu{slot}u"], writes=[ku], signal=(kc == 7))
                    s_ = sg[cnt % 2]
                    S.op("act", lambda e, s_=s_, pg=pg: e.activation(out=s_, in_=pg[:], func=AF.Silu), reads=[kg], writes=[f"sg{cnt % 2}"])
                    S.op("dve", lambda e, s_=s_, pu=pu, c=c, c0=c0, tg=tg: e.tensor_tensor(
                        out=ACT[:, c - c0, tg * 512:(tg + 1) * 512], in0=s_, in1=pu[:], op=ALU.mult),
                        reads=[f"sg{cnt % 2}", ku], writes=[f"act{c - c0}_{tg}"])
                    cnt += 1
            for t in range(NT):
                for hf in range(2):
                    pd = self.psf[4 + (2 * t + hf) % 2]
                    kd = f"psf{4 + (2 * t + hf) % 2}"
                    for cc in range(ncp):
                        S.op("pe", lambda e, cc=cc, t=t, hf=hf, pd=pd, ncp=ncp: e.matmul(pd[:], lhsT=ACT[:, cc, t * 128:(t + 1) * 128],
                                                                             rhs=WD[:, cc, hf * 512:(hf + 1) * 512],
                                                                             start=(cc == 0), stop=(cc == ncp - 1)),
                             reads=[f"act{cc}_{t // 4}", "wd"], writes=[kd], signal=(cc == ncp - 1))
                    S.op("dve", lambda e, t=t, hf=hf, pd=pd: e.scalar_tensor_tensor(
                        out=X[:, t, hf * 512:(hf + 1) * 512], in0=pd[:], scalar=0.5, in1=X[:, t, hf * 512:(hf + 1) * 512],
                        op0=ALU.mult, op1=ALU.add), reads=[kd, f"x{t}"], writes=[f"x{t}"])

    def rotary(self, ps, out_bf, t, half, cosT, sinT, tmp):
        S = self.S
        a, b = tmp[0][:, :, 0:half], tmp[1][:, :, 0:half]
        cb = cosT[:, t:t + 1, :].to_broadcast([128, 6, half])
        sbb = sinT[:, t:t + 1, :].to_broadcast([128, 6, half])
        x1, x2 = ps[:, :, 0:half], ps[:, :, half:2 * half]
        rk, wk = self._rot_keys
        S.op("dve", lambda e: e.tensor_tensor(out=a, in0=x1, in1=cb, op=ALU.mult), reads=rk, writes=["rot_a"])
        S.op("dve", lambda e: e.tensor_tensor(out=b, in0=x2, in1=sbb, op=ALU.mult), reads=rk, writes=["rot_b"])
        S.op("dve", lambda e: e.tensor_tensor(out=out_bf[:, :, 0:half], in0=a, in1=b, op=ALU.subtract), reads=["rot_a", "rot_b"], writes=wk)
        S.op("dve", lambda e: e.tensor_tensor(out=a, in0=x2, in1=cb, op=ALU.mult), reads=rk, writes=["rot_a"])
        S.op("dve", lambda e: e.tensor_tensor(out=b, in0=x1, in1=sbb, op=ALU.mult), reads=rk, writes=["rot_b"])
        S.op("dve", lambda e: e.tensor_tensor(out=out_bf[:, :, half:2 * half], in0=a, in1=b, op=ALU.add), reads=["rot_a", "rot_b"], writes=wk)

    def mixer(self, l):
        S = self.S
        self.norm_to_hT(self.norms[l, 1:2, :])
        X = self.carve(A_X, [128, NT, D_MODEL], F32)
        xkeys = [f"x{t}" for t in range(NT)]
        S.dma("sp", lambda e: e.dma_start(out=self.xsp.rearrange("p (t f) -> p t f", t=NT), in_=X), reads=xkeys, writes=["xsp"])
        S.dma("pool", lambda e: e.dma_start(out=self.cpar[:], in_=self.convp[l]), writes=["cpar"])
        S.barrier()
        if self.stop == "spill":
            return
        HT = self.carve(A_HT, [128, 8, TOK], BF16)
        QRT = self.carve(A_QRT, [128, 3, TOK], BF16)
        KRT = self.carve(A_KRT, [128, 3, TOK], BF16)
        VTOK = self.carve(A_VTOK, [128, NT, 384], BF16)
        RG = self.carve(A_RG, [128, NT, 384], BF16)
        MQT = self.carve(A_MQT, [128, 3, TOK], BF16)
        MKT = self.carve(A_MKT, [128, 3, TOK], BF16)
        KVS = self.carve(A_KVS, [128, 8, 3, 64], F32)
        UT = self.carve(A_UT, [128, 2, 2080], BF16)
        WIN = [self.carve(A_WIN + 6144 * i, [128, 8, 384], BF16) for i in range(2)]
        CW = self.carve(A_CW, [128, 4, 8, 128], BF16)
        VIMG = self.carve(A_WOUT, [128, 3, NT, 128], BF16)
        tokb = [self.carve(A_TMP + 768 * i, [128, 6, 64], BF16) for i in range(3)]
        rtmp = [self.carve(A_TMP + 2304 + 768 * i, [128, 6, 32], F32) for i in range(2)]
        winv = self.w["w_in"][l].rearrange("(k p) f -> p k f", p=128)
        hkeys = [f"hT{t}" for t in range(NT)]
        qs = self.cfv("qs").rearrange("p (a b) -> p a b", a=2)
        ks = self.cfv("ks").rearrange("p (a b) -> p a b", a=2)
        bc6 = lambda v: v.unsqueeze(2).to_broadcast([128, 6, 64])
        pcnt = 0
        tb = 0
        groups = [("rv", 768), ("rg", 1152), ("rk", 384), ("rq", 0), ("mv", 2304), ("mk", 1920), ("mq", 1536)]
        for gi, (gname, c0) in enumerate(groups):
            if self.stop is not None and self.stop.startswith("g:") and gi >= int(self.stop[2:]):
                return
            W = WIN[gi % 2]
            S.dma("pool", lambda e, W=W, c0=c0: e.dma_start(out=W, in_=winv[:, :, c0:c0 + 384]), writes=[f"win{gi % 2}"])
            for t in range(NT):
                ps = self.psf[pcnt % 2]
                pk = f"psf{pcnt % 2}"
                pcnt += 1
                for kc in range(8):
                    S.op("pe", lambda e, kc=kc, t=t, ps=ps, W=W: e.matmul(ps[:, 0:384], lhsT=HT[:, kc, t * 128:(t + 1) * 128], rhs=W[:, kc, :],
                                                                         start=(kc == 0), stop=(kc == 7)),
                         reads=[f"hT{t}", f"win{gi % 2}"], writes=[pk], signal=(kc == 7))
                psv = ps[:, 0:384].rearrange("p (h d) -> p h d", h=6)
                if gname == "rv":
                    S.op("act", lambda e, t=t, ps=ps: e.activation(out=VTOK[:, t, :], in_=ps[:, 0:384], func=AF.Copy), reads=[pk], writes=[f"vtok{t}"])
                elif gname == "rg":
                    S.op("act", lambda e, t=t, ps=ps: e.activation(out=RG[:, t, :], in_=ps[:, 0:384], func=AF.Silu), reads=[pk], writes=[f"rg{t}"])
                elif gname in ("rk", "rq"):
                    ob = tokb[tb % 3]
                    obk = f"tokb{tb % 3}"
                    tb += 1
                    self._rot_keys = ([pk, "rt_cos", "rt_sin"], [obk])
                    self.rotary(psv, ob, t, 32, self.cosr, self.sinr, rtmp)
                    sc = bc6((ks if gname == "rk" else qs)[:, t % 2, :])
                    obf = ob.rearrange("p h d -> p (h d)")
                    S.op("dve", lambda e, ob=ob, sc=sc: e.tensor_tensor(out=ob, in0=ob, in1=sc, op=ALU.mult), reads=[obk, "cf"], writes=[obk])
                    pb = self.psb[t % 2]
                    for pr in range(3):
                        S.op("pe", lambda e, pr=pr, pb=pb, obf=obf: e.transpose(out=pb[:, pr * 128:(pr + 1) * 128], in_=obf[:, pr * 128:(pr + 1) * 128],
                                                                               identity=self.identb[:]),
                             reads=[obk, "identb"], writes=[f"psb{t % 2}"], signal=(pr == 2))
                    dst = (KRT if gname == "rk" else QRT)
                    dk = ("krt" if gname == "rk" else "qrt") + str(t)
                    self.copy(self.cp_eng(), dst[:, :, t * 128:(t + 1) * 128], pb[:, 0:384].rearrange("p (a b) -> p a b", a=3),
                              reads=[f"psb{t % 2}"], writes=[dk])
                    if gname == "rk":
                        pkv = self.psf[2]
                        for pr in range(3):
                            S.op("pe", lambda e, pr=pr, t=t, obf=obf, pkv=pkv: e.matmul(
                                pkv[:, pr * 128:(pr + 1) * 128], lhsT=obf[:, pr * 128:(pr + 1) * 128], rhs=VTOK[:, t, pr * 128:(pr + 1) * 128],
                                start=(t % 2 == 0 and pr == 0), stop=(t % 2 == 1 and pr == 2)),
                                reads=[obk, f"vtok{t}"], writes=["psf2"], signal=(pr == 2))
                        if t % 2 == 1:
                            ch = t // 2
                            pv = pkv[:, 0:384].rearrange("p (a b) -> p a b", a=3)
                            S.op("act", lambda e, ch=ch, pv=pv: e.activation(out=KVS[0:64, ch, :, :], in_=pv[0:64, :, 0:64], func=AF.Copy),
                                 reads=["psf2"], writes=[f"kvs{ch}a"])
                            S.op("dve", lambda e, ch=ch, pv=pv: e.tensor_copy(out=KVS[64:128, ch, :, :], in_=pv[64:128, :, 64:128]),
                                 reads=["psf2"], writes=[f"kvs{ch}b"])
                elif gname == "mv":
                    S.op("act", lambda e, t=t, ps=ps: e.activation(out=VIMG[:, :, t, :], in_=ps[:, 0:384].rearrange("p (a b) -> p a b", a=3), func=AF.Copy),
                         reads=[pk], writes=[f"vimg{t}"])
                elif gname in ("mk", "mq"):
                    ob = tokb[tb % 3]
                    obk = f"tokb{tb % 3}"
                    tb += 1
                    obf = ob.rearrange("p h d -> p (h d)")
                    S.op("dve", lambda e, ob=ob, psv=psv: e.tensor_copy(out=ob[:, :, 16:64], in_=psv[:, :, 16:64]), reads=[pk], writes=[obk])
                    self._rot_keys = ([pk, "rt_cos", "rt_sin"], [obk])
                    self.rotary(psv, ob, t, 8, self.cosm, self.sinm, rtmp)
                    pb = self.psb[t % 2]
                    for pr in range(3):
                        S.op("pe", lambda e, pr=pr, pb=pb, obf=obf: e.transpose(out=pb[:, pr * 128:(pr + 1) * 128], in_=obf[:, pr * 128:(pr + 1) * 128],
                                                                               identity=self.identb[:]),
                             reads=[obk, "identb"], writes=[f"psb{t % 2}"], signal=(pr == 2))
                    dst = (MKT if gname == "mk" else MQT)
                    dk = ("mkt" if gname == "mk" else "mqt") + str(t)
                    self.copy(self.cp_eng(), dst[:, :, t * 128:(t + 1) * 128], pb[:, 0:384].rearrange("p (a b) -> p a b", a=3),
                              reads=[f"psb{t % 2}"], writes=[dk])
        if self.stop == "g:7":
            return
        S.op("dve", lambda e: e.tensor_reduce(out=self.kmsb[:], in_=MKT.rearrange("p a (b c) -> p (a b) c", c=256),
                                              axis=mybir.AxisListType.X, op=ALU.add),
             reads=[f"mkt{t}" for t in range(NT)], writes=["kmsb"])
        S.op("dve", lambda e: e.tensor_scalar(out=self.kmsb[:], in0=self.kmsb[:], scalar1=1.0 / 256, scalar2=None, op0=ALU.mult),
             reads=["kmsb"], writes=["kmsb"])
        S.dma("sp", lambda e: e.dma_start(out=self.ccB_in[:, 0:24], in_=self.kmsb[:]), reads=["kmsb"], writes=["ccB_in"])
        for pr in range(3):
            S.dma("sp", lambda e, pr=pr: e.dma_start(out=self.ccK_in[pr], in_=MKT[:, pr, :]),
                  reads=[f"mkt{t}" for t in range(NT)], writes=[f"ccK_in{pr}"])
            S.dma("sp", lambda e, pr=pr: e.dma_start(out=self.ccV_in[pr], in_=VIMG[:, pr, :, :].rearrange("p t c -> p (t c)")),
                  reads=[f"vimg{t}" for t in range(NT)], writes=[f"ccV_in{pr}"])
        dec = self.cfv("dec").unsqueeze(2).to_broadcast([128, 3, 64])
        S.op("dve", lambda e: e.tensor_copy(out=self.state[:], in_=KVS[:, 0, :, :]), reads=["kvs0a", "kvs0b"], writes=["state"])
        S.op("dve", lambda e: e.tensor_tensor(out=self.state[:], in0=self.state[:], in1=dec, op=ALU.mult), reads=["state", "cf"], writes=["state"])
        for ch in range(1, 8):
            S.op("dve", lambda e, ch=ch: e.tensor_tensor(out=self.state[:], in0=self.state[:], in1=KVS[:, ch, :, :], op=ALU.add),
                 reads=["state", f"kvs{ch}a", f"kvs{ch}b"], writes=["state"])
            S.op("dve", lambda e: e.tensor_tensor(out=self.state[:], in0=self.state[:], in1=dec, op=ALU.mult), reads=["state", "cf"], writes=["state"])
        S.dma("sp", lambda e: e.dma_start(out=self.ccB_in[:, 24:216], in_=self.state[:].rearrange("p a b -> p (a b)")),
              reads=["state"], writes=["ccB_in"])
        for j, c0 in enumerate((2688, 2816, 2944, 3072)):
            S.dma("pool", lambda e, j=j, c0=c0: e.dma_start(out=CW[:, j, :, :], in_=winv[:, :, c0:c0 + 128]), writes=[f"cw{j}"])
        sgm = [self.carve(A_TMP + 4096 + 2048 * i, [128, 512], F32) for i in range(2)]
        cc_ = 0
        for c in range(2):
            for tg in range(4):
                pa, pg = self.psf[cc_ % 2], self.psf[4 + cc_ % 2]
                ka, kg = f"psf{cc_ % 2}", f"psf{4 + cc_ % 2}"
                hk = [f"hT{t}" for t in range(4 * tg, 4 * tg + 4)]
                for kc in range(8):
                    S.op("pe", lambda e, kc=kc, c=c, tg=tg, pa=pa: e.matmul(pa[:], lhsT=CW[:, c, kc, :], rhs=HT[:, kc, tg * 512:(tg + 1) * 512],
                                                                         start=(kc == 0), stop=(kc == 7)),
                         reads=hk + [f"cw{c}"], writes=[ka], signal=(kc == 7))
                for kc in range(8):
                    S.op("pe", lambda e, kc=kc, c=c, tg=tg, pg=pg: e.matmul(pg[:], lhsT=CW[:, 2 + c, kc, :], rhs=HT[:, kc, tg * 512:(tg + 1) * 512],
                                                                         start=(kc == 0), stop=(kc == 7)),
                         reads=hk + [f"cw{2 + c}"], writes=[kg], signal=(kc == 7))
                s_ = sgm[cc_ % 2]
                S.op("act", lambda e, s_=s_, pg=pg: e.activation(out=s_, in_=pg[:], func=AF.Sigmoid), reads=[kg], writes=[f"sgm{cc_ % 2}"])
                S.op("dve", lambda e, s_=s_, pa=pa, c=c, tg=tg: e.tensor_tensor(out=UT[:, c, 32 + tg * 512:32 + (tg + 1) * 512], in0=pa[:], in1=s_, op=ALU.mult),
                     reads=[ka, f"sgm{cc_ % 2}"], writes=[f"ut{c}_{tg}"])
                cc_ += 1
        S.op("dve", lambda e: e.tensor_copy(out=self.tail_f[:], in_=UT[:, :, 2050:2080]), reads=["ut0_3", "ut1_3"], writes=["tail_f"])
        S.dma("sp", lambda e: e.dma_start(out=self.ccB_in[:, 216:276], in_=self.tail_f[:].rearrange("p a b -> p (a b)")),
              reads=["tail_f"], writes=["ccB_in"])
        if self.stop == "proj":
            return
        S.cc(lambda e: e.collective_compute("AllGather", ALU.bypass, replica_groups=[[0, 1, 2, 3], [4, 5, 6, 7]],
                                            ins=[self.ccB_in], outs=[self.ccB_out]), reads=["ccB_in"], writes=["ccB_out"])
        for pr in range(3):
            S.cc(lambda e, pr=pr: e.collective_compute("AllGather", ALU.bypass, replica_groups=[[0, 1, 2, 3], [4, 5, 6, 7]],
                                                       ins=[self.ccK_in[pr]], outs=[self.ccK_out[pr]]), reads=[f"ccK_in{pr}"], writes=[f"ccK_out{pr}"])
            S.cc(lambda e, pr=pr: e.collective_compute("AllGather", ALU.bypass, replica_groups=[[0, 1, 2, 3], [4, 5, 6, 7]],
                                                       ins=[self.ccV_in[pr]], outs=[self.ccV_out[pr]]), reads=[f"ccV_in{pr}"], writes=[f"ccV_out{pr}"])
        ccBv = self.ccB_out.rearrange("(r p) w -> p r w", p=128)
        S.dma("sp", lambda e: e.dma_start(out=self.send[:], in_=ccBv[:, :, 24:216]), reads=["ccB_out"], writes=["send"])
        S.dma("sp", lambda e: e.dma_start(out=self.tails[:], in_=ccBv[:, :, 216:276]), reads=["ccB_out"], writes=["tails"])
        for h in range(6):
            S.dma("sp", lambda e, h=h: e.dma_start(
                out=self.kmall[:, h, :].rearrange("p (r n) -> p r n", r=4),
                in_=ccBv[(h % 2) * 64:(h % 2) * 64 + 64, :, (h // 2) * 8:(h // 2) * 8 + 8]), reads=["ccB_out"], writes=["kmall"])
        if self.stop == "cc":
            return
        coefr = self.cfv("coef").rearrange("p (r a) -> p r a", r=4)
        coef = [coefr[:, r_, :].unsqueeze(2).to_broadcast([128, 3, 64]) for r_ in range(4)]
        sv = self.send[:].rearrange("p r (a b) -> p r a b", a=3)
        stt = self.carve(A_TMP + 3840, [128, 3, 64], F32)
        S.op("dve", lambda e: e.tensor_tensor(out=self.state[:], in0=sv[:, 0], in1=coef[0], op=ALU.mult), reads=["send", "cf", "state"], writes=["state"])
        for r_ in range(1, 4):
            S.op("dve", lambda e, r_=r_: e.tensor_tensor(out=stt, in0=sv[:, r_], in1=coef[r_], op=ALU.mult), reads=["send", "cf"], writes=["stt"])
            S.op("dve", lambda e: e.tensor_tensor(out=self.state[:], in0=self.state[:], in1=stt, op=ALU.add), reads=["state", "stt"], writes=["state"])
        MIXT = self.carve(A_HT, [128, 8, TOK], BF16)
        ptb = [self.carve(A_TMP + 4608 + 768 * i, [128, 384], BF16) for i in range(3)]
        ytok = [self.carve(A_TMP + 6912 + 768 * i, [128, 384], BF16) for i in range(2)]
        ysq = self.carve(A_TMP + 8448, [128, 6, 64], F32)
        rs6 = self.carve(A_TMP + 9984, [128, 6], F32)
        pti = 0
        for ch in range(8):
            S.op("act", lambda e: e.activation(out=self.stbf[:], in_=self.state[:], func=AF.Copy), reads=["state"], writes=["stbf"])
            t0, t1 = 2 * ch, 2 * ch + 1
            po = [self.psf[2], self.psf[3]]
            pok = ["psf2", "psf3"]
            first = [True, True]
            for h in range(6):
                pr, hh = h // 2, h % 2
                prt = slice(hh * 64, hh * 64 + 64)
                ps = self.psf[pti % 2]
                psk = f"psf{pti % 2}"
                S.op("pe", lambda e, ps=ps, pr=pr, prt=prt, t0=t0: e.matmul(ps[:, 0:256], lhsT=KRT[prt, pr, t0 * 128:(t0 + 1) * 128],
                                                                         rhs=QRT[prt, pr, t0 * 128:(t0 + 2) * 128], start=True, stop=False),
                     reads=[f"krt{t0}", f"qrt{t0}", f"qrt{t1}"], writes=[psk], signal=False)
                S.op("pe", lambda e, ps=ps, pr=pr, prt=prt, t1=t1: e.matmul(ps[:, 256:384], lhsT=KRT[prt, pr, t1 * 128:(t1 + 1) * 128],
                                                                         rhs=QRT[prt, pr, t1 * 128:(t1 + 1) * 128], start=False, stop=True),
                     reads=[f"krt{t1}", f"qrt{t1}"], writes=[psk])
                pt = ptb[pti % 3]
                ptk = f"ptb{pti % 3}"
                pti += 1
                S.op("dve", lambda e, pt=pt, ps=ps: e.tensor_tensor(out=pt, in0=ps[:, 0:384], in1=self.mask01[:], op=ALU.mult),
                     reads=[psk, "mask01"], writes=[ptk])
                hc = slice(h * 64, h * 64 + 64)
                S.op("pe", lambda e, pt=pt, hc=hc, t0=t0, f0=first[0]: e.matmul(po[0][:, hc], lhsT=pt[:, 0:128], rhs=VTOK[:, t0, hc], start=f0, stop=False),
                     reads=[ptk, f"vtok{t0}"], writes=[pok[0]], signal=False)
                first[0] = False
                S.op("pe", lambda e, hc=hc, pr=pr, prt=prt, t0=t0, h=h: e.matmul(po[0][:, hc], lhsT=QRT[prt, pr, t0 * 128:(t0 + 1) * 128],
                                                                              rhs=self.stbf[prt, pr, :], start=False, stop=(h == 5)),
                     reads=[f"qrt{t0}", "stbf"], writes=[pok[0]], signal=(h == 5))
                S.op("pe", lambda e, pt=pt, hc=hc, t0=t0, f1=first[1]: e.matmul(po[1][:, hc], lhsT=pt[:, 128:256], rhs=VTOK[:, t0, hc], start=f1, stop=False),
                     reads=[ptk, f"vtok{t0}"], writes=[pok[1]], signal=False)
                first[1] = False
                S.op("pe", lambda e, pt=pt, hc=hc, t1=t1: e.matmul(po[1][:, hc], lhsT=pt[:, 256:384], rhs=VTOK[:, t1, hc], start=False, stop=False),
                     reads=[ptk, f"vtok{t1}"], writes=[pok[1]], signal=False)
                S.op("pe", lambda e, hc=hc, pr=pr, prt=prt, t1=t1, h=h: e.matmul(po[1][:, hc], lhsT=QRT[prt, pr, t1 * 128:(t1 + 1) * 128],
                                                                              rhs=self.stbf[prt, pr, :], start=False, stop=(h == 5)),
                     reads=[f"qrt{t1}", "stbf"], writes=[pok[1]], signal=(h == 5))
            for ci, t in enumerate((t0, t1)):
                pv = po[ci][:, 0:384].rearrange("p (h d) -> p h d", h=6)
                S.op("act", lambda e, pv=pv: e.activation(out=ysq, in_=pv, func=AF.Square), reads=[pok[ci]], writes=["ysq"])
                S.op("dve", lambda e: e.tensor_reduce(out=self.ssq[:], in_=ysq, axis=mybir.AxisListType.X, op=ALU.add), reads=["ysq"], writes=["ssq"])
                S.op("dve", lambda e: e.tensor_scalar(out=rs6, in0=self.ssq[:], scalar1=1.0 / 64, scalar2=EPS, op0=ALU.mult, op1=ALU.add),
                     reads=["ssq"], writes=["rs6"])
                S.op("act", lambda e: e.activation(out=rs6, in_=rs6, func=AF.Sqrt), reads=["rs6"], writes=["rs6"])
                S.op("dve", lambda e: e.reciprocal(out=rs6, in_=rs6), reads=["rs6"], writes=["rs6"])
                yt = ytok[t % 2]
                ytk = f"ytok{t % 2}"
                rgv = RG[:, t, :].rearrange("p (h d) -> p h d", h=6)
                ytv = yt.rearrange("p (h d) -> p h d", h=6)
                for h in range(6):
                    S.op("dve", lambda e, h=h, pv=pv, ytv=ytv, rgv=rgv: e.scalar_tensor_tensor(
                        out=ytv[:, h, :], in0=pv[:, h, :], scalar=rs6[:, h:h + 1], in1=rgv[:, h, :], op0=ALU.mult, op1=ALU.mult),
                        reads=[pok[ci], "rs6", f"rg{t}"], writes=[ytk], signal=(h == 5))
                pb = self.psb[t % 2]
                for pr in range(3):
                    S.op("pe", lambda e, pr=pr, pb=pb, yt=yt: e.transpose(out=pb[:, pr * 128:(pr + 1) * 128], in_=yt[:, pr * 128:(pr + 1) * 128],
                                                                         identity=self.identb[:]),
                         reads=[ytk, "identb"], writes=[f"psb{t % 2}"], signal=(pr == 2))
                self.copy(self.cp_eng(), MIXT[:, 0:3, t * 128:(t + 1) * 128], pb[:, 0:384].rearrange("p (a b) -> p a b", a=3),
                          reads=[f"psb{t % 2}"], writes=[f"mixT{t}"])
            S.op("dve", lambda e, ch=ch: e.tensor_tensor(out=self.state[:], in0=self.state[:], in1=KVS[:, ch, :, :], op=ALU.add),
                 reads=["state", f"kvs{ch}a", f"kvs{ch}b"], writes=["state"])
            S.op("dve", lambda e: e.tensor_tensor(out=self.state[:], in0=self.state[:], in1=dec, op=ALU.mult), reads=["state", "cf"], writes=["state"])
        if "mixret" in self.taps and l == 0:
            S.barrier()
            self.tap_mixT("mixret")
        S.barrier()
        if self.stop == "ret":
            return
        self.conv(l)
        S.barrier()
        if self.stop == "conv":
            return
        self.moba(l)
        S.barrier()
        if self.stop == "moba":
            return
        if "mixT" in self.taps and l == 0:
            self.tap_mixT("mixT")
            S.barrier()
        WOUT = self.carve(A_WOUT, [128, 8, D_MODEL], BF16)
        S.dma("pool", lambda e: e.dma_start(out=WOUT, in_=self.w["w_out"][l].rearrange("(k p) f -> p k f", p=128)),
              writes=["wout", "gain"] + [f"vimg{t}" for t in range(NT)])
        S.dma("sp", lambda e: e.dma_start(out=X, in_=self.xsp.rearrange("p (t f) -> p t f", t=NT)), reads=["xsp"], writes=xkeys)
        for t in range(NT):
            for hf in range(2):
                pd = self.psf[4 + (2 * t + hf) % 2]
                kd = f"psf{4 + (2 * t + hf) % 2}"
                for kc in range(8):
                    S.op("pe", lambda e, kc=kc, t=t, hf=hf, pd=pd: e.matmul(pd[:], lhsT=MIXT[:, kc, t * 128:(t + 1) * 128],
                                                                         rhs=WOUT[:, kc, hf * 512:(hf + 1) * 512], start=(kc == 0), stop=(kc == 7)),
                         reads=["wout"], writes=[kd], signal=(kc == 7))
                S.op("dve", lambda e, t=t, hf=hf, pd=pd: e.tensor_tensor(out=X[:, t, hf * 512:(hf + 1) * 512], in0=pd[:],
                                                                       in1=X[:, t, hf * 512:(hf + 1) * 512], op=ALU.add),
                     reads=[kd, f"x{t}"], writes=[f"x{t}"])

    def tap_mixT(self, name):
        S = self.S
        MIXT = self.carve(A_HT, [128, 8, TOK], BF16)
        tmp = self.carve(A_CONVTMP, [128, TOK], F32)
        for kc in range(8):
            S.op("dve", lambda e, kc=kc: e.tensor_copy(out=tmp, in_=MIXT[:, kc, :]), writes=["taptmp"])
            S.dma("sp", lambda e, kc=kc: e.dma_start(out=self.tapouts[name][:, kc * TOK:(kc + 1) * TOK], in_=tmp), reads=["taptmp"],
                  writes=["tap_" + name + str(kc)])
        S.barrier()

    def conv(self, l):
        S = self.S
        UT = self.carve(A_UT, [128, 2, 2080], BF16)
        MIXT = self.carve(A_HT, [128, 8, TOK], BF16)
        sel = self.cfv("sel")
        identf = self.cfv("identf")
        bd = self.cfv("bd")
        tl = self.tails[:]
        S.op("dve", lambda e: e.tensor_scalar(out=self.halo[:], in0=tl[:, 0, :], scalar1=sel[:, 0:1], scalar2=None, op0=ALU.mult),
             reads=["tails", "cf"], writes=["halo"])
        for r_ in range(1, 4):
            S.op("dve", lambda e, r_=r_: e.scalar_tensor_tensor(out=self.halo[:], in0=tl[:, r_, :], scalar=sel[:, r_:r_ + 1], in1=self.halo[:],
                                                               op0=ALU.mult, op1=ALU.add), reads=["tails", "cf", "halo"], writes=["halo"])
        S.op("dve", lambda e: e.tensor_copy(out=UT[:, :, 2:32], in_=self.halo[:].rearrange("p (a b) -> p a b", a=2)), reads=["halo"], writes=["ut_halo"])
        diag = [self.carve(A_CONVTMP + 256 * i, [128, 128], BF16) for i in range(4)]
        ysb = self.carve(A_CONVTMP + 1024, [128, 512], F32)
        ysq = self.carve(A_CONVTMP + 3072, [128, 512], F32)
        msb = self.carve(A_CONVTMP + 5120, [128, 512], F32)
        var = self.carve(A_CONVTMP + 7168, [128, 512], F32)
        dj = 0
        for c in range(2):
            for j in range(31):
                dg = diag[dj % 4]
                dk = f"diag{dj % 4}"
                dj += 1
                S.op("dve", lambda e, dg=dg, c=c, j=j: e.tensor_scalar(out=dg, in0=identf, scalar1=self.cpar[:, c, j:j + 1], scalar2=None, op0=ALU.mult),
                     reads=["cf", "cpar"], writes=[dk])
                for tg in range(4):
                    S.op("pe", lambda e, dg=dg, c=c, j=j, tg=tg: e.matmul(self.psf[tg][:], lhsT=dg, rhs=UT[:, c, 2 + tg * 512 + j:2 + tg * 512 + j + 512],
                                                                         start=(j == 0), stop=(j == 30)),
                         reads=[dk, f"ut{c}_{tg}", "ut_halo"] + ([f"ut{c}_{tg - 1}"] if tg > 0 else []), writes=[f"psf{tg}"], signal=(tg == 3))
            for tg in range(4):
                pk = f"psf{tg}"
                S.op("act", lambda e, tg=tg, c=c: e.activation(out=ysb, in_=self.psf[tg][:], func=AF.Identity, bias=self.cpar[:, c, 31:32], scale=1.0),
                     reads=[pk, "cpar"], writes=["c_ysb"])
                S.op("act", lambda e: e.activation(out=ysq, in_=ysb, func=AF.Square), reads=["c_ysb"], writes=["c_ysq"])
                S.op("pe", lambda e: e.matmul(self.psf[4][:], lhsT=bd, rhs=ysb, start=True, stop=True), reads=["cf", "c_ysb"], writes=["psf4"])
                S.op("pe", lambda e: e.matmul(self.psf[5][:], lhsT=bd, rhs=ysq, start=True, stop=True), reads=["cf", "c_ysq"], writes=["psf5"])
                S.op("act", lambda e: e.activation(out=msb, in_=self.psf[4][:], func=AF.Copy), reads=["psf4"], writes=["c_msb"])
                S.op("dve", lambda e: e.tensor_tensor(out=var, in0=msb, in1=msb, op=ALU.mult), reads=["c_msb"], writes=["c_var"])
                S.op("dve", lambda e: e.tensor_tensor(out=var, in0=self.psf[5][:], in1=var, op=ALU.subtract), reads=["psf5", "c_var"], writes=["c_var"])
                S.op("dve", lambda e: e.tensor_scalar(out=var, in0=var, scalar1=EPS, scalar2=0.0, op0=ALU.add, op1=ALU.max), reads=["c_var"], writes=["c_var"])
                S.op("act", lambda e: e.activation(out=var, in_=var, func=AF.Sqrt), reads=["c_var"], writes=["c_var"])
                S.op("dve", lambda e: e.reciprocal(out=var, in_=var), reads=["c_var"], writes=["c_var"])
                S.op("dve", lambda e: e.tensor_tensor(out=ysb, in0=ysb, in1=msb, op=ALU.subtract), reads=["c_ysb", "c_msb"], writes=["c_ysb"])
                S.op("dve", lambda e: e.tensor_tensor(out=ysb, in0=ysb, in1=var, op=ALU.mult), reads=["c_ysb", "c_var"], writes=["c_ysb"])
                S.op("dve", lambda e, c=c: e.tensor_scalar(out=ysb, in0=ysb, scalar1=self.cpar[:, c, 32:33], scalar2=self.cpar[:, c, 33:34],
                                                          op0=ALU.mult, op1=ALU.add), reads=["c_ysb", "cpar"], writes=["c_ysb"])
                S.op("act", lambda e, c=c, tg=tg: e.activation(out=MIXT[:, 6 + c, tg * 512:(tg + 1) * 512], in_=ysb, func=AF.Silu),
                     reads=["c_ysb"], writes=[f"mixTc{c}_{tg}"])

    def moba(self, l):
        S = self.S
        MQT = self.carve(A_MQT, [128, 3, TOK], BF16)
        QAUG = self.carve(A_QAUG, [96, 6, TOK], BF16)
        MIXT = self.carve(A_HT, [128, 8, TOK], BF16)
        KT, VR = self.KT, self.VR
        onesf = self.cfv("onesf")
        cap = self.cfv("cap").rearrange("p (a b) -> p a b", a=8)
        pastneg = self.cfv("pastneg").rearrange("p (a b) -> p a b", a=8)
        futneg = self.cfv("futneg").rearrange("p (a b) -> p a b", a=8)
        for i in range(4):
            S.dma("pool", lambda e, i=i: e.dma_start(out=KT[i][64:96, :], in_=self.er_in[i]), writes=[f"kt{i}"])
            S.op("dve", lambda e, i=i: e.memset(VR[i][:, :, 64:66], 1.0), writes=[f"vr{i}"])
        for h in range(6):
            S.dma("sp", lambda e, h=h: e.dma_start(out=QAUG[0:64, h, :], in_=MQT[(h % 2) * 64:(h % 2) * 64 + 64, h // 2, :]),
                  reads=[f"mqt{t}" for t in range(NT)], writes=[f"qaug{h}"])
        S.op("dve", lambda e: e.tensor_copy(out=self.kmhi[:], in_=self.kmall[:]), reads=["kmall"], writes=["kmhi"])
        S.op("dve", lambda e: e.tensor_copy(out=self.kmt[:], in_=self.kmhi[:]), reads=["kmhi"], writes=["kmt"])
        S.op("dve", lambda e: e.tensor_tensor(out=self.kmt[:], in0=self.kmall[:], in1=self.kmt[:], op=ALU.subtract), reads=["kmall", "kmt"], writes=["kmt"])
        S.op("dve", lambda e: e.tensor_copy(out=self.kmlo[:], in_=self.kmt[:]), reads=["kmt"], writes=["kmlo"])
        gi = 0
        for h in range(6):
            for tg in range(4):
                pb = self.psb[(h * 4 + tg) % 2]
                pbk = f"psb{(h * 4 + tg) % 2}"
                for tt in range(4):
                    t = 4 * tg + tt
                    b = t // 2
                    pg = self.psf[gi % 4]
                    pgk = f"psf{gi % 4}"
                    g1, m8, t1, mt = self.g1[gi % 4], self.m8[gi % 4], self.t1[gi % 4], self.mtpad[gi % 4]
                    k_ = gi % 4
                    gi += 1
                    S.op("pe", lambda e, pg=pg, h=h, t=t: e.matmul(pg[:, 0:32], lhsT=QAUG[0:64, h, t * 128:(t + 1) * 128], rhs=self.kmhi[:, h, :],
                                                                 start=True, stop=False), reads=[f"qaug{h}", "kmhi"], writes=[pgk], signal=False)
                    S.op("pe", lambda e, pg=pg, h=h, t=t: e.matmul(pg[:, 0:32], lhsT=QAUG[0:64, h, t * 128:(t + 1) * 128], rhs=self.kmlo[:, h, :],
                                                                 start=False, stop=True), reads=[f"qaug{h}", "kmlo"], writes=[pgk])
                    S.op("dve", lambda e, pg=pg, g1=g1, b=b: e.tensor_tensor(out=g1[:], in0=pg[:, 0:32], in1=cap[:, b, :], op=ALU.min),
                         reads=[pgk, "cf"], writes=[f"g1_{k_}"])
                    S.op("dve", lambda e, g1=g1, m8=m8: e.max(out=m8[:], in_=g1[:]), reads=[f"g1_{k_}"], writes=[f"m8_{k_}"])
                    S.op("dve", lambda e, g1=g1, m8=m8, t1=t1, b=b: e.scalar_tensor_tensor(out=t1[:], in0=g1[:], scalar=m8[:, 2:3], in1=pastneg[:, b, :],
                                                                                       op0=ALU.is_lt, op1=ALU.mult),
                         reads=[f"g1_{k_}", f"m8_{k_}", "cf"], writes=[f"t1_{k_}"])
                    S.op("dve", lambda e, t1=t1, mt=mt, b=b: e.tensor_tensor(out=mt[:, 64:96], in0=t1[:], in1=futneg[:, b, :], op=ALU.add),
                         reads=[f"t1_{k_}", "cf"], writes=[f"mtpad{k_}"])
                    S.op("pe", lambda e, mt=mt, pb=pb, tt=tt: e.transpose(out=pb[0:96, tt * 128:(tt + 1) * 128], in_=mt[:], identity=self.identb[:]),
                         reads=[f"mtpad{k_}", "identb"], writes=[pbk], signal=(tt == 3))
                self.copy(self.cp_eng(), QAUG[64:96, h, tg * 512:(tg + 1) * 512], pb[64:96, 0:512], reads=[pbk], writes=[f"qaug{h}"])
        NPT = 4
        PT = [self.carve(A_TMP + 1024 * i, [128, 512], BF16) for i in range(NPT)]
        osb = self.carve(A_TMP + 4096, [128, 512], F32)
        rec = self.carve(A_TMP + 6144, [128, 512], F32)
        otmp = self.carve(A_TMP + 8192, [128, 512], BF16)
        SB = [(self.psf[4][:], "psf4"), (self.psf[5][:], "psf5"),
              (self.psb[0][:].bitcast(F32), "psb0"), (self.psb[1][:].bitcast(F32), "psb1")]
        NSB = len(SB)
        for h in range(6):
            steps = []
            for i in range(4):
                S.dma("sp", lambda e, i=i, h=h: e.dma_start(
                    out=KT[i][0:64, :], in_=self.ccK_out[h // 2][i * 128 + (h % 2) * 64:i * 128 + (h % 2) * 64 + 64, :]),
                    reads=[f"ccK_out{h // 2}"], writes=[f"kt{i}"])
                S.dma("sp", lambda e, i=i, h=h: e.dma_start(
                    out=VR[i][:, :, 0:64],
                    in_=self.ccV_out[h // 2][i * 128:i * 128 + 128, :].rearrange("p (t c) -> p t c", c=128)[:, :, (h % 2) * 64:(h % 2) * 64 + 64]),
                    reads=[f"ccV_out{h // 2}"], writes=[f"vr{i}"])
                for t in range(NT):
                    for g in range(4):
                        steps.append((i, t, g))
            ns = len(steps)

            def emit_s(k):
                i, t, g = steps[k]
                ps, psk = SB[k % NSB]
                need_c = (4 * g <= t < 4 * g + 4)
                S.op("pe", lambda e, ps=ps, i=i, t=t, g=g, h=h: e.matmul(ps, lhsT=KT[i][0:96, t * 128:(t + 1) * 128], rhs=QAUG[:, h, g * 512:(g + 1) * 512],
                                                                       start=True, stop=not need_c),
                     reads=[f"kt{i}", f"qaug{h}"], writes=[psk], signal=not need_c)
                if need_c:
                    S.op("pe", lambda e, ps=ps, i=i, t=t, g=g: e.matmul(ps, lhsT=self.identR[:, i, :], rhs=self.cmask[:, t - 4 * g, :], start=False, stop=True),
                         reads=["identR", "cmask"], writes=[psk])

            def emit_pv(k):
                i, t, g = steps[k]
                ps, psk = SB[k % NSB]
                pt = PT[k % NPT]
                S.op("act", lambda e, ps=ps, pt=pt: e.activation(out=pt, in_=ps, func=AF.Exp, scale=0.125), reads=[psk], writes=[f"pt{k % NPT}"])
                S.op("pe", lambda e, pt=pt, i=i, t=t, g=g: e.matmul(self.psf[g][0:65, :], lhsT=VR[i][:, t, 0:65], rhs=pt,
                                                                  start=(i == 0 and t == 0), stop=(i == 3 and t == NT - 1)),
                     reads=[f"pt{k % NPT}", f"vr{i}"], writes=[f"psf{g}"], signal=True)

            LOOK = NSB - 1
            for k in range(min(LOOK, ns)):
                emit_s(k)
            for k in range(ns):
                if k + LOOK < ns:
                    emit_s(k + LOOK)
                emit_pv(k)
            for g in range(4):
                po = self.psf[g]
                S.op("act", lambda e, po=po: e.activation(out=osb[0:65, :], in_=po[0:65, :], func=AF.Copy), reads=[f"psf{g}"], writes=["osb"])
                S.op("dve", lambda e: e.reciprocal(out=rec[64:65, :], in_=osb[64:65, :]), reads=["osb"], writes=["rec"])
                pr_ = self.psf[4 + g % 2]
                prk = f"psf{4 + g % 2}"
                S.op("pe", lambda e, pr_=pr_: e.matmul(pr_[0:64, :], lhsT=onesf[64:65, 0:64], rhs=rec[64:65, :], start=True, stop=True),
                     reads=["cf", "rec"], writes=[prk])
                ch = 3 + h // 2
                if h % 2 == 0:
                    S.op("dve", lambda e, pr_=pr_, g=g, ch=ch: e.tensor_tensor(out=MIXT[0:64, ch, g * 512:(g + 1) * 512], in0=osb[0:64, :], in1=pr_[0:64, :], op=ALU.mult),
                         reads=["osb", prk], writes=[f"mixTm{h}_{g}"])
                else:
                    S.op("dve", lambda e, pr_=pr_: e.tensor_tensor(out=otmp[0:64, :], in0=osb[0:64, :], in1=pr_[0:64, :], op=ALU.mult),
                         reads=["osb", prk], writes=["otmp"])
                    S.dma("sp", lambda e, g=g, ch=ch: e.dma_start(out=MIXT[64:128, ch, g * 512:(g + 1) * 512], in_=otmp[0:64, :]),
                          reads=["otmp"], writes=[f"mixTm{h}_{g}"])

    def final(self):
        S = self.S
        S.barrier()
        X = self.carve(A_X, [128, NT, D_MODEL], F32)
        gain = self.carve(A_GAIN, [128, D_MODEL], F32)
        S.dma("sp", lambda e: e.dma_start(out=gain, in_=self.fnorm[0:1, :].to_broadcast([128, D_MODEL])), writes=["gain"])
        self.rms_stats()
        yo = self.y_out.rearrange("(t p) f -> p t f", p=128)
        ob = [self.carve(A_HT + 4096 * i, [128, D_MODEL], F32) for i in range(2)]
        for t in range(NT):
            o = ob[t % 2]
            S.op("dve", lambda e, t=t, o=o: e.scalar_tensor_tensor(out=o, in0=X[:, t, :], scalar=self.rstd[:, t:t + 1], in1=gain, op0=ALU.mult, op1=ALU.mult),
                 reads=[f"x{t}", "rstd", "gain"], writes=[f"fo{t % 2}"])
            S.dma("sp", lambda e, t=t, o=o: e.dma_start(out=yo[:, t, :], in_=o), reads=[f"fo{t % 2}"], writes=[f"y{t}"])


_PROG_CACHE = {}


def _get_prog(depth, do_final, taps=(), stop=None):
    key = (depth, do_final, tuple(taps), stop)
    if key not in _PROG_CACHE:
        p = Prog(depth, do_final, taps, stop)
        p.build()
        _PROG_CACHE[key] = p
    return _PROG_CACHE[key]


def _layer_inputs(inp, l0, l1):
    f32 = np.float32
    sl = slice(l0, l1)
    d = {}
    for f in (1, 2):
        d[f"wg{f}"] = np.ascontiguousarray(inp[f"ffn{f}_wg"][sl], f32)
        d[f"wu{f}"] = np.ascontiguousarray(inp[f"ffn{f}_wu"][sl], f32)
        d[f"wd{f}"] = np.ascontiguousarray(inp[f"ffn{f}_wd"][sl], f32)
    d["w_in"] = np.ascontiguousarray(inp["w_in"][sl], f32)
    d["w_out"] = np.ascontiguousarray(inp["w_out"][sl], f32)
    d["norms"] = np.ascontiguousarray(np.stack([inp["ffn1_norm"][sl], inp["mix_norm"][sl], inp["ffn2_norm"][sl]], 1), f32)
    d["fnorm"] = np.ascontiguousarray(inp["final_norm"], f32).reshape(1, D_MODEL)
    nl = l1 - l0
    cp = np.zeros((nl, 128, 2, 34), f32)
    cw = np.asarray(inp["conv_w"][sl], f32)
    cp[:, :, :, 0:31] = cw.transpose(0, 2, 1).reshape(nl, 2, 128, 31).transpose(0, 2, 1, 3)
    for j, nm in ((31, "conv_b"), (32, "conv_ln_g"), (33, "conv_ln_b")):
        cp[:, :, :, j] = np.asarray(inp[nm][sl], f32).reshape(nl, 2, 128).transpose(0, 2, 1)
    d["convp"] = cp
    return d


def _run(inp, xs, l0, l1, do_final, taps=(), stop=None):
    prog = _get_prog(l1 - l0, do_final, taps, stop)
    shared = _layer_inputs(inp, l0, l1)
    pos = np.asarray(inp["positions"], np.int32)
    in_maps = []
    for c in range(NCORES):
        b, r = c // 4, c % 4
        cf, cm, er = _host_consts(r)
        m = dict(shared)
        m["x"] = np.ascontiguousarray(xs[c], np.float32)
        m["pos"] = np.ascontiguousarray(pos[b, r * TOK:(r + 1) * TOK].reshape(NT, 128).T)
        m["cf"] = cf
        m["cmask"] = cm
        m["erows"] = er
        in_maps.append(m)
    res = run_bass_kernel_spmd(prog.nc, in_maps, core_ids=list(range(NCORES)))
    return res.results


FUSED = True


def kernel(**inputs):
    inp = {k: np.asarray(v) for k, v in inputs.items()}
    x = np.asarray(inp["x"], np.float32)
    xs = [x[c // 4, (c % 4) * TOK:(c % 4 + 1) * TOK] for c in range(NCORES)]
    if FUSED:
        res = _run(inp, xs, 0, DEPTH, True)
        xs = [r["y"] for r in res]
    else:
        for l in range(DEPTH):
            res = _run(inp, xs, l, l + 1, l == DEPTH - 1)
            xs = [r["y"] for r in res]
    out = np.zeros((2, SEQ, D_MODEL), np.float32)
    for c in range(NCORES):
        out[c // 4, (c % 4) * TOK:(c % 4 + 1) * TOK] = xs[c]
    return out
```
